# Optimizing a Trainium2 kernel written in Bass

```python
import jax
import jax.numpy as jnp
from jax import lax
import numpy as np

D_MODEL = 2048
BATCH = 4
SEQ = 4096
DEPTH = 1

N_HEADS = 16
N_KV_GROUPS = 4
HEADS_PER_GROUP = N_HEADS // N_KV_GROUPS
HEAD_DIM = 128
L_CMP = 32
STRIDE_CMP = 16
CMP_HIDDEN = HEAD_DIM
L_SEL = 64
N_SEL = 16
WINDOW = 512
Q_BLOCK = 32
CONV_CH = D_MODEL // 2
CONV_WIDTH = 31
PEER_HEADS = 8
PEER_NKEYS = 128
PEER_EXPERTS = PEER_NKEYS * PEER_NKEYS
PEER_QDIM = 256
PEER_HALF = PEER_QDIM // 2
PEER_TOPK = 16
PEER_CHUNK = 128

EPS = 1e-6
NEG_BIG = 1e30

Q_COLS = N_HEADS * HEAD_DIM
KV_COLS = N_KV_GROUPS * HEAD_DIM
IN_SPLIT_SIZES = (Q_COLS,) + (KV_COLS,) * 6 + (3 * N_HEADS, 2 * CONV_CH, 2 * D_MODEL)
IN_COLS = sum(IN_SPLIT_SIZES)
IN_SPLIT_POINTS = tuple(int(v) for v in np.cumsum(IN_SPLIT_SIZES)[:-1])

kernel_name = 'hybrid_nsa_conformer_peer_adaln'


def rms_norm(x, g):
    xf = x.astype(jnp.float32)
    y = xf * lax.rsqrt(jnp.mean(xf * xf, axis=-1, keepdims=True) + EPS)
    return (y * g).astype(x.dtype)


def layer_norm(x, g, b):
    xf = x.astype(jnp.float32)
    mu = jnp.mean(xf, axis=-1, keepdims=True)
    xc = xf - mu
    y = xc * lax.rsqrt(jnp.mean(xc * xc, axis=-1, keepdims=True) + EPS)
    return (y * g + b).astype(x.dtype)


def masked_softmax(s, mask):
    s = jnp.where(mask, s.astype(jnp.float32), -NEG_BIG)
    p = jax.nn.softmax(s, axis=-1)
    return jnp.where(mask, p, 0.0)


def compress_blocks(kv, pos, w1, w2):
    B, S, G, dk = kv.shape
    n_cmp = (S - L_CMP) // STRIDE_CMP + 1
    idx = jnp.arange(n_cmp)[:, None] * STRIDE_CMP + jnp.arange(L_CMP)[None, :]
    blocks = kv[:, idx] + pos[None, None, :, None, :]
    blocks = blocks.transpose(0, 1, 3, 2, 4).reshape(B, n_cmp, G, L_CMP * dk)
    return jax.nn.gelu(blocks @ w1) @ w2


def nsa_attention(q, k_cmp, v_cmp, k_slc, v_slc, k_win, v_win, gates):
    B, S, G, HPG, DK = q.shape
    scale = DK ** -0.5
    n_cmp = k_cmp.shape[1]
    n_blk = S // L_SEL
    n_sel = min(N_SEL, n_blk)
    n_q = S // Q_BLOCK
    cmp_start = jnp.arange(n_cmp) * STRIDE_CMP
    cmp_end = cmp_start + L_CMP - 1
    blk_start = jnp.arange(n_blk) * L_SEL
    overlap = ((cmp_start[:, None] < blk_start[None, :] + L_SEL)
               & (cmp_start[:, None] + L_CMP > blk_start[None, :])).astype(jnp.float32)
    k_blocks = k_slc.reshape(B, n_blk, L_SEL, G, DK).transpose(0, 3, 1, 2, 4)
    v_blocks = v_slc.reshape(B, n_blk, L_SEL, G, DK).transpose(0, 3, 1, 2, 4)
    k_win_p = jnp.pad(k_win, ((0, 0), (WINDOW, 0), (0, 0), (0, 0)))
    v_win_p = jnp.pad(v_win, ((0, 0), (WINDOW, 0), (0, 0), (0, 0)))
    bi = jnp.arange(B)[:, None, None, None]
    gi = jnp.arange(G)[None, :, None, None]
    q_chunks = q.reshape(B, n_q, Q_BLOCK, G, HPG, DK).transpose(1, 0, 2, 3, 4, 5)
    g_chunks = gates.reshape(B, n_q, Q_BLOCK, G, HPG, 3).transpose(1, 0, 2, 3, 4, 5)
    starts = jnp.arange(n_q, dtype=jnp.int32) * Q_BLOCK

    def query_block(args):
        q_c, g_c, s0 = args
        t = s0 + jnp.arange(Q_BLOCK, dtype=jnp.int32)
        s = jnp.einsum('btghd,bngd->bghtn', q_c, k_cmp) * scale
        p_cmp = masked_softmax(s, cmp_end[None, :] <= t[:, None])
        o_cmp = jnp.einsum('bghtn,bngd->btghd', p_cmp.astype(v_cmp.dtype), v_cmp)
        imp = jnp.einsum('bghtn,nj->bgtj', p_cmp, overlap)
        cur = (t // L_SEL)[:, None]
        j = jnp.arange(n_blk)[None, :]
        forced = (j == 0) | (j == cur) | (j == cur - 1)
        score = jnp.where(forced, NEG_BIG, jnp.where(blk_start[None, :] <= t[:, None], imp, -NEG_BIG))
        _, idx = lax.top_k(score, n_sel)
        k_g = k_blocks[bi, gi, idx]
        v_g = v_blocks[bi, gi, idx]
        s = jnp.einsum('btghd,bgtnld->bghtnl', q_c, k_g) * scale
        kpos = idx[..., None] * L_SEL + jnp.arange(L_SEL)
        mask = (kpos <= t[None, None, :, None, None])[:, :, None]
        flat = n_sel * L_SEL
        p = masked_softmax(s.reshape(B, G, HPG, Q_BLOCK, flat),
                           mask.reshape(B, G, 1, Q_BLOCK, flat)).reshape(s.shape)
        o_slc = jnp.einsum('bghtnl,bgtnld->btghd', p.astype(v_g.dtype), v_g)
        k_w = lax.dynamic_slice_in_dim(k_win_p, s0, WINDOW + Q_BLOCK, axis=1)
        v_w = lax.dynamic_slice_in_dim(v_win_p, s0, WINDOW + Q_BLOCK, axis=1)
        kpos_w = s0 - WINDOW + jnp.arange(WINDOW + Q_BLOCK, dtype=jnp.int32)
        diff = t[:, None] - kpos_w[None, :]
        mask_w = (diff >= 0) & (diff < WINDOW) & (kpos_w[None, :] >= 0)
        s = jnp.einsum('btghd,bkgd->bghtk', q_c, k_w) * scale
        p = masked_softmax(s, mask_w)
        o_win = jnp.einsum('bghtk,bkgd->btghd', p.astype(v_w.dtype), v_w)
        o = g_c[..., 0:1] * o_cmp + g_c[..., 1:2] * o_slc + g_c[..., 2:3] * o_win
        return o.reshape(B, Q_BLOCK, G * HPG * DK)

    out = lax.map(query_block, (q_chunks, g_chunks, starts))
    return out.transpose(1, 0, 2, 3).reshape(B, S, G * HPG * DK)


def conformer_conv(a, dw_w, dw_b, ln_g, ln_b, pw_w, pw_b):
    u, v = jnp.split(a, 2, axis=-1)
    u = u * jax.nn.sigmoid(v)
    y = lax.conv_general_dilated(u, dw_w[:, None, :], window_strides=(1,),
                                 padding=[(CONV_WIDTH - 1, 0)],
                                 dimension_numbers=('NWC', 'WIO', 'NWC'),
                                 feature_group_count=CONV_CH)
    y = jax.nn.silu(layer_norm(y + dw_b, ln_g, ln_b))
    return y @ pw_w + pw_b


def peer_ffn(h, w_q, sub_keys, u_emb, v_emb):
    B, S, D = h.shape
    n_tok = B * S
    hf = h.reshape(n_tok, D)
    q = (hf @ w_q).reshape(n_tok, PEER_HEADS, 2, PEER_HALF)
    s = jnp.einsum('nhpd,hpkd->nhpk', q, sub_keys).astype(jnp.float32)
    s1, i1 = lax.top_k(s[:, :, 0], PEER_TOPK)
    s2, i2 = lax.top_k(s[:, :, 1], PEER_TOPK)
    cand_s = (s1[..., :, None] + s2[..., None, :]).reshape(n_tok, PEER_HEADS, PEER_TOPK * PEER_TOPK)
    cand_i = (i1[..., :, None] * PEER_NKEYS + i2[..., None, :]).reshape(n_tok, PEER_HEADS, PEER_TOPK * PEER_TOPK)
    top_s, pos = lax.top_k(cand_s, PEER_TOPK)
    expert = jnp.take_along_axis(cand_i, pos, axis=-1)
    gate = jax.nn.softmax(top_s, axis=-1)
    n_chunk = n_tok // PEER_CHUNK
    E = PEER_HEADS * PEER_TOPK
    xs = (hf.reshape(n_chunk, PEER_CHUNK, D),
          expert.reshape(n_chunk, PEER_CHUNK, E),
          gate.reshape(n_chunk, PEER_CHUNK, E))

    def token_chunk(args):
        x_c, e_c, g_c = args
        a = jnp.einsum('td,ted->te', x_c, u_emb[e_c]).astype(jnp.float32)
        coef = (jax.nn.gelu(a) * g_c).astype(v_emb.dtype)
        return jnp.einsum('te,ted->td', coef, v_emb[e_c])

    return lax.map(token_chunk, xs).reshape(B, S, D)


def hybrid_layer(x, c, norm1_g, norm2_g, w_ada, b_ada, w_in, b_in, q_norm_g, k_norm_g,
                 cmp_pos_k, cmp_pos_v, cmp_k_w1, cmp_k_w2, cmp_v_w1, cmp_v_w2, w_nsa_out,
                 conv_dw_w, conv_dw_b, conv_ln_g, conv_ln_b, conv_pw_w, conv_pw_b, w_out,
                 peer_w_q, peer_sub_keys, peer_u, peer_v):
    B, S, D = x.shape
    mod = jax.nn.silu(c) @ w_ada + b_ada
    shift1, scale1, gate1, shift2, scale2, gate2 = jnp.split(mod[:, None, :], 6, axis=-1)
    h = rms_norm(x, norm1_g) * (1.0 + scale1) + shift1
    z = h @ w_in + b_in
    q, kc, vc, ks, vs, kw, vw, g_nsa, a_glu, g_merge = jnp.split(z, IN_SPLIT_POINTS, axis=-1)
    gsh = (B, S, N_KV_GROUPS, HEAD_DIM)
    q = rms_norm(q.reshape(B, S, N_KV_GROUPS, HEADS_PER_GROUP, HEAD_DIM), q_norm_g)
    k_cmp = rms_norm(compress_blocks(kc.reshape(gsh), cmp_pos_k, cmp_k_w1, cmp_k_w2), k_norm_g[0])
    v_cmp = compress_blocks(vc.reshape(gsh), cmp_pos_v, cmp_v_w1, cmp_v_w2)
    k_slc = rms_norm(ks.reshape(gsh), k_norm_g[1])
    k_win = rms_norm(kw.reshape(gsh), k_norm_g[2])
    gates = jax.nn.sigmoid(g_nsa.reshape(B, S, N_KV_GROUPS, HEADS_PER_GROUP, 3))
    y_attn = nsa_attention(q, k_cmp, v_cmp, k_slc, vs.reshape(gsh), k_win, vw.reshape(gsh), gates) @ w_nsa_out
    y_conv = conformer_conv(a_glu, conv_dw_w, conv_dw_b, conv_ln_g, conv_ln_b, conv_pw_w, conv_pw_b)
    g_m = jax.nn.sigmoid(g_merge.reshape(B, S, 2, D))
    mixed = (g_m[:, :, 0] * y_attn + g_m[:, :, 1] * y_conv) @ w_out
    x = x + gate1 * mixed
    h2 = rms_norm(x, norm2_g) * (1.0 + scale2) + shift2
    return x + gate2 * peer_ffn(h2, peer_w_q, peer_sub_keys, peer_u, peer_v)


def setup_inputs(seed: int = 0) -> dict:
    key = jax.random.key(seed)
    ks = jax.random.split(key, 28)
    f32 = jnp.float32
    L = DEPTH

    def nrm(k, shape, s):
        return jax.random.normal(k, shape, f32) * s

    return {
        'x': nrm(ks[0], (BATCH, SEQ, D_MODEL), 1.0),
        'c': nrm(ks[1], (BATCH, D_MODEL), 1.0),
        'norm1_g': 1.0 + nrm(ks[2], (L, D_MODEL), 0.02),
        'norm2_g': 1.0 + nrm(ks[3], (L, D_MODEL), 0.02),
        'w_ada': nrm(ks[4], (L, D_MODEL, 6 * D_MODEL), 0.5 * D_MODEL ** -0.5),
        'b_ada': nrm(ks[5], (L, 6 * D_MODEL), 0.02),
        'w_in': nrm(ks[6], (L, D_MODEL, IN_COLS), D_MODEL ** -0.5),
        'b_in': nrm(ks[7], (L, IN_COLS), 0.02),
        'q_norm_g': 1.0 + nrm(ks[8], (L, HEAD_DIM), 0.02),
        'k_norm_g': 1.0 + nrm(ks[9], (L, 3, HEAD_DIM), 0.02),
        'cmp_pos_k': nrm(ks[10], (L, L_CMP, HEAD_DIM), 0.1),
        'cmp_pos_v': nrm(ks[11], (L, L_CMP, HEAD_DIM), 0.1),
        'cmp_k_w1': nrm(ks[12], (L, L_CMP * HEAD_DIM, CMP_HIDDEN), (L_CMP * HEAD_DIM) ** -0.5),
        'cmp_k_w2': nrm(ks[13], (L, CMP_HIDDEN, HEAD_DIM), CMP_HIDDEN ** -0.5),
        'cmp_v_w1': nrm(ks[14], (L, L_CMP * HEAD_DIM, CMP_HIDDEN), (L_CMP * HEAD_DIM) ** -0.5),
        'cmp_v_w2': nrm(ks[15], (L, CMP_HIDDEN, HEAD_DIM), CMP_HIDDEN ** -0.5),
        'w_nsa_out': nrm(ks[16], (L, Q_COLS, D_MODEL), Q_COLS ** -0.5),
        'conv_dw_w': nrm(ks[17], (L, CONV_WIDTH, CONV_CH), CONV_WIDTH ** -0.5),
        'conv_dw_b': nrm(ks[18], (L, CONV_CH), 0.02),
        'conv_ln_g': 1.0 + nrm(ks[19], (L, CONV_CH), 0.02),
        'conv_ln_b': nrm(ks[20], (L, CONV_CH), 0.02),
        'conv_pw_w': nrm(ks[21], (L, CONV_CH, D_MODEL), CONV_CH ** -0.5),
        'conv_pw_b': nrm(ks[22], (L, D_MODEL), 0.02),
        'w_out': nrm(ks[23], (L, D_MODEL, D_MODEL), D_MODEL ** -0.5),
        'peer_w_q': nrm(ks[24], (L, D_MODEL, PEER_HEADS * PEER_QDIM), D_MODEL ** -0.5),
        'peer_sub_keys': nrm(ks[25], (L, PEER_HEADS, 2, PEER_NKEYS, PEER_HALF), PEER_HALF ** -0.5),
        'peer_u': nrm(ks[26], (L, PEER_EXPERTS, D_MODEL), D_MODEL ** -0.5),
        'peer_v': nrm(ks[27], (L, PEER_EXPERTS, D_MODEL), 1.0),
    }


def reference(x, c, norm1_g, norm2_g, w_ada, b_ada, w_in, b_in, q_norm_g, k_norm_g,
              cmp_pos_k, cmp_pos_v, cmp_k_w1, cmp_k_w2, cmp_v_w1, cmp_v_w2, w_nsa_out,
              conv_dw_w, conv_dw_b, conv_ln_g, conv_ln_b, conv_pw_w, conv_pw_b, w_out,
              peer_w_q, peer_sub_keys, peer_u, peer_v):
    for l in range(DEPTH):
        x = hybrid_layer(x, c, norm1_g[l], norm2_g[l], w_ada[l], b_ada[l], w_in[l], b_in[l],
                         q_norm_g[l], k_norm_g[l], cmp_pos_k[l], cmp_pos_v[l],
                         cmp_k_w1[l], cmp_k_w2[l], cmp_v_w1[l], cmp_v_w2[l], w_nsa_out[l],
                         conv_dw_w[l], conv_dw_b[l], conv_ln_g[l], conv_ln_b[l],
                         conv_pw_w[l], conv_pw_b[l], w_out[l],
                         peer_w_q[l], peer_sub_keys[l], peer_u[l], peer_v[l])
    return x
```

```python
import numpy as np
import ml_dtypes
from contextlib import ExitStack
import concourse.bass as bass
import concourse.mybir as mybir
from concourse.bass_utils import run_bass_kernel_spmd

F32 = mybir.dt.float32
BF16 = mybir.dt.bfloat16
AF = mybir.ActivationFunctionType
ALU = mybir.AluOpType
AX = mybir.AxisListType

ENGS = ("pe", "act", "dve", "pool", "sp")
NDSEM = 24


class Op:
    __slots__ = ("eng", "fn", "deps", "is_dma", "sig", "idx", "dma_n")


class Sched:
    def __init__(self, nc):
        self.nc = nc
        self.ops = []
        self.last_w = {}
        self.rd_eng = {}
        self.rd_dma = {}
        self.n_dma = {"hw": 0, "sw": 0}
        self.fence_deps = set()
        self.last_on_eng = {}
        self.dma_since_fence = []

    def add(self, eng, fn, reads=(), writes=(), dma=False):
        op = Op()
        op.eng, op.fn, op.is_dma, op.sig, op.dma_n = eng, fn, dma, None, -1
        op.idx = len(self.ops)
        deps = set(self.fence_deps)
        for r in reads:
            w = self.last_w.get(r)
            if w is not None:
                deps.add(w)
        for k in writes:
            w = self.last_w.get(k)
            if w is not None:
                deps.add(w)
            for rd in self.rd_eng.get(k, {}).values():
                deps.add(rd)
            for rd in self.rd_dma.get(k, ()):
                deps.add(rd)
        op.deps = deps
        for r in reads:
            if dma:
                self.rd_dma.setdefault(r, []).append(op.idx)
            else:
                self.rd_eng.setdefault(r, {})[eng] = op.idx
        for k in writes:
            self.last_w[k] = op.idx
            self.rd_eng[k] = {}
            self.rd_dma[k] = []
        if dma:
            cls = "sw" if eng == "pool" else "hw"
            op.dma_n = (cls, self.n_dma[cls])
            self.n_dma[cls] += 1
            self.dma_since_fence.append(op.idx)
        else:
            self.last_on_eng[eng] = op.idx
        self.ops.append(op)
        return op

    def fence(self):
        d = set(self.last_on_eng.values()) | set(self.dma_since_fence)
        self.fence_deps = d
        self.dma_since_fence = []
        self.last_w.clear()
        self.rd_eng.clear()
        self.rd_dma.clear()

    def emit(self, final_ops=()):
        nc, ops = self.nc, self.ops
        needed = [False] * len(ops)
        for op in ops:
            for d in op.deps:
                needed[d] = True
        for op in final_ops:
            needed[op.idx] = True
        esem = {e: nc.alloc_semaphore(name="se_" + e) for e in ENGS}
        dsem = {("hw", i): nc.alloc_semaphore(name="sdh_%d" % i) for i in range(NDSEM)}
        dsem.update({("sw", i): nc.alloc_semaphore(name="sds_%d" % i) for i in range(NDSEM)})
        cnt = {e: 0 for e in ENGS}
        for op in ops:
            if op.is_dma:
                op.sig = (dsem[(op.dma_n[0], op.dma_n[1] % NDSEM)], 16 * (op.dma_n[1] // NDSEM + 1))
            elif needed[op.idx]:
                cnt[op.eng] += 1
                op.sig = (esem[op.eng], cnt[op.eng])
        per_eng = {e: [op for op in ops if op.eng == e] for e in ENGS}
        nwait = {e: 0 for e in ENGS}

        def run_engine(ename, eng):
            known = {e: 0 for e in ENGS}
            known_d = {}
            for op in per_eng[ename]:
                waits = {}
                for d in op.deps:
                    p = ops[d]
                    if p.is_dma:
                        k, v = (p.dma_n[0], p.dma_n[1] % NDSEM), p.sig[1]
                        if known_d.get(k, 0) < v:
                            waits[("d", k)] = max(waits.get(("d", k), 0), v)
                    else:
                        if p.eng == ename and ename == "pe":
                            continue
                        v = p.sig[1]
                        if known[p.eng] < v:
                            waits[("e", p.eng)] = max(waits.get(("e", p.eng), 0), v)
                if op.is_dma and op.dma_n[1] >= NDSEM:
                    k, v = (op.dma_n[0], op.dma_n[1] % NDSEM), 16 * (op.dma_n[1] // NDSEM)
                    if known_d.get(k, 0) < v:
                        waits[("d", k)] = max(waits.get(("d", k), 0), v)
                for (kind, k), v in waits.items():
                    if kind == "d":
                        eng.wait_ge(dsem[k], v)
                        known_d[k] = v
                    else:
                        eng.wait_ge(esem[k], v)
                        known[k] = v
                    nwait[ename] += 1
                ins = op.fn(eng)
                if op.sig is not None:
                    ins.then_inc(op.sig[0], 16 if op.is_dma else 1)
            if ename == "sp":
                for op in final_ops:
                    eng.wait_ge(op.sig[0], op.sig[1])

        with nc.Block() as block:
            @block.tensor
            def _(e):
                run_engine("pe", e)

            @block.scalar
            def _(e):
                run_engine("act", e)

            @block.vector
            def _(e):
                run_engine("dve", e)

            @block.gpsimd
            def _(e):
                run_engine("pool", e)

            @block.sync
            def _(e):
                run_engine("sp", e)
        return {e: (len(per_eng[e]), nwait[e]) for e in ENGS}


class T:
    __slots__ = ("ap", "key")

    def __init__(self, ap, key):
        self.ap, self.key = ap, key

    def __getitem__(self, idx):
        return T(self.ap[idx], self.key)

    def k(self, *sub):
        return T(self.ap, (self.key,) + sub)


D = 2048
NTOK = 2048
NPRE = 2048
KC = 16
EPS = 1e-6
OQ, OKC, OVC, OKS, OVS, OKW, OVW, OGN, OGLU, OGM = 0, 2048, 2560, 3072, 3584, 4096, 4608, 5120, 5168, 7216
INC = 11312

CFG = {"phases": "ABCDFGH", "debug": ()}


class Builder:
    def __init__(self):
        self.nc = bass.Bass("TRN2", target_bir_lowering=False)
        self.S = Sched(self.nc)
        self.dram = {}
        self.uid = 0
        self.ps = None

    def din(self, name, shape, dt=F32):
        t = T(self.nc.dram_tensor(name, list(shape), dt, kind="ExternalInput").ap(), name)
        self.dram[name] = t
        return t

    def dscr(self, name, shape, dt, out=False):
        kind = "ExternalOutput" if (out or name in CFG["debug"]) else "Internal"
        t = T(self.nc.dram_tensor(name, list(shape), dt, kind=kind).ap(), name)
        self.dram[name] = t
        return t

    def sb(self, st, name, shape, dt):
        self.uid += 1
        h = st.enter_context(self.nc.sbuf_tensor("%s_%d" % (name, self.uid), list(shape), dt))
        return T(h.ap(), "%s_%d" % (name, self.uid))

    def psum_banks(self):
        self.ps = [T(self.nc.alloc_psum_tensor("psb%d" % i, [128, 512], F32).ap(), "ps%d" % i) for i in range(8)]

    def dma(self, q, out, in_, wkey=None, reads=None):
        self.uid += 1
        wk = out.key if wkey is None else wkey
        return self.S.add(q, lambda e, o=out.ap, i=in_.ap: e.dma_start(out=o, in_=i),
                          reads=[in_.key] if reads is None else reads, writes=[wk], dma=True)

    def mm(self, out, lhsT, rhs, start, stop, sgc=False, reads=()):
        return self.S.add("pe", lambda e, o=out.ap, l=lhsT.ap, r=rhs.ap, s=start, p=stop, g=sgc:
                          e.matmul(o, lhsT=l, rhs=r, start=s, stop=p, skip_group_check=g),
                          reads=[lhsT.key, rhs.key] + list(reads), writes=[out.key])

    def transpose(self, out, in_, ident):
        return self.S.add("pe", lambda e, o=out.ap, i=in_.ap, d=ident.ap: e.transpose(o, i, d),
                          reads=[in_.key, ident.key], writes=[out.key])

    def act(self, out, in_, func, bias=None, scale=1.0, accum=None, reads=()):
        rd = [in_.key] + list(reads)
        kw = {}
        if isinstance(bias, T):
            rd.append(bias.key)
            kw["bias"] = bias.ap
        elif bias is not None:
            kw["bias"] = bias
        if isinstance(scale, T):
            rd.append(scale.key)
            kw["scale"] = scale.ap
        else:
            kw["scale"] = scale
        wr = [out.key]
        if accum is not None:
            kw["accum_out"] = accum.ap
            wr.append(accum.key)
        return self.S.add("act", lambda e, o=out.ap, i=in_.ap, f=func, k=kw: e.activation(out=o, in_=i, func=f, **k),
                          reads=rd, writes=wr)

    def tt(self, eng, out, in0, in1, op, reads=()):
        return self.S.add(eng, lambda e, o=out.ap, a=in0.ap, b=in1.ap, p=op: e.tensor_tensor(out=o, in0=a, in1=b, op=p),
                          reads=[in0.key, in1.key] + list(reads), writes=[out.key])

    def ts(self, eng, out, in0, s1, op0, s2=None, op1=None):
        rd = [in0.key]
        a1 = s1
        if isinstance(s1, T):
            rd.append(s1.key)
            a1 = s1.ap
        a2 = s2
        if isinstance(s2, T):
            rd.append(s2.key)
            a2 = s2.ap
        kw = {} if op1 is None else {"op1": op1}
        return self.S.add(eng, lambda e, o=out.ap, a=in0.ap, x=a1, y=a2, p=op0, k=kw:
                          e.tensor_scalar(out=o, in0=a, scalar1=x, scalar2=y, op0=p, **k),
                          reads=rd, writes=[out.key])

    def stt(self, eng, out, in0, scalar, in1, op0, op1):
        rd = [in0.key, in1.key]
        sc = scalar
        if isinstance(scalar, T):
            rd.append(scalar.key)
            sc = scalar.ap
        return self.S.add(eng, lambda e, o=out.ap, a=in0.ap, s=sc, b=in1.ap, p=op0, q=op1:
                          e.scalar_tensor_tensor(out=o, in0=a, scalar=s, in1=b, op0=p, op1=q),
                          reads=rd, writes=[out.key])

    def copy(self, eng, out, in_, reads=None):
        rd = [in_.key] if reads is None else reads
        if eng == "act":
            return self.S.add("act", lambda e, o=out.ap, i=in_.ap: e.copy(out=o, in_=i), reads=rd, writes=[out.key])
        return self.S.add(eng, lambda e, o=out.ap, i=in_.ap: e.tensor_copy(out=o, in_=i), reads=rd, writes=[out.key])

    def recip(self, out, in_):
        return self.S.add("dve", lambda e, o=out.ap, i=in_.ap: e.reciprocal(out=o, in_=i), reads=[in_.key], writes=[out.key])

    def memset(self, eng, out, val):
        return self.S.add(eng, lambda e, o=out.ap, v=val: e.memset(o, v), writes=[out.key])

    def gelu_tanh(self, out, xs, t1, t2):
        self.tt("pool", t1, xs, xs, ALU.mult)
        self.ts("pool", t1, t1, 0.044715, ALU.mult, 1.0, ALU.add)
        self.tt("pool", t1, t1, xs, ALU.mult)
        self.act(t2, t1, AF.Exp, scale=-1.5957691216)
        self.ts("dve", t2, t2, 1.0, ALU.add)
        self.recip(t2, t2)
        self.tt("pool", out, xs, t2, ALU.mult)

    def rstd_from_psum(self, st_tile, ps, n_feat, width):
        self.act(st_tile, ps, AF.Sqrt, bias=self.eps_t, scale=1.0 / n_feat)
        self.recip(st_tile, st_tile)


def build_program():
    B = Builder()
    nc, S = B.nc, B.S
    ph = CFG["phases"]
    xT = B.din("xT", [128, KC, NPRE + NTOK])
    xtok = B.din("xtok", [NTOK, D])
    c_col = B.din("c_col", [128, KC])
    g1_col = B.din("g1_col", [128, KC])
    g2_col = B.din("g2_col", [128, KC])
    w_ada = B.din("w_ada", [D, 6 * D])
    b_ada = B.din("b_ada", [1, 6 * D])
    w_in = B.din("w_in", [D, INC])
    b_in = B.din("b_in", [1, INC])
    b_in_col = B.din("b_in_col", [128, 96])
    qkg_col = B.din("qkg_col", [128, 4])
    pvalid = B.din("pvalid", [128, 1])
    cmp_w1 = [B.din("cmp_k_w1", [4096, 128]), B.din("cmp_v_w1", [4096, 128])]
    cmp_w2 = [B.din("cmp_k_w2", [128, 128]), B.din("cmp_v_w2", [128, 128])]
    cmp_posT = [B.din("cmp_posT_k", [128, 32]), B.din("cmp_posT_v", [128, 32])]
    ident_in = B.din("ident", [128, 128])
    OVL_in = B.din("OVL", [128, 2, 64])
    SELB_in = B.din("SELB", [NTOK, 64])
    CMPM_in = B.din("CMPM", [128, 2, NTOK])
    WINM_in = B.din("WINM", [4, 128, 8, 512])
    SELC_in = B.din("SELC", [128, 4, 512])
    EX_in = B.din("EX", [64, 32, 128])
    w_nsa_out = B.din("w_nsa_out", [D, D])
    dwT = B.din("dwT", [128, 8, 31])
    cvec = B.din("cvec", [128, 3, 8])
    pwb_col = B.din("pwb_col", [128, 16])
    conv_pw_w = B.din("conv_pw_w", [1024, D])
    w_out = B.din("w_out", [D, D])
    peer_w_q = B.din("peer_w_q", [D, D])
    keysT_in = B.din("keysT", [128, 16, 128])
    peer_uT = B.din("peer_uT", [D, 16384] if "H" in ph else [128, 128])
    peer_v = B.din("peer_v", [16384, D] if "H" in ph else [128, 128])
    out = B.dscr("out", [NTOK, D], F32, out=True)
    modd = B.dscr("modd", [1, 6 * D], F32)
    QT = B.dscr("QT", [16, 128, NTOK], BF16)
    KCT = B.dscr("KCT", [4, 128, NPRE + NTOK], BF16)
    VCT = B.dscr("VCT", [4, 128, NPRE + NTOK], BF16)
    KST = B.dscr("KST", [4, 128, NPRE + NTOK], BF16)
    KWT = B.dscr("KWT", [4, 128, NPRE + NTOK], BF16)
    VS = B.dscr("VS", [NPRE + NTOK, 512], BF16)
    VW = B.dscr("VW", [NPRE + NTOK, 512], BF16)
    GNT = B.dscr("GNT", [48, NTOK], F32)
    UT = B.dscr("UT", [1024, 32 + NTOK], F32)
    GM = B.dscr("GM", [32, 128, NTOK], BF16)
    OT = B.dscr("OT", [16, 128, NTOK], BF16)
    YCG = B.dscr("YCG", [16, 128, NTOK], BF16)
    X1 = B.dscr("X1", [NTOK, D], F32)
    B.psum_banks()
    ps = B.ps
    pst = T(ps[7].ap.bitcast(BF16), "ps7")
    pst6 = T(ps[6].ap.bitcast(BF16), "ps6")
    final_ops = []

    with ExitStack() as gst:
        ones_bf = B.sb(gst, "ones_bf", [128, 128], BF16)
        B.memset("pool", ones_bf, 1.0)
        one_f = B.sb(gst, "one_f", [128, 1], F32)
        B.memset("pool", one_f, 1.0)
        eps_t = B.sb(gst, "eps_t", [128, 1], F32)
        B.memset("pool", eps_t, EPS)
        B.eps_t = eps_t
        modc = B.sb(gst, "modc", [128, 96], F32)
        A1 = B.sb(gst, "A1", [128, KC], F32)
        A2 = B.sb(gst, "A2", [128, KC], F32)
        bcol = B.sb(gst, "bcol", [128, 96], F32)
        B.dma("sp", bcol, b_in_col)
        gcol = B.sb(gst, "gcol", [128, 4], F32)
        B.dma("sp", gcol, qkg_col)
        gqs = B.sb(gst, "gqs", [128, 1], F32)
        B.ts("dve", gqs, gcol[:, 0:1], 128.0 ** -0.5, ALU.mult)
        pv = B.sb(gst, "pv", [128, 1], F32)
        B.dma("sp", pv, pvalid)
        ident = B.sb(gst, "ident", [128, 128], BF16)
        B.dma("pool", ident, ident_in)
        ones_f = B.sb(gst, "ones_f", [128, 128], F32)
        B.memset("pool", ones_f, 1.0)
        kcmpT = B.sb(gst, "kcmpT", [128, 4, 256], BF16)
        vcmp = B.sb(gst, "vcmp", [128, 4, 2, 128], BF16)

        if "A" in ph:
            with ExitStack() as st:
                cc = B.sb(st, "cc", [128, KC], F32)
                B.dma("sp", cc, c_col)
                sc = B.sb(st, "sc", [128, KC], F32)
                B.act(sc, cc, AF.Silu)
                brow = B.sb(st, "brow", [1, 6 * D], F32)
                B.dma("sp", brow, b_ada)
                mrow = B.sb(st, "mrow", [1, 6 * D], F32)
                wa = [B.sb(st, "wa%d" % i, [128, KC, 512], F32) for i in range(2)]
                for n in range(24):
                    w = wa[n % 2]
                    B.dma("sp" if n % 2 == 0 else "act", w, T(w_ada.ap[:, n * 512:(n + 1) * 512].rearrange("(kc p) n -> p kc n", p=128), "w_ada"))
                    pt = ps[n % 2]
                    for kc in range(KC):
                        B.mm(pt[0:1, :], sc[:, kc:kc + 1], w[:, kc, :], kc == 0, kc == KC - 1)
                    B.tt("dve", mrow[0:1, n * 512:(n + 1) * 512].k(n), pt[0:1, :], brow[0:1, n * 512:(n + 1) * 512], ALU.add)
                    for j in range(4):
                        jj = n * 4 + j
                        B.mm(ps[2][:, jj:jj + 1].k(jj), mrow[0:1, jj * 128:(jj + 1) * 128].k(n), one_f[0:1, 0:1], True, True)
                B.copy("dve", modc, T(ps[2].ap[:, 0:96], ps[2].key), reads=[(ps[2].key, jj) for jj in range(96)])
                B.dma("sp", modd, mrow, reads=[(mrow.key, n) for n in range(24)])
                g1 = B.sb(st, "g1", [128, KC], F32)
                B.dma("sp", g1, g1_col)
                B.stt("dve", A1, modc[:, 16:32], 1.0, g1, ALU.add, ALU.mult)
                g2c = B.sb(st, "g2c", [128, KC], F32)
                B.dma("sp", g2c, g2_col)
                B.stt("dve", A2, modc[:, 64:80], 1.0, g2c, ALU.add, ALU.mult)
            S.fence()

        def proj_pass(st, tok0, own):
            hT = B.sb(st, "hT", [128, KC, 2048], BF16)
            with ExitStack() as s1:
                xs = [B.sb(s1, "xs%d" % i, [128, KC, 256], F32) for i in range(2)]
                sq = [B.sb(s1, "sq%d" % i, [128, KC, 256], BF16) for i in range(2)]
                rs = [B.sb(s1, "rs%d" % i, [128, 256], F32) for i in range(2)]
                tm = [B.sb(s1, "tm%d" % i, [128, 256], F32) for i in range(2)]
                for sg in range(8):
                    x_, q_, r_ = xs[sg % 2], sq[sg % 2], rs[sg % 2]
                    c0 = tok0 + sg * 256
                    B.dma("sp", x_, T(xT.ap[:, :, c0:c0 + 256], "xT"))
                    B.act(q_, x_, AF.Square)
                    pt = ps[sg % 2]
                    for kc in range(KC):
                        B.mm(pt[:, 0:256], ones_bf, q_[:, kc, :], kc == 0, kc == KC - 1)
                    B.rstd_from_psum(r_, pt[:, 0:256], D, 256)
                    for kc in range(KC):
                        t_ = tm[kc % 2]
                        B.stt("dve", t_, x_[:, kc, :], A1[:, kc:kc + 1], r_, ALU.mult, ALU.mult)
                        B.act(hT[:, kc, sg * 256:(sg + 1) * 256].k(kc, sg // 2), t_, AF.Identity, bias=modc[:, kc:kc + 1])
            S.fence()
            if "HTD" in CFG["debug"] and own:
                htd = B.dscr("HTD", [128, KC, 2048], BF16)
                B.dma("sp", htd, hT, reads=[(hT.key, kc, g_) for kc in range(KC) for g_ in range(4)])
            wb = [B.sb(st, "wb%d" % i, [128, KC, 512], BF16) for i in range(3)]
            wctr = [0]

            def load_w(c0, cw):
                w = wb[wctr[0] % 3]
                wctr[0] += 1
                B.dma("pool", w[:, :, 0:cw], T(w_in.ap[:, c0:c0 + cw].rearrange("(kc p) n -> p kc n", p=128), "w_in"))
                return w

            pctr = [0]

            def fm_matmul(w, m0, mw, grp):
                pt = ps[pctr[0] % 4]
                pctr[0] += 1
                for kc in range(KC):
                    B.mm(pt[0:mw, :], w[:, kc, m0:m0 + mw], hT[:, kc, grp * 512:(grp + 1) * 512].k(kc, grp), kc == 0, kc == KC - 1)
                return pt

            ob = [B.sb(st, "ob%d" % i, [128, 512], BF16) for i in range(3)]
            of = [B.sb(st, "of%d" % i, [128, 512], F32) for i in range(3)]
            sqb = [B.sb(st, "sqb%d" % i, [128, 512], BF16) for i in range(2)]
            rq = [B.sb(st, "rq%d" % i, [128, 512], F32) for i in range(2)]
            octr = [0]

            def normed_seg(c0, nchunk, bcol0, gsc, dst, dst_tok0, grps=(0, 1, 2, 3)):
                for c4 in range(0, nchunk, 4):
                    w = load_w(c0 + c4 * 128, 512)
                    for m in range(4):
                        ch = c4 + m
                        for grp in grps:
                            pt = fm_matmul(w, m * 128, 128, grp)
                            i = octr[0]
                            octr[0] += 1
                            qf, sq_, r_, o_ = of[i % 3], sqb[i % 2], rq[i % 2], ob[i % 3]
                            bc_ = bcol[:, bcol0 + ch:bcol0 + ch + 1]
                            B.act(qf, pt, AF.Identity, bias=bc_)
                            B.act(sq_, pt, AF.Square, bias=bc_)
                            p2 = ps[4 + i % 2]
                            B.mm(p2, ones_bf, sq_, True, True)
                            B.rstd_from_psum(r_, p2, 128, 512)
                            B.stt("dve", o_, qf, gsc, r_, ALU.mult, ALU.mult)
                            B.dma("sp", T(dst.ap[ch, :, dst_tok0 + grp * 512:dst_tok0 + (grp + 1) * 512], dst.key), o_, wkey=(dst.key, ch, grp, tok0))

            def plain_seg(c0, nchunk, bcol0, dst, dst_tok0):
                for c4 in range(0, nchunk, 4):
                    w = load_w(c0 + c4 * 128, 512)
                    for m in range(4):
                        ch = c4 + m
                        for grp in range(4):
                            pt = fm_matmul(w, m * 128, 128, grp)
                            i = octr[0]
                            octr[0] += 1
                            o_ = ob[i % 3]
                            B.act(o_, pt, AF.Identity, bias=bcol[:, bcol0 + ch:bcol0 + ch + 1])
                            B.dma("sp", T(dst.ap[ch, :, dst_tok0 + grp * 512:dst_tok0 + (grp + 1) * 512], dst.key), o_, wkey=(dst.key, ch, grp, tok0))

            def tokmajor_seg(c0, dst, dst_tok0, brow_b, tiles=range(16)):
                w = load_w(c0, 512)
                for tt_ in tiles:
                    pt = ps[pctr[0] % 4]
                    pctr[0] += 1
                    for kc in range(KC):
                        B.mm(pt, hT[:, kc, tt_ * 128:(tt_ + 1) * 128].k(kc, tt_ // 4), w[:, kc, :], kc == 0, kc == KC - 1)
                    i = octr[0]
                    octr[0] += 1
                    o_ = ob[i % 3]
                    B.tt("dve", o_, pt, brow_b, ALU.add)
                    r0 = dst_tok0 + tt_ * 128
                    B.dma("sp", T(dst.ap[r0:r0 + 128, :], dst.key), o_, wkey=(dst.key, tt_, tok0))

            def glu_seg(grps, dst_c0, keep_last32):
                for half in range(2):
                    wu = load_w(OGLU + half * 512, 512)
                    wv = load_w(OGLU + 1024 + half * 512, 512)
                    for m in range(4):
                        ch = half * 4 + m
                        for grp in grps:
                            pu = fm_matmul(wu, m * 128, 128, grp)
                            pvv = fm_matmul(wv, m * 128, 128, grp)
                            i = octr[0]
                            octr[0] += 1
                            sg_, o_ = of[i % 3], rq[i % 2]
                            B.act(sg_, pvv, AF.Sigmoid, bias=bcol[:, 56 + ch:57 + ch])
                            B.stt("dve", o_, pu, bcol[:, 48 + ch:49 + ch], sg_, ALU.add, ALU.mult)
                            if keep_last32:
                                B.ts("dve", o_[:, 480:512], o_[:, 480:512], pv[:, 0:1], ALU.mult)
                                B.dma("sp", T(UT.ap[ch * 128:(ch + 1) * 128, 0:32], "UT"), o_[:, 480:512], wkey=("UT", ch, "pre"))
                            else:
                                c_ = 32 + grp * 512
                                B.dma("sp", T(UT.ap[ch * 128:(ch + 1) * 128, c_:c_ + 512], "UT"), o_, wkey=("UT", ch, grp))

            bvs = B.sb(st, "bvs", [128, 512], F32)
            bvw = B.sb(st, "bvw", [128, 512], F32)
            B.dma("sp", bvs, T(b_in.ap[0:1, OVS:OVS + 512].partition_broadcast(128), "b_in"))
            B.dma("sp", bvw, T(b_in.ap[0:1, OVW:OVW + 512].partition_broadcast(128), "b_in"))
            plain_seg(OKC, 4, 16, KCT, tok0)
            if CFG.get("bquick"):
                return
            plain_seg(OVC, 4, 20, VCT, tok0)
            normed_seg(OKS, 4, 24, gcol[:, 2:3], KST, tok0)
            normed_seg(OKW, 4, 28, gcol[:, 3:4], KWT, tok0, grps=(0, 1, 2, 3) if own else (3,))
            tokmajor_seg(OVS, VS, tok0, bvs)
            tokmajor_seg(OVW, VW, tok0, bvw, tiles=range(16) if own else range(12, 16))
            if not own:
                glu_seg([3], 0, True)
            else:
                normed_seg(OQ, 16, 0, gqs[:, 0:1], QT, 0)
                glu_seg([0, 1, 2, 3], 32, False)
                w = load_w(OGN, 48)
                for grp in range(4):
                    pt = fm_matmul(w, 0, 48, grp)
                    i = octr[0]
                    octr[0] += 1
                    o_ = of[i % 3]
                    B.act(o_[0:48, :], pt[0:48, :], AF.Sigmoid, bias=bcol[0:48, 32:33])
                    B.dma("sp", T(GNT.ap[:, grp * 512:(grp + 1) * 512], "GNT"), o_[0:48, :], wkey=("GNT", grp))
                for c4 in range(0, 32, 4):
                    w = load_w(OGM + c4 * 128, 512)
                    for m in range(4):
                        ch = c4 + m
                        for grp in range(4):
                            pt = fm_matmul(w, m * 128, 128, grp)
                            i = octr[0]
                            octr[0] += 1
                            o_ = ob[i % 3]
                            B.act(o_, pt, AF.Sigmoid, bias=bcol[:, 64 + ch:65 + ch])
                            B.dma("sp", T(GM.ap[ch, :, grp * 512:(grp + 1) * 512], "GM"), o_, wkey=("GM", ch, grp))

        if "B" in ph:
            with ExitStack() as st:
                proj_pass(st, 0, False)
            S.fence()
            with ExitStack() as st:
                proj_pass(st, NPRE, True)
            S.fence()


        if "C" in ph:
            with ExitStack() as st:
                for kv in range(2):
                    w1 = B.sb(st, "w1_%d" % kv, [128, 32, 128], BF16)
                    B.dma("pool", w1, T(cmp_w1[kv].ap.rearrange("(l d) h -> d l h", d=128), "cw1"))
                    w2 = B.sb(st, "w2_%d" % kv, [128, 128], BF16)
                    B.dma("pool", w2, cmp_w2[kv])
                    posT = B.sb(st, "posT_%d" % kv, [128, 32], BF16)
                    B.dma("pool", posT, cmp_posT[kv])
                    posb = B.sb(st, "posb_%d" % kv, [128, 1], F32)
                    for l in range(32):
                        B.mm(ps[6][:, 0:1], w1[:, l, :], posT[:, l:l + 1], l == 0, l == 31)
                    B.copy("dve", posb, ps[6][:, 0:1])
                    src = KCT if kv == 0 else VCT
                    for g in range(4):
                        xt_ = B.sb(st, "cx_%d_%d" % (kv, g), [128, NPRE + NTOK], BF16)
                        B.dma("sp", xt_, T(src.ap[g], src.key))
                        pt = ps[g % 2]
                        for l in range(32):
                            B.mm(pt[:, 0:255], w1[:, l, :], T(xt_.ap[:, l:l + 16 * 254 + 1:16], xt_.key), l == 0, l == 31)
                        hid = B.sb(st, "hid_%d_%d" % (kv, g), [128, 256], BF16)
                        hx = B.sb(st, "hx_%d_%d" % (kv, g), [128, 256], F32)
                        h1 = B.sb(st, "h1_%d_%d" % (kv, g), [128, 256], F32)
                        h2_ = B.sb(st, "h2_%d_%d" % (kv, g), [128, 256], F32)
                        B.act(hx[:, 0:255], pt[:, 0:255], AF.Identity, bias=posb)
                        B.gelu_tanh(hid[:, 0:255], hx[:, 0:255], h1[:, 0:255], h2_[:, 0:255])
                        if kv == 0:
                            p2 = ps[2 + g % 2]
                            B.mm(p2[:, 0:255], w2, hid[:, 0:255], True, True)
                            sq_ = B.sb(st, "csq_%d" % g, [128, 256], BF16)
                            B.act(sq_[:, 0:255], p2[:, 0:255], AF.Square)
                            p3 = ps[4 + g % 2]
                            B.mm(p3[:, 0:255], ones_bf, sq_[:, 0:255], True, True)
                            r_ = B.sb(st, "crs_%d" % g, [128, 256], F32)
                            B.rstd_from_psum(r_[:, 0:255], p3[:, 0:255], 128, 255)
                            B.stt("dve", kcmpT[:, g, 0:255].k(g), p2[:, 0:255], gcol[:, 1:2], r_[:, 0:255], ALU.mult, ALU.mult)
                        else:
                            for c in range(2):
                                rows = 128 if c == 0 else 127
                                p2 = ps[2 + c]
                                B.mm(p2[0:rows, 0:128], hid[:, c * 128:c * 128 + rows], w2, True, True)
                                B.copy("dve", vcmp[0:rows, g, c, :].k(g, c), p2[0:rows, 0:128])
                if "KCMPD" in CFG["debug"]:
                    B.dma("sp", B.dscr("KCMPD", [128, 4, 256], BF16), kcmpT, reads=[(kcmpT.key, g) for g in range(4)])
                    B.dma("sp", B.dscr("VCMPD", [128, 4, 2, 128], BF16), vcmp, reads=[(vcmp.key, g, c) for g in range(4) for c in range(2)])
            S.fence()

        if "D" in ph:
            with ExitStack() as st:
                OVL = B.sb(st, "OVL", [128, 2, 64], BF16)
                B.dma("pool", OVL, OVL_in)
                SELC = B.sb(st, "SELC", [128, 4, 512], BF16)
                B.dma("pool", SELC, SELC_in)
                EX = B.sb(st, "EX", [64, 32, 128], BF16)
                B.dma("pool", EX, EX_in)
                KS_g = B.sb(st, "KS_g", [128, NPRE + NTOK], BF16)
                KW_g = B.sb(st, "KW_g", [128, NPRE + NTOK], BF16)
                VS_g = B.sb(st, "VS_g", [128, 32, 128], BF16)
                VW_g = B.sb(st, "VW_g", [128, 32, 128], BF16)
                BMs = B.sb(st, "BMs", [128, 32, 512], BF16)
                q4s = [B.sb(st, "q4_%d" % i, [128, 4, 512], BF16) for i in range(2)]
                gbs = B.sb(st, "gbs", [128, 12, 512], F32)
                WINM = B.sb(st, "WINM", [128, 8, 512], BF16)
                CMPM = B.sb(st, "CMPM", [128, 2, 512], BF16)
                SELB = B.sb(st, "SELB", [128, 4, 64], F32)
                oacc = B.sb(st, "oacc", [128, 4, 512], F32)
                Ef = [B.sb(st, "Ef%d" % i, [128, 512], F32) for i in range(2)]
                Eb = [B.sb(st, "Eb%d" % i, [128, 512], BF16) for i in range(6)]
                Pb = [B.sb(st, "Pb%d" % i, [128, 512], BF16) for i in range(6)]
                Em2 = [[B.sb(st, "Em%d_%d" % (k, i), [128, 512], BF16) for i in range(2)] for k in range(2)]
                Pn2 = [[B.sb(st, "Pn%d_%d" % (k, i), [128, 512], BF16) for i in range(2)] for k in range(2)]
                rden = B.sb(st, "rden", [128, 512], F32)
                wgt = B.sb(st, "wgt", [128, 512], F32)
                tmpo = B.sb(st, "tmpo", [128, 512], F32)
                ob_ = [B.sb(st, "obD%d" % i, [128, 512], BF16) for i in range(2)]
                sc = B.sb(st, "sc", [128, 64], F32)
                sc2 = B.sb(st, "sc2", [128, 64], F32)
                m8a = B.sb(st, "m8a", [128, 8], F32)
                m8b = B.sb(st, "m8b", [128, 8], F32)
                thr = B.sb(st, "thr", [128, 1], F32)
                maskf = B.sb(st, "maskf", [128, 64], BF16)
                maskT = B.sb(st, "maskT", [64, 512], BF16)
                PS_S = [ps[0], ps[1]]
                PS_ON, PS_DEN, PS_IMP, PS_BM = ps[2], ps[3], ps[4], ps[5]
                ectr = [0]

                bctr = [0]
                PS_S3 = [ps[0], ps[1], ps[6]]

                def branch_epilogue(hh, row, first, pON, pDEN):
                    steps = [lambda: B.ts("dve", rden, pDEN, 1e-30, ALU.max)]
                    for qq in range(4):
                        steps.append(lambda qq=qq: B.recip(rden[:, qq * 128:(qq + 1) * 128], rden[:, qq * 128:(qq + 1) * 128]))
                    steps.append(lambda: B.tt("dve", wgt, rden, gbs[:, row, :].k(row), ALU.mult))
                    steps.append(lambda: B.tt("dve", tmpo, pON, wgt, ALU.mult))
                    steps.append(lambda: B.tt("pool", oacc[:, hh, :].k(hh), oacc[:, hh, :].k(hh), tmpo, ALU.add))
                    return steps

                for g in range(4):
                    B.dma("sp", KS_g, T(KST.ap[g], "KST"))
                    B.dma("sp", KW_g[:, 1536:NPRE + NTOK], T(KWT.ap[g, :, 1536:NPRE + NTOK], "KWT"))
                    B.dma("sp", VS_g, T(VS.ap[:, g * 128:(g + 1) * 128].rearrange("(c p) d -> p c d", p=128), "VS"))
                    B.dma("sp", VW_g[:, 12:32, :], T(VW.ap[1536:NPRE + NTOK, g * 128:(g + 1) * 128].rearrange("(c p) d -> p c d", p=128), "VW"))
                    for qt in range(4):
                        u0 = qt * 512
                        q4 = q4s[qt % 2]
                        B.dma("sp", q4, T(QT.ap[4 * g:4 * g + 4, :, u0:u0 + 512].rearrange("h d t -> d h t"), "QT"))
                        for r in range(12):
                            row = g * 12 + r
                            B.dma("sp", gbs[:, r, :].k(r), T(GNT.ap[row:row + 1, u0:u0 + 512].partition_broadcast(128), "GNT"))
                        B.dma("pool", WINM, T(WINM_in.ap[qt], "WINM_in"))
                        B.dma("pool", CMPM, T(CMPM_in.ap[:, :, u0:u0 + 512], "CMPM_in"))
                        B.dma("sp", SELB, T(SELB_in.ap[u0:u0 + 512, :].rearrange("(a p) j -> p a j", p=128), "SELB_in"))
                        nch = (NPRE + u0 + 512) // 128
                        for hh in range(4):
                            Em, Pn = Em2[hh % 2], Pn2[hh % 2]
                            for c in range(2):
                                rows = 128 if c == 0 else 127
                                pS = PS_S[ectr[0] % 2]
                                ef = Ef[ectr[0] % 2]
                                ectr[0] += 1
                                B.mm(pS[0:rows, :], kcmpT[:, g, c * 128:c * 128 + rows].k(g), q4[:, hh, :], True, True)
                                B.act(ef[0:rows, :], pS[0:rows, :], AF.Exp)
                                B.tt("dve", Em[c][0:rows, :], ef[0:rows, :], CMPM[0:rows, c, :], ALU.mult)
                            for c in range(2):
                                rows = 128 if c == 0 else 127
                                B.mm(PS_DEN, ones_bf[0:rows, :], Em[c][0:rows, :], c == 0, c == 1)
                            B.ts("dve", rden, PS_DEN, 1e-30, ALU.max)
                            B.recip(rden, rden)
                            for c in range(2):
                                rows = 128 if c == 0 else 127
                                B.tt("dve", Pn[c][0:rows, :], Em[c][0:rows, :], rden[0:rows, :], ALU.mult)
                            for c in range(2):
                                rows = 128 if c == 0 else 127
                                B.mm(PS_ON, vcmp[0:rows, g, c, :].k(g, c), Pn[c][0:rows, :], c == 0, c == 1)
                            for ut in range(4):
                                for c in range(2):
                                    rows = 128 if c == 0 else 127
                                    B.mm(PS_IMP[:, ut * 64:(ut + 1) * 64].k(ut), Pn[c][0:rows, ut * 128:(ut + 1) * 128], OVL[0:rows, c, :],
                                         hh == 0 and c == 0 and ut == 0, hh == 3 and c == 1, sgc=True)
                            B.tt("dve", oacc[:, hh, :].k(hh), PS_ON, gbs[:, hh * 3 + 0, :].k(hh * 3), ALU.mult)
                        for ut in range(4):
                            B.tt("dve", sc, PS_IMP[:, ut * 64:(ut + 1) * 64].k(ut), SELB[:, ut, :], ALU.add,
                                 reads=[(PS_IMP.key, u_) for u_ in range(4)])
                            S.add("dve", lambda e, o=m8a.ap, i=sc.ap: e.max(out=o, in_=i), reads=[sc.key], writes=[m8a.key])
                            S.add("dve", lambda e, o=sc2.ap, r=m8a.ap, i=sc.ap: e.match_replace(out=o, in_to_replace=r, in_values=i, imm_value=-3.0e38),
                                  reads=[sc.key, m8a.key], writes=[sc2.key])
                            S.add("dve", lambda e, o=m8b.ap, i=sc2.ap: e.max(out=o, in_=i), reads=[sc2.key], writes=[m8b.key])
                            B.ts("dve", thr, m8b[:, 7:8], -1.0e29, ALU.max)
                            B.ts("dve", maskf, sc, thr[:, 0:1], ALU.is_ge)
                            B.transpose(pst[0:64, ut * 128:(ut + 1) * 128], maskf, ident)
                            B.copy("act", maskT[:, ut * 128:(ut + 1) * 128], pst[0:64, ut * 128:(ut + 1) * 128])
                        for c in range(nch):
                            pbm = PS_BM if c % 2 == 0 else ps[6]
                            B.mm(pbm, EX[:, c, :], maskT, True, True)
                            di = c - (nch - 4)
                            if di >= 0:
                                B.tt("dve", BMs[:, c, :].k(c), pbm, SELC[:, di, :], ALU.mult)
                            else:
                                B.copy("act", BMs[:, c, :].k(c), pbm)
                        LAG = 3
                        pend_ep = []
                        for hh in range(4):
                            for br in (1, 2):
                                if br == 1:
                                    chunks = [(c, KS_g, VS_g, BMs[:, c, :].k(c)) for c in range(nch)]
                                else:
                                    c0 = (NPRE + u0 - 512) // 128
                                    chunks = [(c0 + i, KW_g, VW_g, WINM[:, i, :]) for i in range(8)]
                                bctr[0] += 1
                                pON, pDEN = (ps[2], ps[3]) if bctr[0] % 2 == 0 else (ps[4], ps[5])
                                n = len(chunks)
                                pbs = {}
                                for ci in range(n + LAG):
                                    if ci < n:
                                        c, Kg, Vg, msk = chunks[ci]
                                        pS = PS_S3[ectr[0] % 3]
                                        eb, pb = Eb[ectr[0] % 6], Pb[ectr[0] % 6]
                                        ectr[0] += 1
                                        B.mm(pS, Kg[:, c * 128:(c + 1) * 128], q4[:, hh, :], True, True)
                                        B.act(eb, pS, AF.Exp)
                                        B.tt("dve", pb, eb, msk, ALU.mult)
                                        pbs[ci] = pb
                                        if pend_ep and ci >= 1:
                                            pend_ep.pop(0)()
                                    cj_ = ci - LAG
                                    if cj_ >= 0:
                                        c, Kg, Vg, msk = chunks[cj_]
                                        B.mm(pON, Vg[:, c, :], pbs[cj_], cj_ == 0, cj_ == n - 1)
                                        B.mm(pDEN, ones_bf, pbs[cj_], cj_ == 0, cj_ == n - 1)
                                for fn in pend_ep:
                                    fn()
                                pend_ep = branch_epilogue(hh, hh * 3 + br, False, pON, pDEN)
                                if br == 2:
                                    o_ = ob_[hh % 2]
                                    pend_ep.append(lambda o_=o_, hh=hh: B.copy("act", o_, oacc[:, hh, :].k(hh)))
                                    pend_ep.append(lambda o_=o_, hh=hh: B.dma("sp", T(OT.ap[4 * g + hh, :, u0:u0 + 512], "OT"), o_, wkey=("OT", g, hh, qt)))
                            if hh == 3:
                                for fn in pend_ep:
                                    fn()
                                pend_ep = []
            S.fence()

        if "F" in ph:
            with ExitStack() as st:
                dw = B.sb(st, "dw", [128, 8, 31], F32)
                B.dma("sp", dw, dwT)
                cv = B.sb(st, "cv", [128, 3, 8], F32)
                B.dma("sp", cv, cvec)
                pwb = B.sb(st, "pwb", [128, 16], F32)
                B.dma("sp", pwb, pwb_col)
                pw = B.sb(st, "pw", [128, 8, D], BF16)
                for cg in range(4):
                    B.dma("pool", pw[:, :, cg * 512:(cg + 1) * 512].k(cg), T(conv_pw_w.ap[:, cg * 512:(cg + 1) * 512].rearrange("(c p) n -> p c n", p=128), "pww"))
                HT = 1024
                Y = B.sb(st, "Y", [128, 8, HT], F32)
                ycT = B.sb(st, "ycT", [128, 8, HT], BF16)
                ysq = [B.sb(st, "ysq%d" % i, [128, 512], F32) for i in range(2)]
                mean = B.sb(st, "mean", [128, 512], F32)
                msq = B.sb(st, "msq", [128, 512], F32)
                rstd = B.sb(st, "rstdF", [128, 512], F32)
                zt = [B.sb(st, "zt%d" % i, [128, 512], F32) for i in range(2)]
                gm1 = [B.sb(st, "gm1_%d" % i, [128, 512], BF16) for i in range(2)]
                yo = [B.sb(st, "yo%d" % i, [128, 512], BF16) for i in range(2)]
                Dg = B.sb(st, "Dg", [128, 8, 31, 128], BF16)
                for cj in range(8):
                    for k in range(31):
                        B.ts("dve" if k % 2 == 0 else "pool", Dg[:, cj, k, :].k(cj, k), ident, dw[:, cj, k:k + 1], ALU.mult)
                Ub = [B.sb(st, "Ub%d" % i, [128, HT + 32], BF16) for i in range(2)]
                for th in range(2):
                    t0 = th * HT
                    for cj in range(8):
                        U = Ub[cj % 2]
                        B.dma("pool", U, T(UT.ap[cj * 128:(cj + 1) * 128, t0:t0 + HT + 32], "UT"))
                        for grp in range(2):
                            pt = ps[4 + (cj * 2 + grp) % 4]
                            for k in range(31):
                                o = 2 + k + grp * 512
                                B.mm(pt, Dg[:, cj, k, :].k(cj, k), U[:, o:o + 512], k == 0, k == 30)
                            B.act(Y[:, cj, grp * 512:(grp + 1) * 512].k(cj), pt, AF.Identity, bias=cv[:, 0, cj:cj + 1])
                    for grp in range(2):
                        cs = slice(grp * 512, (grp + 1) * 512)
                        p1, p2 = ps[0], ps[1]
                        for cj in range(8):
                            B.mm(p1, ones_f, Y[:, cj, cs].k(cj), cj == 0, cj == 7)
                        for cj in range(8):
                            q_ = ysq[cj % 2]
                            B.act(q_, Y[:, cj, cs].k(cj), AF.Square)
                            B.mm(p2, ones_f, q_, cj == 0, cj == 7)
                        B.ts("dve", mean, p1, 1.0 / 1024, ALU.mult)
                        B.tt("dve", msq, mean, mean, ALU.mult)
                        B.stt("dve", rstd, p2, 1.0 / 1024, msq, ALU.mult, ALU.subtract)
                        B.act(rstd, rstd, AF.Sqrt, bias=eps_t, scale=1.0)
                        B.recip(rstd, rstd)
                        for cj in range(8):
                            z_ = zt[cj % 2]
                            B.tt("dve", z_, Y[:, cj, cs].k(cj), mean, ALU.subtract)
                            B.tt("dve", z_, z_, rstd, ALU.mult)
                            B.act(ycT[:, cj, cs].k(cj, grp), z_, AF.Silu, bias=cv[:, 2, cj:cj + 1], scale=cv[:, 1, cj:cj + 1])
                        for j in range(16):
                            pt = ps[2 + j % 2]
                            for cj in range(8):
                                B.mm(pt, pw[:, cj, j * 128:(j + 1) * 128].k(j // 4), ycT[:, cj, cs].k(cj, grp), cj == 0, cj == 7)
                            g_ = gm1[j % 2]
                            c0 = t0 + grp * 512
                            B.dma("sp", g_, T(GM.ap[16 + j, :, c0:c0 + 512], "GM"))
                            o_ = yo[j % 2]
                            B.stt("dve", o_, pt, pwb[:, j:j + 1], g_, ALU.add, ALU.mult)
                            B.dma("sp", T(YCG.ap[j, :, c0:c0 + 512], "YCG"), o_, wkey=("YCG", j, th, grp))
            S.fence()

        if "G" in ph:
            with ExitStack() as st:
                g1b = B.sb(st, "g1b", [128, D], F32)
                B.dma("sp", g1b, T(modd.ap[0:1, 2 * D:3 * D].partition_broadcast(128), "modd"))
                HT = 1024
                oT = B.sb(st, "oT", [128, 16, HT], BF16)
                mixT = B.sb(st, "mixT", [128, 16, HT], BF16)
                wbs = [B.sb(st, "wg%d" % i, [128, KC, 512], BF16) for i in range(3)]
                gm0 = [B.sb(st, "gm0_%d" % i, [128, HT], BF16) for i in range(2)]
                ycg = [B.sb(st, "ycg_%d" % i, [128, HT], BF16) for i in range(2)]
                t1 = [B.sb(st, "t1_%d" % i, [128, 512], F32) for i in range(2)]
                xt_ = [B.sb(st, "xg_%d" % i, [128, 512], F32) for i in range(3)]
                xo_ = [B.sb(st, "xo_%d" % i, [128, 512], F32) for i in range(3)]
                wc = [0]
                for th in range(2):
                    t0 = th * HT
                    B.dma("sp", oT, T(OT.ap[:, :, t0:t0 + HT].rearrange("h d t -> d h t"), "OT"))
                    for cg in range(4):
                        w = wbs[wc[0] % 3]
                        wc[0] += 1
                        B.dma("pool", w, T(w_nsa_out.ap[:, cg * 512:(cg + 1) * 512].rearrange("(kc p) n -> p kc n", p=128), "wno"))
                        for m in range(4):
                            j = cg * 4 + m
                            g_, y_ = gm0[j % 2], ycg[j % 2]
                            B.dma("sp", g_, T(GM.ap[j, :, t0:t0 + HT], "GM"))
                            B.dma("sp", y_, T(YCG.ap[j, :, t0:t0 + HT], "YCG"))
                            for grp in range(2):
                                cs = slice(grp * 512, (grp + 1) * 512)
                                pt = ps[(j * 2 + grp) % 4]
                                for h in range(16):
                                    B.mm(pt, w[:, h, m * 128:(m + 1) * 128], oT[:, h, cs], h == 0, h == 15)
                                t_ = t1[grp]
                                B.tt("dve", t_, pt, g_[:, cs], ALU.mult)
                                B.tt("pool", mixT[:, j, cs].k(j, grp), t_, y_[:, cs], ALU.add)
                    for cg in range(4):
                        w = wbs[wc[0] % 3]
                        wc[0] += 1
                        B.dma("pool", w, T(w_out.ap[:, cg * 512:(cg + 1) * 512].rearrange("(kc p) n -> p kc n", p=128), "wout"))
                        for tt_ in range(8):
                            i = cg * 8 + tt_
                            pt = ps[4 + i % 3]
                            for kc in range(KC):
                                B.mm(pt, mixT[:, kc, tt_ * 128:(tt_ + 1) * 128].k(kc, tt_ // 4), w[:, kc, :], kc == 0, kc == KC - 1)
                            x_, o_ = xt_[i % 3], xo_[i % 3]
                            r0 = t0 + tt_ * 128
                            B.dma("sp", x_, T(xtok.ap[r0:r0 + 128, cg * 512:(cg + 1) * 512], "xtok"))
                            B.tt("dve", o_, pt, g1b[:, cg * 512:(cg + 1) * 512], ALU.mult)
                            B.tt("pool", o_, o_, x_, ALU.add)
                            B.dma("sp", T(X1.ap[r0:r0 + 128, cg * 512:(cg + 1) * 512], "X1"), o_, wkey=("X1", th, cg, tt_))
            S.fence()

        if "H" in ph:
            with ExitStack() as st:
                NT = 512
                ntile = NT // 128
                g2b = B.sb(st, "g2b", [128, D], F32)
                B.dma("sp", g2b, T(modd.ap[0:1, 5 * D:6 * D].partition_broadcast(128), "modd"))
                h2T = B.sb(st, "h2T", [128, KC, NT], BF16)
                acc = B.sb(st, "acc", [128, ntile, D], F32)
                tau = B.sb(st, "tau", [128, ntile, 8], F32)
                negb = B.sb(st, "negb", [128, ntile, 8], F32)
                rz = B.sb(st, "rz", [128, ntile, 8], F32)
                wbs = [B.sb(st, "wh%d" % i, [128, KC, 512], BF16) for i in range(2)]
                Vb = [B.sb(st, "Vb%d" % i, [128, 4, D], BF16) for i in range(2)]
                x1t = B.sb(st, "x1t", [128, D], F32)
                h2f = B.sb(st, "h2f", [128, D], F32)
                h2b = B.sb(st, "h2b", [128, D], BF16)
                ssq = B.sb(st, "ssq", [128, 1], F32)
                sall4 = B.sb(st, "sall4", [128, ntile, 16, 128], F32)
                gxb = [T(h2f.ap[:, 0:512], (h2f.key, "gx0")), T(h2f.ap[:, 512:1024], (h2f.key, "gx1"))]
                g1b_ = [T(h2f.ap[:, 1024:1536], (h2f.key, "g10")), T(h2f.ap[:, 1536:2048], (h2f.key, "g11"))]
                wcnt = [0]

                def vmax(o, i):
                    S.add("dve", lambda e, o=o.ap, i=i.ap: e.max(out=o, in_=i), reads=[i.key], writes=[o.key])

                def vmr(o, r, i):
                    S.add("dve", lambda e, o=o.ap, r=r.ap, i=i.ap: e.match_replace(out=o, in_to_replace=r, in_values=i, imm_value=-3.0e38),
                          reads=[r.key, i.key], writes=[o.key])

                for tg in range(CFG.get('h_tg', NTOK // NT)):
                    for tt_ in range(ntile):
                        for dc in range(4):
                            B.memset("pool", T(acc.ap[:, tt_, dc * 512:(dc + 1) * 512], (acc.key, tt_, dc)), 0.0)
                    with ExitStack() as s1:
                        keysT = B.sb(s1, "keysT", [128, 16, 128], BF16)
                        B.dma("pool", keysT, keysT_in)
                        qT = B.sb(s1, "qT", [128, 16, NT], BF16)
                        s2 = B.sb(s1, "s2", [128, 128], F32)
                        tv = B.sb(s1, "tv", [128, 16, 16], F32)
                        cand = B.sb(s1, "cand", [128, 256], F32)
                        cand2 = B.sb(s1, "cand2", [128, 256], F32)
                        ce = B.sb(s1, "ce", [128, 256], F32)
                        m3 = B.sb(s1, "m3", [128, 3, 8], F32)
                        ntau = B.sb(s1, "ntau", [128, 1], F32)
                        zz = B.sb(s1, "zz", [128, 8], F32)
                        for tt_ in range(ntile):
                            r0 = tg * NT + tt_ * 128
                            B.dma("sp", x1t, T(X1.ap[r0:r0 + 128, :], "X1"))
                            B.memset("dve", ssq, 0.0)
                            B.act(h2b, x1t, AF.Square, accum=ssq)
                            B.act(ssq, ssq, AF.Sqrt, bias=eps_t, scale=1.0 / D)
                            B.recip(ssq, ssq)
                            B.ts("dve", h2b, x1t, ssq[:, 0:1], ALU.mult)
                            for half in range(2):
                                pT = pst if half == 0 else pst6
                                for k8 in range(8):
                                    kc = half * 8 + k8
                                    B.transpose(pT[:, k8 * 128:(k8 + 1) * 128].k("h", k8), h2b[:, kc * 128:(kc + 1) * 128], ident)
                                for k8 in range(8):
                                    kc = half * 8 + k8
                                    B.act(T(h2T.ap[:, kc, tt_ * 128:(tt_ + 1) * 128], (h2T.key, kc, tt_)),
                                          pT[:, k8 * 128:(k8 + 1) * 128].k("h", k8), AF.Identity,
                                          bias=modc[:, 48 + kc:49 + kc], scale=A2[:, kc:kc + 1],
                                          reads=[(pT.key, "h", k_) for k_ in range(8)])
                        for cg in range(4):
                            w = wbs[wcnt[0] % 2]
                            wcnt[0] += 1
                            B.dma("pool", w, T(peer_w_q.ap[:, cg * 512:(cg + 1) * 512].rearrange("(kc p) n -> p kc n", p=128), "pwq"))
                            for m in range(4):
                                j = cg * 4 + m
                                pt = ps[j % 2]
                                for kc in range(KC):
                                    B.mm(pt, w[:, kc, m * 128:(m + 1) * 128], T(h2T.ap[:, kc, :], (h2T.key, kc, 0)), kc == 0, kc == KC - 1,
                                         reads=[(h2T.key, kc, t_) for t_ in range(1, ntile)])
                                B.copy("act", qT[:, j, :].k(j), pt)
                        for tt_ in range(ntile):
                            cs = slice(tt_ * 128, (tt_ + 1) * 128)
                            for q4_ in range(4):
                                pt = ps[2 + q4_ % 2]
                                for i in range(4):
                                    hp = q4_ * 4 + i
                                    B.mm(pt[:, i * 128:(i + 1) * 128].k(i), qT[:, hp, cs].k(hp), keysT[:, hp, :], True, True)
                                B.copy("act", T(sall4.ap[:, tt_, q4_ * 4:q4_ * 4 + 4, :], (sall4.key, tt_, q4_)),
                                       T(pt.ap.rearrange("p (a k) -> p a k", a=4), pt.key), reads=[(pt.key, i) for i in range(4)])
                            for hp in range(16):
                                sv = T(sall4.ap[:, tt_, hp, :], (sall4.key, tt_, hp // 4))
                                vmax(tv[:, hp, 0:8].k(hp), sv)
                                vmr(s2, tv[:, hp, 0:8].k(hp), sv)
                                vmax(tv[:, hp, 8:16].k(hp), s2)
                            for h in range(8):
                                a_ = T(tv.ap[:, 2 * h, :].unsqueeze(2).to_broadcast([128, 16, 16]), (tv.key, 2 * h))
                                b_ = T(tv.ap[:, 2 * h + 1, :].unsqueeze(1).to_broadcast([128, 16, 16]), (tv.key, 2 * h + 1))
                                B.tt("dve", T(cand.ap.rearrange("p (a b) -> p a b", a=16), cand.key), a_, b_, ALU.add)
                                vmax(m3[:, 0, :], cand)
                                vmr(cand2, m3[:, 0, :], cand)
                                vmax(m3[:, 1, :], cand2)
                                vmr(cand2, m3[:, 1, :], cand2)
                                vmax(m3[:, 2, :], cand2)
                                tau_ = tau[:, tt_, h:h + 1].k(tt_, h)
                                B.stt("dve", tau_, m3[:, 1, 7:8], 0.5, m3[:, 2, 0:1], ALU.mult, ALU.add)
                                B.stt("dve", tau_, m3[:, 2, 0:1], -0.5, tau_, ALU.mult, ALU.add)
                                B.ts("dve", ntau, tau_, -1.0, ALU.mult)
                                B.act(ce, cand, AF.Exp, bias=ntau)
                                B.stt("dve", ce, cand, tau_, ce, ALU.is_ge, ALU.mult)
                                S.add("dve", lambda e, o=zz.ap[:, h:h + 1], i=ce.ap: e.reduce_sum(out=o, in_=i, axis=AX.X), reads=[ce.key], writes=[zz.key])
                            B.recip(T(rz.ap[:, tt_, :], (rz.key, tt_)), zz)
                            B.act(zz, zz, AF.Ln)
                            B.tt("dve", zz, zz, T(tau.ap[:, tt_, :], tau.key), ALU.add)
                            S.ops[-1].deps |= {S.last_w[(tau.key, tt_, h)] for h in range(8)}
                            B.ts("dve", T(negb.ap[:, tt_, :], (negb.key, tt_)), zz, -1.0, ALU.mult)
                    S.fence()
                    with ExitStack() as s5:
                        Gb = [B.sb(s5, "Gb%d" % i, [128, 4, NT], BF16) for i in range(2)]
                        Eb = [B.sb(s5, "EbH%d" % i, [128, 512], F32) for i in range(4)]
                        Wh = [B.sb(s5, "Wh%d" % i, [128, 512], BF16) for i in range(4)]
                        cf = [B.sb(s5, "cf%d" % i, [128, 4, 128], BF16) for i in range(3)]
                        b4s = [B.sb(s5, "b4_%d" % i, [128, 8, 4], F32) for i in range(2)]
                        negs = CFG.get('h_eg', 32)
                        Us, Vs = {}, {}

                        def load_U(eg):
                            Us[eg] = wbs[eg % 2]
                            B.dma("pool", Us[eg], T(peer_uT.ap[:, eg * 512:(eg + 1) * 512].rearrange("(kc p) n -> p kc n", p=128), "puT"))

                        def load_V(eg):
                            Vs[eg] = Vb[eg % 2]
                            B.dma("pool", Vs[eg], T(peer_v.ap[eg * 512:(eg + 1) * 512, :].rearrange("(s p) d -> p s d", p=128), "pv"))

                        def emit_A(eg, sub, kcs):
                            pA = ps[4 + sub % 2]
                            for kc in kcs:
                                B.mm(pA, Us[eg][:, kc, sub * 128:(sub + 1) * 128], T(h2T.ap[:, kc, :], (h2T.key, kc, 0)), kc == 0, kc == KC - 1)

                        def emit_gelu1(sub):
                            pA = ps[4 + sub % 2]
                            gxs = gxb[sub % 2]
                            g1s = g1b_[sub % 2]
                            B.act(gxs, pA, AF.Identity, scale=0.5)
                            B.tt("pool", g1s, gxs, gxs, ALU.mult)
                            B.ts("pool", g1s, g1s, 0.17886, ALU.mult, 1.0, ALU.add)
                            B.tt("pool", g1s, g1s, gxs, ALU.mult)

                        def emit_gelu2(eg, sub):
                            gxs = gxb[sub % 2]
                            g1s = g1b_[sub % 2]
                            B.act(g1s, g1s, AF.Tanh, scale=1.5957691216)
                            B.stt("dve", Gb[eg % 2][:, sub, :].k(sub), g1s, 1.0, gxs, ALU.add, ALU.mult)

                        def emit_coef(unit):
                            eg_, tt2, PSWT_, cf2 = unit
                            B.tt("dve", cf2, T(Gb[eg_ % 2].ap[:, :, tt2 * 128:(tt2 + 1) * 128], (Gb[eg_ % 2].key, 0)),
                                 T(PSWT_.ap.rearrange("p (s t) -> p s t", s=4), PSWT_.key), ALU.mult)
                            S.ops[-1].deps |= {S.last_w[k] for k in [(Gb[eg_ % 2].key, sb_) for sb_ in range(4)] if k in S.last_w}

                        def emit_b4t4(eg_, tt2, slot):
                            s1v = T(sall4.ap[:, tt2].rearrange("q (h p) k -> q h p k", p=2)[:, :, 0, eg_ * 4:eg_ * 4 + 4], sall4.key)
                            B.tt("dve", b4s[slot], s1v, T(negb.ap[:, tt2, :].unsqueeze(2).to_broadcast([128, 8, 4]), negb.key), ALU.add)

                        load_U(0)
                        load_V(0)
                        if negs > 1:
                            load_U(1)
                        for sub in range(4):
                            emit_A(0, sub, range(KC))
                            emit_gelu1(sub)
                            emit_gelu2(0, sub)
                        units = [(eg, tt_) for eg in range(negs) for tt_ in range(ntile)]
                        emit_b4t4(0, 0, 0)
                        done = []
                        pend_add = []
                        pend_g2 = None
                        ectr = 0
                        for ui, (eg, tt_) in enumerate(units):
                            if tt_ == 0 and eg + 2 < negs:
                                load_U(eg + 2)
                            if tt_ == 2 and eg + 1 < negs:
                                load_V(eg + 1)
                            PSWT = ps[2 + ui % 2]
                            cf_ = cf[ui % 3]
                            b4 = b4s[ui % 2]
                            if ui + 1 < len(units):
                                emit_b4t4(units[ui + 1][0], units[ui + 1][1], (ui + 1) % 2)
                            vsrc = done[ui - 2] if ui >= 2 else None
                            for h in range(8):
                                e_, w_ = Eb[ectr % 4], Wh[ectr % 4]
                                ectr += 1
                                s2v = T(sall4.ap[:, tt_, 2 * h + 1, :], sall4.key)
                                for j in range(4):
                                    B.act(e_[:, j * 128:(j + 1) * 128].k(j), s2v, AF.Exp, bias=b4[:, h, j:j + 1])
                                S.add("dve", lambda e, o=w_.ap, a=e_.ap, sc_=rz.ap[:, tt_, h:h + 1]:
                                      e.scalar_tensor_tensor(out=o, in0=a, scalar=sc_, in1=a, op0=ALU.is_ge, op1=ALU.mult),
                                      reads=[(e_.key, j) for j in range(4)] + [(rz.key, tt_)], writes=[w_.key])
                                for fn in pend_add:
                                    fn()
                                pend_add = []
                                if h == 1 and ui >= 1:
                                    emit_coef(done[ui - 1])
                                if h == 2 and pend_g2 is not None:
                                    emit_gelu2(*pend_g2)
                                    pend_g2 = None
                                if eg + 1 < negs:
                                    emit_A(eg + 1, tt_, [2 * h, 2 * h + 1])
                                if vsrc is not None:
                                    eg2, tt2, _, cf2 = vsrc
                                    dc = h // 2
                                    po = ps[6 + dc % 2]
                                    for sub in (2 * (h % 2), 2 * (h % 2) + 1):
                                        B.mm(po, cf2[:, sub, :], Vs[eg2][:, sub, dc * 512:(dc + 1) * 512], sub == 0, sub == 3)
                                    if h % 2 == 1:
                                        a_ = T(acc.ap[:, tt2, dc * 512:(dc + 1) * 512], (acc.key, tt2, dc))
                                        pend_add.append(lambda a_=a_, po=po: B.tt("dve", a_, a_, po, ALU.add))
                                if h % 2 == 0:
                                    w_prev = w_
                                else:
                                    B.tt("pool", w_, w_prev, w_, ALU.add)
                                    for sub in range(4):
                                        B.mm(PSWT[:, sub * 128:(sub + 1) * 128], w_[:, sub * 128:(sub + 1) * 128], ident, h == 1 and sub == 0, h == 7, sgc=True)
                            if eg + 1 < negs:
                                emit_gelu1(tt_)
                                pend_g2 = (eg + 1, tt_)
                            done.append((eg, tt_, PSWT, cf_))
                        for fn in pend_add:
                            fn()
                        emit_coef(done[-1])
                        for vsrc in done[-2:]:
                            eg2, tt2, _, cf2 = vsrc
                            for dc in range(4):
                                po = ps[6 + dc % 2]
                                for sub in range(4):
                                    B.mm(po, cf2[:, sub, :], Vs[eg2][:, sub, dc * 512:(dc + 1) * 512], sub == 0, sub == 3)
                                a_ = T(acc.ap[:, tt2, dc * 512:(dc + 1) * 512], (acc.key, tt2, dc))
                                B.tt("dve", a_, a_, po, ALU.add)
                    S.fence()
                    for tt_ in range(ntile):
                        r0 = tg * NT + tt_ * 128
                        B.dma("sp", x1t, T(X1.ap[r0:r0 + 128, :], "X1"))
                        B.tt("dve", h2f, T(acc.ap[:, tt_, :], acc.key), g2b, ALU.mult)
                        S.ops[-1].deps |= {S.last_w[k] for k in [(acc.key, tt_, dc) for dc in range(4)] if k in S.last_w}
                        B.tt("pool", h2f, h2f, x1t, ALU.add)
                        final_ops.append(B.dma("sp", T(out.ap[r0:r0 + 128, :], "out"), h2f, wkey=("out", r0)))
                    S.fence()
            S.fence()

        if "Z" in ph:
            with ExitStack() as st:
                t_ = B.sb(st, "zz", [128, D], F32)
                for i in range(16):
                    B.dma("sp", t_, T(xtok.ap[i * 128:(i + 1) * 128, :], "xtok"))
                    final_ops.append(B.dma("sp", T(out.ap[i * 128:(i + 1) * 128, :], "out"), t_, wkey=("out", i)))
    stats = S.emit(final_ops)
    return nc, stats


def host_inputs(inputs):
    x = np.asarray(inputs["x"], np.float32)
    c = np.asarray(inputs["c"], np.float32)
    g = lambda k: np.asarray(inputs[k], np.float32)[0]
    w_in, b_in = g("w_in"), g("b_in")
    shared = {
        "w_ada": np.ascontiguousarray(g("w_ada")),
        "b_ada": g("b_ada").reshape(1, -1),
        "w_in": np.ascontiguousarray(w_in),
        "b_in": b_in.reshape(1, -1),
        "g1_col": np.ascontiguousarray(g("norm1_g").reshape(KC, 128).T),
        "g2_col": np.ascontiguousarray(g("norm2_g").reshape(KC, 128).T),
    }
    bc = np.zeros((128, 96), np.float32)
    def put(col0, off, n):
        for i in range(n):
            bc[:, col0 + i] = b_in[off + i * 128: off + (i + 1) * 128]
    put(0, OQ, 16); put(16, OKC, 4); put(20, OVC, 4); put(24, OKS, 4); put(28, OKW, 4)
    bc[0:48, 32] = b_in[OGN:OGN + 48]
    put(48, OGLU, 8); put(56, OGLU + 1024, 8); put(64, OGM, 32)
    shared["b_in_col"] = bc
    kg = g("k_norm_g")
    shared["qkg_col"] = np.ascontiguousarray(np.stack([g("q_norm_g"), kg[0], kg[1], kg[2]], axis=1))
    shared["cmp_k_w1"] = g("cmp_k_w1"); shared["cmp_v_w1"] = g("cmp_v_w1")
    shared["cmp_k_w2"] = g("cmp_k_w2"); shared["cmp_v_w2"] = g("cmp_v_w2")
    shared["cmp_posT_k"] = np.ascontiguousarray(g("cmp_pos_k").T); shared["cmp_posT_v"] = np.ascontiguousarray(g("cmp_pos_v").T)
    shared["w_nsa_out"] = g("w_nsa_out"); shared["conv_pw_w"] = g("conv_pw_w"); shared["w_out"] = g("w_out")
    shared["peer_w_q"] = g("peer_w_q")
    shared["dwT"] = np.ascontiguousarray(g("conv_dw_w").T.reshape(8, 128, 31).transpose(1, 0, 2))
    shared["cvec"] = np.ascontiguousarray(np.stack([g("conv_dw_b"), g("conv_ln_g"), g("conv_ln_b")], 0).reshape(3, 8, 128).transpose(2, 0, 1))
    shared["pwb_col"] = np.ascontiguousarray(g("conv_pw_b").reshape(16, 128).T)
    shared["keysT"] = np.ascontiguousarray(g("peer_sub_keys").reshape(16, 128, 128).transpose(2, 0, 1))
    if "H" in CFG["phases"]:
        shared["peer_uT"] = np.ascontiguousarray(g("peer_u").T)
        shared["peer_v"] = g("peer_v")
    else:
        shared["peer_uT"] = np.zeros((128, 128), np.float32)
        shared["peer_v"] = np.zeros((128, 128), np.float32)
    shared.update(_shared_consts())
    maps = []
    for core in range(8):
        b, hf = core // 2, core % 2
        xb = x[b]
        if hf == 1:
            seg = xb
        else:
            seg = np.concatenate([np.zeros((NPRE, D), np.float32), xb[:NTOK]], axis=0)
        m = dict(shared)
        m["xT"] = np.ascontiguousarray(seg.T.reshape(KC, 128, NPRE + NTOK).transpose(1, 0, 2))
        m["xtok"] = np.ascontiguousarray(xb[hf * NTOK:(hf + 1) * NTOK])
        m["c_col"] = np.ascontiguousarray(c[b].reshape(KC, 128).T)
        m["pvalid"] = np.full((128, 1), float(hf), np.float32)
        m.update(_core_consts(hf))
        maps.append(m)
    return maps


def _shared_consts():
    c = {}
    c["ident"] = np.eye(128, dtype=np.float32)
    i = np.arange(256)[:, None]; j = np.arange(64)[None, :]
    ov = ((16 * i < 64 * j + 64) & (16 * i + 32 > 64 * j) & (i < 255)).astype(np.float32)
    c["OVL"] = np.ascontiguousarray(ov.reshape(2, 128, 64).transpose(1, 0, 2))
    p = np.arange(128)[:, None, None]; di = np.arange(4)[None, :, None]; u = np.arange(512)[None, None, :]
    c["SELC"] = (128 * di + p <= u).astype(np.float32)
    jj = np.arange(64)[:, None, None]; cc = np.arange(32)[None, :, None]; pp = np.arange(128)[None, None, :]
    c["EX"] = (jj == 2 * cc + pp // 64).astype(np.float32)
    return c


def _core_consts(hf):
    c = {}
    u = np.arange(NTOK)
    col = NPRE + u
    cur = col // 64
    j = np.arange(64)[None, :]
    glob_j = j - 32 * (1 - hf)
    glob_cur = (cur - 32 * (1 - hf))[:, None]
    forced = (glob_j == 0) | (glob_j == glob_cur) | (glob_j == glob_cur - 1)
    valid = (glob_j >= 0) & (glob_j <= glob_cur)
    fval = np.where(glob_j == 0, 3e30, np.where(glob_j == glob_cur, 2e30, 1e30))
    selb = np.where(valid, np.where(forced, fval, 0.0), -1e30).astype(np.float32)
    c["SELB"] = selb
    i = np.arange(256)[:, None]
    vis = (16 * i + 31 <= col[None, :]) & (i < 255) & ((i >= 128) | (hf == 1))
    c["CMPM"] = np.ascontiguousarray(vis.astype(np.float32).reshape(2, 128, NTOK).transpose(1, 0, 2))
    wm = np.zeros((4, 128, 8, 512), np.float32)
    p = np.arange(128)[:, None]; uu = np.arange(512)[None, :]
    for qt in range(4):
        q0 = NPRE + qt * 512
        for k in range(8):
            k0 = q0 - 512 + 128 * k
            diff = (q0 + uu) - (k0 + p)
            ok = (diff >= 0) & (diff < 512) & ((k0 + p >= NPRE) | (hf == 1))
            wm[qt, :, k, :] = ok
    c["WINM"] = wm
    return c


_CACHE = {}


def kernel(**inputs):
    maps = host_inputs(inputs)
    if "nc" not in _CACHE:
        _CACHE["nc"] = build_program()
    nc, stats = _CACHE["nc"]
    res = run_bass_kernel_spmd(nc, maps, core_ids=list(range(8)))
    _CACHE["res"] = res
    outp = np.zeros((4, 4096, D), np.float32)
    for core in range(8):
        b, hf = core // 2, core % 2
        outp[b, hf * NTOK:(hf + 1) * NTOK] = res.results[core]["out"]
    return outp
```

```python
import numpy as np
import ml_dtypes
from contextlib import ExitStack
import concourse.bass as bass
import concourse.mybir as mybir
from concourse.bass_utils import run_bass_kernel_spmd

F32 = mybir.dt.float32
BF16 = mybir.dt.bfloat16
AF = mybir.ActivationFunctionType
ALU = mybir.AluOpType
AX = mybir.AxisListType

ENGS = ("pe", "act", "dve", "pool", "sp")
NDSEM = 24


class Op:
    __slots__ = ("eng", "fn", "deps", "is_dma", "sig", "idx", "dma_n")


class Sched:
    def __init__(self, nc):
        self.nc = nc
        self.ops = []
        self.last_w = {}
        self.rd_eng = {}
        self.rd_dma = {}
        self.n_dma = {"hw": 0, "sw": 0}
        self.fence_deps = set()
        self.last_on_eng = {}
        self.dma_since_fence = []

    def add(self, eng, fn, reads=(), writes=(), dma=False):
        op = Op()
        op.eng, op.fn, op.is_dma, op.sig, op.dma_n = eng, fn, dma, None, -1
        op.idx = len(self.ops)
        deps = set(self.fence_deps)
        for r in reads:
            w = self.last_w.get(r)
            if w is not None:
                deps.add(w)
        for k in writes:
            w = self.last_w.get(k)
            if w is not None:
                deps.add(w)
            for rd in self.rd_eng.get(k, {}).values():
                deps.add(rd)
            for rd in self.rd_dma.get(k, ()):
                deps.add(rd)
        op.deps = deps
        for r in reads:
            if dma:
                self.rd_dma.setdefault(r, []).append(op.idx)
            else:
                self.rd_eng.setdefault(r, {})[eng] = op.idx
        for k in writes:
            self.last_w[k] = op.idx
            self.rd_eng[k] = {}
            self.rd_dma[k] = []
        if dma:
            cls = "sw" if eng == "pool" else "hw"
            op.dma_n = (cls, self.n_dma[cls])
            self.n_dma[cls] += 1
            self.dma_since_fence.append(op.idx)
        else:
            self.last_on_eng[eng] = op.idx
        self.ops.append(op)
        return op

    def fence(self):
        d = set(self.last_on_eng.values()) | set(self.dma_since_fence)
        self.fence_deps = d
        self.dma_since_fence = []
        self.last_w.clear()
        self.rd_eng.clear()
        self.rd_dma.clear()

    def emit(self, final_ops=()):
        nc, ops = self.nc, self.ops
        needed = [False] * len(ops)
        for op in ops:
            for d in op.deps:
                needed[d] = True
        for op in final_ops:
            needed[op.idx] = True
        esem = {e: nc.alloc_semaphore(name="se_" + e) for e in ENGS}
        dsem = {("hw", i): nc.alloc_semaphore(name="sdh_%d" % i) for i in range(NDSEM)}
        dsem.update({("sw", i): nc.alloc_semaphore(name="sds_%d" % i) for i in range(NDSEM)})
        cnt = {e: 0 for e in ENGS}
        for op in ops:
            if op.is_dma:
                op.sig = (dsem[(op.dma_n[0], op.dma_n[1] % NDSEM)], 16 * (op.dma_n[1] // NDSEM + 1))
            elif needed[op.idx]:
                cnt[op.eng] += 1
                op.sig = (esem[op.eng], cnt[op.eng])
        per_eng = {e: [op for op in ops if op.eng == e] for e in ENGS}
        nwait = {e: 0 for e in ENGS}

        def run_engine(ename, eng):
            known = {e: 0 for e in ENGS}
            known_d = {}
            for op in per_eng[ename]:
                waits = {}
                for d in op.deps:
                    p = ops[d]
                    if p.is_dma:
                        k, v = (p.dma_n[0], p.dma_n[1] % NDSEM), p.sig[1]
                        if known_d.get(k, 0) < v:
                            waits[("d", k)] = max(waits.get(("d", k), 0), v)
                    else:
                        if p.eng == ename and ename == "pe":
                            continue
                        v = p.sig[1]
                        if known[p.eng] < v:
                            waits[("e", p.eng)] = max(waits.get(("e", p.eng), 0), v)
                if op.is_dma and op.dma_n[1] >= NDSEM:
                    k, v = (op.dma_n[0], op.dma_n[1] % NDSEM), 16 * (op.dma_n[1] // NDSEM)
                    if known_d.get(k, 0) < v:
                        waits[("d", k)] = max(waits.get(("d", k), 0), v)
                for (kind, k), v in waits.items():
                    if kind == "d":
                        eng.wait_ge(dsem[k], v)
                        known_d[k] = v
                    else:
                        eng.wait_ge(esem[k], v)
                        known[k] = v
                    nwait[ename] += 1
                ins = op.fn(eng)
                if op.sig is not None:
                    ins.then_inc(op.sig[0], 16 if op.is_dma else 1)
            if ename == "sp":
                for op in final_ops:
                    eng.wait_ge(op.sig[0], op.sig[1])

        with nc.Block() as block:
            @block.tensor
            def _(e):
                run_engine("pe", e)

            @block.scalar
            def _(e):
                run_engine("act", e)

            @block.vector
            def _(e):
                run_engine("dve", e)

            @block.gpsimd
            def _(e):
                run_engine("pool", e)

            @block.sync
            def _(e):
                run_engine("sp", e)
        return {e: (len(per_eng[e]), nwait[e]) for e in ENGS}


class T:
    __slots__ = ("ap", "key")

    def __init__(self, ap, key):
        self.ap, self.key = ap, key

    def __getitem__(self, idx):
        return T(self.ap[idx], self.key)

    def k(self, *sub):
        return T(self.ap, (self.key,) + sub)


D = 2048
NTOK = 2048
NPRE = 2048
KC = 16
EPS = 1e-6
OQ, OKC, OVC, OKS, OVS, OKW, OVW, OGN, OGLU, OGM = 0, 2048, 2560, 3072, 3584, 4096, 4608, 5120, 5168, 7216
INC = 11312

CFG = {"phases": "ABCDFGH", "debug": ()}


class Builder:
    def __init__(self):
        self.nc = bass.Bass("TRN2", target_bir_lowering=False)
        self.S = Sched(self.nc)
        self.dram = {}
        self.uid = 0
        self.ps = None

    def din(self, name, shape, dt=F32):
        t = T(self.nc.dram_tensor(name, list(shape), dt, kind="ExternalInput").ap(), name)
        self.dram[name] = t
        return t

    def dscr(self, name, shape, dt, out=False):
        kind = "ExternalOutput" if (out or name in CFG["debug"]) else "Internal"
        t = T(self.nc.dram_tensor(name, list(shape), dt, kind=kind).ap(), name)
        self.dram[name] = t
        return t

    def sb(self, st, name, shape, dt):
        self.uid += 1
        h = st.enter_context(self.nc.sbuf_tensor("%s_%d" % (name, self.uid), list(shape), dt))
        return T(h.ap(), "%s_%d" % (name, self.uid))

    def psum_banks(self):
        self.ps = [T(self.nc.alloc_psum_tensor("psb%d" % i, [128, 512], F32).ap(), "ps%d" % i) for i in range(8)]

    def dma(self, q, out, in_, wkey=None, reads=None):
        self.uid += 1
        wk = out.key if wkey is None else wkey
        return self.S.add(q, lambda e, o=out.ap, i=in_.ap: e.dma_start(out=o, in_=i),
                          reads=[in_.key] if reads is None else reads, writes=[wk], dma=True)

    def mm(self, out, lhsT, rhs, start, stop, sgc=False, reads=()):
        return self.S.add("pe", lambda e, o=out.ap, l=lhsT.ap, r=rhs.ap, s=start, p=stop, g=sgc:
                          e.matmul(o, lhsT=l, rhs=r, start=s, stop=p, skip_group_check=g),
                          reads=[lhsT.key, rhs.key] + list(reads), writes=[out.key])

    def transpose(self, out, in_, ident):
        return self.S.add("pe", lambda e, o=out.ap, i=in_.ap, d=ident.ap: e.transpose(o, i, d),
                          reads=[in_.key, ident.key], writes=[out.key])

    def act(self, out, in_, func, bias=None, scale=1.0, accum=None, reads=()):
        rd = [in_.key] + list(reads)
        kw = {}
        if isinstance(bias, T):
            rd.append(bias.key)
            kw["bias"] = bias.ap
        elif bias is not None:
            kw["bias"] = bias
        if isinstance(scale, T):
            rd.append(scale.key)
            kw["scale"] = scale.ap
        else:
            kw["scale"] = scale
        wr = [out.key]
        if accum is not None:
            kw["accum_out"] = accum.ap
            wr.append(accum.key)
        return self.S.add("act", lambda e, o=out.ap, i=in_.ap, f=func, k=kw: e.activation(out=o, in_=i, func=f, **k),
                          reads=rd, writes=wr)

    def tt(self, eng, out, in0, in1, op, reads=()):
        return self.S.add(eng, lambda e, o=out.ap, a=in0.ap, b=in1.ap, p=op: e.tensor_tensor(out=o, in0=a, in1=b, op=p),
                          reads=[in0.key, in1.key] + list(reads), writes=[out.key])

    def ts(self, eng, out, in0, s1, op0, s2=None, op1=None):
        rd = [in0.key]
        a1 = s1
        if isinstance(s1, T):
            rd.append(s1.key)
            a1 = s1.ap
        a2 = s2
        if isinstance(s2, T):
            rd.append(s2.key)
            a2 = s2.ap
        kw = {} if op1 is None else {"op1": op1}
        return self.S.add(eng, lambda e, o=out.ap, a=in0.ap, x=a1, y=a2, p=op0, k=kw:
                          e.tensor_scalar(out=o, in0=a, scalar1=x, scalar2=y, op0=p, **k),
                          reads=rd, writes=[out.key])

    def stt(self, eng, out, in0, scalar, in1, op0, op1):
        rd = [in0.key, in1.key]
        sc = scalar
        if isinstance(scalar, T):
            rd.append(scalar.key)
            sc = scalar.ap
        return self.S.add(eng, lambda e, o=out.ap, a=in0.ap, s=sc, b=in1.ap, p=op0, q=op1:
                          e.scalar_tensor_tensor(out=o, in0=a, scalar=s, in1=b, op0=p, op1=q),
                          reads=rd, writes=[out.key])

    def copy(self, eng, out, in_, reads=None):
        rd = [in_.key] if reads is None else reads
        if eng == "act":
            return self.S.add("act", lambda e, o=out.ap, i=in_.ap: e.copy(out=o, in_=i), reads=rd, writes=[out.key])
        return self.S.add(eng, lambda e, o=out.ap, i=in_.ap: e.tensor_copy(out=o, in_=i), reads=rd, writes=[out.key])

    def recip(self, out, in_):
        return self.S.add("dve", lambda e, o=out.ap, i=in_.ap: e.reciprocal(out=o, in_=i), reads=[in_.key], writes=[out.key])

    def memset(self, eng, out, val):
        return self.S.add(eng, lambda e, o=out.ap, v=val: e.memset(o, v), writes=[out.key])

    def gelu_tanh(self, out, xs, t1, t2):
        self.tt("pool", t1, xs, xs, ALU.mult)
        self.ts("pool", t1, t1, 0.044715, ALU.mult, 1.0, ALU.add)
        self.tt("pool", t1, t1, xs, ALU.mult)
        self.act(t2, t1, AF.Exp, scale=-1.5957691216)
        self.ts("dve", t2, t2, 1.0, ALU.add)
        self.recip(t2, t2)
        self.tt("pool", out, xs, t2, ALU.mult)

    def rstd_from_psum(self, st_tile, ps, n_feat, width):
        self.act(st_tile, ps, AF.Sqrt, bias=self.eps_t, scale=1.0 / n_feat)
        self.recip(st_tile, st_tile)


def build_program():
    B = Builder()
    nc, S = B.nc, B.S
    ph = CFG["phases"]
    xT = B.din("xT", [128, KC, NPRE + NTOK])
    xtok = B.din("xtok", [NTOK, D])
    c_col = B.din("c_col", [128, KC])
    g1_col = B.din("g1_col", [128, KC])
    g2_col = B.din("g2_col", [128, KC])
    w_ada = B.din("w_ada", [D, 6 * D])
    b_ada = B.din("b_ada", [1, 6 * D])
    w_in = B.din("w_in", [D, INC])
    b_in = B.din("b_in", [1, INC])
    b_in_col = B.din("b_in_col", [128, 96])
    qkg_col = B.din("qkg_col", [128, 4])
    pvalid = B.din("pvalid", [128, 1])
    cmp_w1 = [B.din("cmp_k_w1", [4096, 128]), B.din("cmp_v_w1", [4096, 128])]
    cmp_w2 = [B.din("cmp_k_w2", [128, 128]), B.din("cmp_v_w2", [128, 128])]
    cmp_posT = [B.din("cmp_posT_k", [128, 32]), B.din("cmp_posT_v", [128, 32])]
    ident_in = B.din("ident", [128, 128])
    OVL_in = B.din("OVL", [128, 2, 64])
    SELB_in = B.din("SELB", [NTOK, 64])
    CMPM_in = B.din("CMPM", [128, 2, NTOK])
    WINM_in = B.din("WINM", [4, 128, 8, 512])
    SELC_in = B.din("SELC", [128, 4, 512])
    EX_in = B.din("EX", [64, 32, 128])
    w_nsa_out = B.din("w_nsa_out", [D, D])
    dwT = B.din("dwT", [128, 8, 31])
    cvec = B.din("cvec", [128, 3, 8])
    pwb_col = B.din("pwb_col", [128, 16])
    conv_pw_w = B.din("conv_pw_w", [1024, D])
    w_out = B.din("w_out", [D, D])
    peer_w_q = B.din("peer_w_q", [D, D])
    keysT_in = B.din("keysT", [128, 16, 128])
    peer_uT = B.din("peer_uT", [D, 16384] if "H" in ph else [128, 128])
    peer_v = B.din("peer_v", [16384, D] if "H" in ph else [128, 128])
    out = B.dscr("out", [NTOK, D], F32, out=True)
    modd = B.dscr("modd", [1, 6 * D], F32)
    QT = B.dscr("QT", [16, 128, NTOK], BF16)
    KCT = B.dscr("KCT", [4, 128, NPRE + NTOK], BF16)
    VCT = B.dscr("VCT", [4, 128, NPRE + NTOK], BF16)
    KST = B.dscr("KST", [4, 128, NPRE + NTOK], BF16)
    KWT = B.dscr("KWT", [4, 128, NPRE + NTOK], BF16)
    VS = B.dscr("VS", [NPRE + NTOK, 512], BF16)
    VW = B.dscr("VW", [NPRE + NTOK, 512], BF16)
    GNT = B.dscr("GNT", [48, NTOK], F32)
    UT = B.dscr("UT", [1024, 32 + NTOK], F32)
    GM = B.dscr("GM", [32, 128, NTOK], BF16)
    OT = B.dscr("OT", [16, 128, NTOK], BF16)
    YCG = B.dscr("YCG", [16, 128, NTOK], BF16)
    X1 = B.dscr("X1", [NTOK, D], F32)
    B.psum_banks()
    ps = B.ps
    pst = T(ps[7].ap.bitcast(BF16), "ps7")
    pst6 = T(ps[6].ap.bitcast(BF16), "ps6")
    final_ops = []

    with ExitStack() as gst:
        ones_bf = B.sb(gst, "ones_bf", [128, 128], BF16)
        B.memset("pool", ones_bf, 1.0)
        one_f = B.sb(gst, "one_f", [128, 1], F32)
        B.memset("pool", one_f, 1.0)
        eps_t = B.sb(gst, "eps_t", [128, 1], F32)
        B.memset("pool", eps_t, EPS)
        B.eps_t = eps_t
        modc = B.sb(gst, "modc", [128, 96], F32)
        A1 = B.sb(gst, "A1", [128, KC], F32)
        A2 = B.sb(gst, "A2", [128, KC], F32)
        bcol = B.sb(gst, "bcol", [128, 96], F32)
        B.dma("sp", bcol, b_in_col)
        gcol = B.sb(gst, "gcol", [128, 4], F32)
        B.dma("sp", gcol, qkg_col)
        gqs = B.sb(gst, "gqs", [128, 1], F32)
        B.ts("dve", gqs, gcol[:, 0:1], 128.0 ** -0.5, ALU.mult)
        pv = B.sb(gst, "pv", [128, 1], F32)
        B.dma("sp", pv, pvalid)
        ident = B.sb(gst, "ident", [128, 128], BF16)
        B.dma("pool", ident, ident_in)
        ones_f = B.sb(gst, "ones_f", [128, 128], F32)
        B.memset("pool", ones_f, 1.0)
        kcmpT = B.sb(gst, "kcmpT", [128, 4, 256], BF16)
        vcmp = B.sb(gst, "vcmp", [128, 4, 2, 128], BF16)

        if "A" in ph:
            with ExitStack() as st:
                cc = B.sb(st, "cc", [128, KC], F32)
                B.dma("sp", cc, c_col)
                sc = B.sb(st, "sc", [128, KC], F32)
                B.act(sc, cc, AF.Silu)
                brow = B.sb(st, "brow", [1, 6 * D], F32)
                B.dma("sp", brow, b_ada)
                mrow = B.sb(st, "mrow", [1, 6 * D], F32)
                wa = [B.sb(st, "wa%d" % i, [128, KC, 512], F32) for i in range(2)]
                for n in range(24):
                    w = wa[n % 2]
                    B.dma("sp" if n % 2 == 0 else "act", w, T(w_ada.ap[:, n * 512:(n + 1) * 512].rearrange("(kc p) n -> p kc n", p=128), "w_ada"))
                    pt = ps[n % 2]
                    for kc in range(KC):
                        B.mm(pt[0:1, :], sc[:, kc:kc + 1], w[:, kc, :], kc == 0, kc == KC - 1)
                    B.tt("dve", mrow[0:1, n * 512:(n + 1) * 512].k(n), pt[0:1, :], brow[0:1, n * 512:(n + 1) * 512], ALU.add)
                    for j in range(4):
                        jj = n * 4 + j
                        B.mm(ps[2][:, jj:jj + 1].k(jj), mrow[0:1, jj * 128:(jj + 1) * 128].k(n), one_f[0:1, 0:1], True, True)
                B.copy("dve", modc, T(ps[2].ap[:, 0:96], ps[2].key), reads=[(ps[2].key, jj) for jj in range(96)])
                B.dma("sp", modd, mrow, reads=[(mrow.key, n) for n in range(24)])
                g1 = B.sb(st, "g1", [128, KC], F32)
                B.dma("sp", g1, g1_col)
                B.stt("dve", A1, modc[:, 16:32], 1.0, g1, ALU.add, ALU.mult)
                g2c = B.sb(st, "g2c", [128, KC], F32)
                B.dma("sp", g2c, g2_col)
                B.stt("dve", A2, modc[:, 64:80], 1.0, g2c, ALU.add, ALU.mult)
            S.fence()

        def proj_pass(st, tok0, own):
            hT = B.sb(st, "hT", [128, KC, 2048], BF16)
            with ExitStack() as s1:
                xs = [B.sb(s1, "xs%d" % i, [128, KC, 256], F32) for i in range(2)]
                sq = [B.sb(s1, "sq%d" % i, [128, KC, 256], BF16) for i in range(2)]
                rs = [B.sb(s1, "rs%d" % i, [128, 256], F32) for i in range(2)]
                tm = [B.sb(s1, "tm%d" % i, [128, 256], F32) for i in range(2)]
                for sg in range(8):
                    x_, q_, r_ = xs[sg % 2], sq[sg % 2], rs[sg % 2]
                    c0 = tok0 + sg * 256
                    B.dma("sp", x_, T(xT.ap[:, :, c0:c0 + 256], "xT"))
                    B.act(q_, x_, AF.Square)
                    pt = ps[sg % 2]
                    for kc in range(KC):
                        B.mm(pt[:, 0:256], ones_bf, q_[:, kc, :], kc == 0, kc == KC - 1)
                    B.rstd_from_psum(r_, pt[:, 0:256], D, 256)
                    for kc in range(KC):
                        t_ = tm[kc % 2]
                        B.stt("dve", t_, x_[:, kc, :], A1[:, kc:kc + 1], r_, ALU.mult, ALU.mult)
                        B.act(hT[:, kc, sg * 256:(sg + 1) * 256].k(kc, sg // 2), t_, AF.Identity, bias=modc[:, kc:kc + 1])
            S.fence()
            if "HTD" in CFG["debug"] and own:
                htd = B.dscr("HTD", [128, KC, 2048], BF16)
                B.dma("sp", htd, hT, reads=[(hT.key, kc, g_) for kc in range(KC) for g_ in range(4)])
            wb = [B.sb(st, "wb%d" % i, [128, KC, 512], BF16) for i in range(3)]
            wctr = [0]

            def load_w(c0, cw):
                w = wb[wctr[0] % 3]
                wctr[0] += 1
                B.dma("pool", w[:, :, 0:cw], T(w_in.ap[:, c0:c0 + cw].rearrange("(kc p) n -> p kc n", p=128), "w_in"))
                return w

            pctr = [0]

            def fm_matmul(w, m0, mw, grp):
                pt = ps[pctr[0] % 4]
                pctr[0] += 1
                for kc in range(KC):
                    B.mm(pt[0:mw, :], w[:, kc, m0:m0 + mw], hT[:, kc, grp * 512:(grp + 1) * 512].k(kc, grp), kc == 0, kc == KC - 1)
                return pt

            ob = [B.sb(st, "ob%d" % i, [128, 512], BF16) for i in range(3)]
            of = [B.sb(st, "of%d" % i, [128, 512], F32) for i in range(3)]
            sqb = [B.sb(st, "sqb%d" % i, [128, 512], BF16) for i in range(2)]
            rq = [B.sb(st, "rq%d" % i, [128, 512], F32) for i in range(2)]
            octr = [0]

            def normed_seg(c0, nchunk, bcol0, gsc, dst, dst_tok0, grps=(0, 1, 2, 3)):
                for c4 in range(0, nchunk, 4):
                    w = load_w(c0 + c4 * 128, 512)
                    for m in range(4):
                        ch = c4 + m
                        for grp in grps:
                            pt = fm_matmul(w, m * 128, 128, grp)
                            i = octr[0]
                            octr[0] += 1
                            qf, sq_, r_, o_ = of[i % 3], sqb[i % 2], rq[i % 2], ob[i % 3]
                            bc_ = bcol[:, bcol0 + ch:bcol0 + ch + 1]
                            B.act(qf, pt, AF.Identity, bias=bc_)
                            B.act(sq_, pt, AF.Square, bias=bc_)
                            p2 = ps[4 + i % 2]
                            B.mm(p2, ones_bf, sq_, True, True)
                            B.rstd_from_psum(r_, p2, 128, 512)
                            B.stt("dve", o_, qf, gsc, r_, ALU.mult, ALU.mult)
                            B.dma("sp", T(dst.ap[ch, :, dst_tok0 + grp * 512:dst_tok0 + (grp + 1) * 512], dst.key), o_, wkey=(dst.key, ch, grp, tok0))

            def plain_seg(c0, nchunk, bcol0, dst, dst_tok0):
                for c4 in range(0, nchunk, 4):
                    w = load_w(c0 + c4 * 128, 512)
                    for m in range(4):
                        ch = c4 + m
                        for grp in range(4):
                            pt = fm_matmul(w, m * 128, 128, grp)
                            i = octr[0]
                            octr[0] += 1
                            o_ = ob[i % 3]
                            B.act(o_, pt, AF.Identity, bias=bcol[:, bcol0 + ch:bcol0 + ch + 1])
                            B.dma("sp", T(dst.ap[ch, :, dst_tok0 + grp * 512:dst_tok0 + (grp + 1) * 512], dst.key), o_, wkey=(dst.key, ch, grp, tok0))

            def tokmajor_seg(c0, dst, dst_tok0, brow_b, tiles=range(16)):
                w = load_w(c0, 512)
                for tt_ in tiles:
                    pt = ps[pctr[0] % 4]
                    pctr[0] += 1
                    for kc in range(KC):
                        B.mm(pt, hT[:, kc, tt_ * 128:(tt_ + 1) * 128].k(kc, tt_ // 4), w[:, kc, :], kc == 0, kc == KC - 1)
                    i = octr[0]
                    octr[0] += 1
                    o_ = ob[i % 3]
                    B.tt("dve", o_, pt, brow_b, ALU.add)
                    r0 = dst_tok0 + tt_ * 128
                    B.dma("sp", T(dst.ap[r0:r0 + 128, :], dst.key), o_, wkey=(dst.key, tt_, tok0))

            def glu_seg(grps, dst_c0, keep_last32):
                for half in range(2):
                    wu = load_w(OGLU + half * 512, 512)
                    wv = load_w(OGLU + 1024 + half * 512, 512)
                    for m in range(4):
                        ch = half * 4 + m
                        for grp in grps:
                            pu = fm_matmul(wu, m * 128, 128, grp)
                            pvv = fm_matmul(wv, m * 128, 128, grp)
                            i = octr[0]
                            octr[0] += 1
                            sg_, o_ = of[i % 3], rq[i % 2]
                            B.act(sg_, pvv, AF.Sigmoid, bias=bcol[:, 56 + ch:57 + ch])
                            B.stt("dve", o_, pu, bcol[:, 48 + ch:49 + ch], sg_, ALU.add, ALU.mult)
                            if keep_last32:
                                B.ts("dve", o_[:, 480:512], o_[:, 480:512], pv[:, 0:1], ALU.mult)
                                B.dma("sp", T(UT.ap[ch * 128:(ch + 1) * 128, 0:32], "UT"), o_[:, 480:512], wkey=("UT", ch, "pre"))
                            else:
                                c_ = 32 + grp * 512
                                B.dma("sp", T(UT.ap[ch * 128:(ch + 1) * 128, c_:c_ + 512], "UT"), o_, wkey=("UT", ch, grp))

            bvs = B.sb(st, "bvs", [128, 512], F32)
            bvw = B.sb(st, "bvw", [128, 512], F32)
            B.dma("sp", bvs, T(b_in.ap[0:1, OVS:OVS + 512].partition_broadcast(128), "b_in"))
            B.dma("sp", bvw, T(b_in.ap[0:1, OVW:OVW + 512].partition_broadcast(128), "b_in"))
            plain_seg(OKC, 4, 16, KCT, tok0)
            if CFG.get("bquick"):
                return
            plain_seg(OVC, 4, 20, VCT, tok0)
            normed_seg(OKS, 4, 24, gcol[:, 2:3], KST, tok0)
            normed_seg(OKW, 4, 28, gcol[:, 3:4], KWT, tok0, grps=(0, 1, 2, 3) if own else (3,))
            tokmajor_seg(OVS, VS, tok0, bvs)
            tokmajor_seg(OVW, VW, tok0, bvw, tiles=range(16) if own else range(12, 16))
            if not own:
                glu_seg([3], 0, True)
            else:
                normed_seg(OQ, 16, 0, gqs[:, 0:1], QT, 0)
                glu_seg([0, 1, 2, 3], 32, False)
                w = load_w(OGN, 48)
                for grp in range(4):
                    pt = fm_matmul(w, 0, 48, grp)
                    i = octr[0]
                    octr[0] += 1
                    o_ = of[i % 3]
                    B.act(o_[0:48, :], pt[0:48, :], AF.Sigmoid, bias=bcol[0:48, 32:33])
                    B.dma("sp", T(GNT.ap[:, grp * 512:(grp + 1) * 512], "GNT"), o_[0:48, :], wkey=("GNT", grp))
                for c4 in range(0, 32, 4):
                    w = load_w(OGM + c4 * 128, 512)
                    for m in range(4):
                        ch = c4 + m
                        for grp in range(4):
                            pt = fm_matmul(w, m * 128, 128, grp)
                            i = octr[0]
                            octr[0] += 1
                            o_ = ob[i % 3]
                            B.act(o_, pt, AF.Sigmoid, bias=bcol[:, 64 + ch:65 + ch])
                            B.dma("sp", T(GM.ap[ch, :, grp * 512:(grp + 1) * 512], "GM"), o_, wkey=("GM", ch, grp))

        if "B" in ph:
            with ExitStack() as st:
                proj_pass(st, 0, False)
            S.fence()
            with ExitStack() as st:
                proj_pass(st, NPRE, True)
            S.fence()


        if "C" in ph:
            with ExitStack() as st:
                for kv in range(2):
                    w1 = B.sb(st, "w1_%d" % kv, [128, 32, 128], BF16)
                    B.dma("pool", w1, T(cmp_w1[kv].ap.rearrange("(l d) h -> d l h", d=128), "cw1"))
                    w2 = B.sb(st, "w2_%d" % kv, [128, 128], BF16)
                    B.dma("pool", w2, cmp_w2[kv])
                    posT = B.sb(st, "posT_%d" % kv, [128, 32], BF16)
                    B.dma("pool", posT, cmp_posT[kv])
                    posb = B.sb(st, "posb_%d" % kv, [128, 1], F32)
                    for l in range(32):
                        B.mm(ps[6][:, 0:1], w1[:, l, :], posT[:, l:l + 1], l == 0, l == 31)
                    B.copy("dve", posb, ps[6][:, 0:1])
                    src = KCT if kv == 0 else VCT
                    for g in range(4):
                        xt_ = B.sb(st, "cx_%d_%d" % (kv, g), [128, NPRE + NTOK], BF16)
                        B.dma("sp", xt_, T(src.ap[g], src.key))
                        pt = ps[g % 2]
                        for l in range(32):
                            B.mm(pt[:, 0:255], w1[:, l, :], T(xt_.ap[:, l:l + 16 * 254 + 1:16], xt_.key), l == 0, l == 31)
                        hid = B.sb(st, "hid_%d_%d" % (kv, g), [128, 256], BF16)
                        hx = B.sb(st, "hx_%d_%d" % (kv, g), [128, 256], F32)
                        h1 = B.sb(st, "h1_%d_%d" % (kv, g), [128, 256], F32)
                        h2_ = B.sb(st, "h2_%d_%d" % (kv, g), [128, 256], F32)
                        B.act(hx[:, 0:255], pt[:, 0:255], AF.Identity, bias=posb)
                        B.gelu_tanh(hid[:, 0:255], hx[:, 0:255], h1[:, 0:255], h2_[:, 0:255])
                        if kv == 0:
                            p2 = ps[2 + g % 2]
                            B.mm(p2[:, 0:255], w2, hid[:, 0:255], True, True)
                            sq_ = B.sb(st, "csq_%d" % g, [128, 256], BF16)
                            B.act(sq_[:, 0:255], p2[:, 0:255], AF.Square)
                            p3 = ps[4 + g % 2]
                            B.mm(p3[:, 0:255], ones_bf, sq_[:, 0:255], True, True)
                            r_ = B.sb(st, "crs_%d" % g, [128, 256], F32)
                            B.rstd_from_psum(r_[:, 0:255], p3[:, 0:255], 128, 255)
                            B.stt("dve", kcmpT[:, g, 0:255].k(g), p2[:, 0:255], gcol[:, 1:2], r_[:, 0:255], ALU.mult, ALU.mult)
                        else:
                            for c in range(2):
                                rows = 128 if c == 0 else 127
                                p2 = ps[2 + c]
                                B.mm(p2[0:rows, 0:128], hid[:, c * 128:c * 128 + rows], w2, True, True)
                                B.copy("dve", vcmp[0:rows, g, c, :].k(g, c), p2[0:rows, 0:128])
                if "KCMPD" in CFG["debug"]:
                    B.dma("sp", B.dscr("KCMPD", [128, 4, 256], BF16), kcmpT, reads=[(kcmpT.key, g) for g in range(4)])
                    B.dma("sp", B.dscr("VCMPD", [128, 4, 2, 128], BF16), vcmp, reads=[(vcmp.key, g, c) for g in range(4) for c in range(2)])
            S.fence()

        if "D" in ph:
            with ExitStack() as st:
                OVL = B.sb(st, "OVL", [128, 2, 64], BF16)
                B.dma("pool", OVL, OVL_in)
                SELC = B.sb(st, "SELC", [128, 4, 512], BF16)
                B.dma("pool", SELC, SELC_in)
                EX = B.sb(st, "EX", [64, 32, 128], BF16)
                B.dma("pool", EX, EX_in)
                KS_g = B.sb(st, "KS_g", [128, NPRE + NTOK], BF16)
                KW_g = B.sb(st, "KW_g", [128, NPRE + NTOK], BF16)
                VS_g = B.sb(st, "VS_g", [128, 32, 128], BF16)
                VW_g = B.sb(st, "VW_g", [128, 32, 128], BF16)
                BMs = B.sb(st, "BMs", [128, 32, 512], BF16)
                q4s = [B.sb(st, "q4_%d" % i, [128, 4, 512], BF16) for i in range(2)]
                gbs = B.sb(st, "gbs", [128, 12, 512], F32)
                WINM = B.sb(st, "WINM", [128, 8, 512], BF16)
                CMPM = B.sb(st, "CMPM", [128, 2, 512], BF16)
                SELB = B.sb(st, "SELB", [128, 4, 64], F32)
                oacc = B.sb(st, "oacc", [128, 4, 512], F32)
                Ef = [B.sb(st, "Ef%d" % i, [128, 512], F32) for i in range(2)]
                Eb = [B.sb(st, "Eb%d" % i, [128, 512], BF16) for i in range(6)]
                Pb = [B.sb(st, "Pb%d" % i, [128, 512], BF16) for i in range(6)]
                Em2 = [[B.sb(st, "Em%d_%d" % (k, i), [128, 512], BF16) for i in range(2)] for k in range(2)]
                Pn2 = [[B.sb(st, "Pn%d_%d" % (k, i), [128, 512], BF16) for i in range(2)] for k in range(2)]
                rden = B.sb(st, "rden", [128, 512], F32)
                wgt = B.sb(st, "wgt", [128, 512], F32)
                tmpo = B.sb(st, "tmpo", [128, 512], F32)
                ob_ = [B.sb(st, "obD%d" % i, [128, 512], BF16) for i in range(2)]
                sc = B.sb(st, "sc", [128, 64], F32)
                sc2 = B.sb(st, "sc2", [128, 64], F32)
                m8a = B.sb(st, "m8a", [128, 8], F32)
                m8b = B.sb(st, "m8b", [128, 8], F32)
                thr = B.sb(st, "thr", [128, 1], F32)
                maskf = B.sb(st, "maskf", [128, 64], BF16)
                maskT = B.sb(st, "maskT", [64, 512], BF16)
                PS_S = [ps[0], ps[1]]
                PS_ON, PS_DEN, PS_IMP, PS_BM = ps[2], ps[3], ps[4], ps[5]
                ectr = [0]

                bctr = [0]
                PS_S3 = [ps[0], ps[1], ps[6]]

                def branch_epilogue(hh, row, first, pON, pDEN):
                    steps = [lambda: B.ts("dve", rden, pDEN, 1e-30, ALU.max)]
                    for qq in range(4):
                        steps.append(lambda qq=qq: B.recip(rden[:, qq * 128:(qq + 1) * 128], rden[:, qq * 128:(qq + 1) * 128]))
                    steps.append(lambda: B.tt("dve", wgt, rden, gbs[:, row, :].k(row), ALU.mult))
                    steps.append(lambda: B.tt("dve", tmpo, pON, wgt, ALU.mult))
                    steps.append(lambda: B.tt("pool", oacc[:, hh, :].k(hh), oacc[:, hh, :].k(hh), tmpo, ALU.add))
                    return steps

                for g in range(4):
                    B.dma("sp", KS_g, T(KST.ap[g], "KST"))
                    B.dma("sp", KW_g[:, 1536:NPRE + NTOK], T(KWT.ap[g, :, 1536:NPRE + NTOK], "KWT"))
                    B.dma("sp", VS_g, T(VS.ap[:, g * 128:(g + 1) * 128].rearrange("(c p) d -> p c d", p=128), "VS"))
                    B.dma("sp", VW_g[:, 12:32, :], T(VW.ap[1536:NPRE + NTOK, g * 128:(g + 1) * 128].rearrange("(c p) d -> p c d", p=128), "VW"))
                    for qt in range(4):
                        u0 = qt * 512
                        q4 = q4s[qt % 2]
                        B.dma("sp", q4, T(QT.ap[4 * g:4 * g + 4, :, u0:u0 + 512].rearrange("h d t -> d h t"), "QT"))
                        for r in range(12):
                            row = g * 12 + r
                            B.dma("sp", gbs[:, r, :].k(r), T(GNT.ap[row:row + 1, u0:u0 + 512].partition_broadcast(128), "GNT"))
                        B.dma("pool", WINM, T(WINM_in.ap[qt], "WINM_in"))
                        B.dma("pool", CMPM, T(CMPM_in.ap[:, :, u0:u0 + 512], "CMPM_in"))
                        B.dma("sp", SELB, T(SELB_in.ap[u0:u0 + 512, :].rearrange("(a p) j -> p a j", p=128), "SELB_in"))
                        nch = (NPRE + u0 + 512) // 128
                        for hh in range(4):
                            Em, Pn = Em2[hh % 2], Pn2[hh % 2]
                            for c in range(2):
                                rows = 128 if c == 0 else 127
                                pS = PS_S[ectr[0] % 2]
                                ef = Ef[ectr[0] % 2]
                                ectr[0] += 1
                                B.mm(pS[0:rows, :], kcmpT[:, g, c * 128:c * 128 + rows].k(g), q4[:, hh, :], True, True)
                                B.act(ef[0:rows, :], pS[0:rows, :], AF.Exp)
                                B.tt("dve", Em[c][0:rows, :], ef[0:rows, :], CMPM[0:rows, c, :], ALU.mult)
                            for c in range(2):
                                rows = 128 if c == 0 else 127
                                B.mm(PS_DEN, ones_bf[0:rows, :], Em[c][0:rows, :], c == 0, c == 1)
                            B.ts("dve", rden, PS_DEN, 1e-30, ALU.max)
                            B.recip(rden, rden)
                            for c in range(2):
                                rows = 128 if c == 0 else 127
                                B.tt("dve", Pn[c][0:rows, :], Em[c][0:rows, :], rden[0:rows, :], ALU.mult)
                            for c in range(2):
                                rows = 128 if c == 0 else 127
                                B.mm(PS_ON, vcmp[0:rows, g, c, :].k(g, c), Pn[c][0:rows, :], c == 0, c == 1)
                            for ut in range(4):
                                for c in range(2):
                                    rows = 128 if c == 0 else 127
                                    B.mm(PS_IMP[:, ut * 64:(ut + 1) * 64].k(ut), Pn[c][0:rows, ut * 128:(ut + 1) * 128], OVL[0:rows, c, :],
                                         hh == 0 and c == 0 and ut == 0, hh == 3 and c == 1, sgc=True)
                            B.tt("dve", oacc[:, hh, :].k(hh), PS_ON, gbs[:, hh * 3 + 0, :].k(hh * 3), ALU.mult)
                        for ut in range(4):
                            B.tt("dve", sc, PS_IMP[:, ut * 64:(ut + 1) * 64].k(ut), SELB[:, ut, :], ALU.add,
                                 reads=[(PS_IMP.key, u_) for u_ in range(4)])
                            S.add("dve", lambda e, o=m8a.ap, i=sc.ap: e.max(out=o, in_=i), reads=[sc.key], writes=[m8a.key])
                            S.add("dve", lambda e, o=sc2.ap, r=m8a.ap, i=sc.ap: e.match_replace(out=o, in_to_replace=r, in_values=i, imm_value=-3.0e38),
                                  reads=[sc.key, m8a.key], writes=[sc2.key])
                            S.add("dve", lambda e, o=m8b.ap, i=sc2.ap: e.max(out=o, in_=i), reads=[sc2.key], writes=[m8b.key])
                            B.ts("dve", thr, m8b[:, 7:8], -1.0e29, ALU.max)
                            B.ts("dve", maskf, sc, thr[:, 0:1], ALU.is_ge)
                            B.transpose(pst[0:64, ut * 128:(ut + 1) * 128], maskf, ident)
                            B.copy("act", maskT[:, ut * 128:(ut + 1) * 128], pst[0:64, ut * 128:(ut + 1) * 128])
                        for c in range(nch):
                            pbm = PS_BM if c % 2 == 0 else ps[6]
                            B.mm(pbm, EX[:, c, :], maskT, True, True)
                            di = c - (nch - 4)
                            if di >= 0:
                                B.tt("dve", BMs[:, c, :].k(c), pbm, SELC[:, di, :], ALU.mult)
                            else:
                                B.copy("act", BMs[:, c, :].k(c), pbm)
                        LAG = 3
                        pend_ep = []
                        for hh in range(4):
                            for br in (1, 2):
                                if br == 1:
                                    chunks = [(c, KS_g, VS_g, BMs[:, c, :].k(c)) for c in range(nch)]
                                else:
                                    c0 = (NPRE + u0 - 512) // 128
                                    chunks = [(c0 + i, KW_g, VW_g, WINM[:, i, :]) for i in range(8)]
                                bctr[0] += 1
                                pON, pDEN = (ps[2], ps[3]) if bctr[0] % 2 == 0 else (ps[4], ps[5])
                                n = len(chunks)
                                pbs = {}
                                for ci in range(n + LAG):
                                    if ci < n:
                                        c, Kg, Vg, msk = chunks[ci]
                                        pS = PS_S3[ectr[0] % 3]
                                        eb, pb = Eb[ectr[0] % 6], Pb[ectr[0] % 6]
                                        ectr[0] += 1
                                        B.mm(pS, Kg[:, c * 128:(c + 1) * 128], q4[:, hh, :], True, True)
                                        B.act(eb, pS, AF.Exp)
                                        B.tt("dve", pb, eb, msk, ALU.mult)
                                        pbs[ci] = pb
                                        if pend_ep and ci >= 1:
                                            pend_ep.pop(0)()
                                    cj_ = ci - LAG
                                    if cj_ >= 0:
                                        c, Kg, Vg, msk = chunks[cj_]
                                        B.mm(pON, Vg[:, c, :], pbs[cj_], cj_ == 0, cj_ == n - 1)
                                        B.mm(pDEN, ones_bf, pbs[cj_], cj_ == 0, cj_ == n - 1)
                                for fn in pend_ep:
                                    fn()
                                pend_ep = branch_epilogue(hh, hh * 3 + br, False, pON, pDEN)
                                if br == 2:
                                    o_ = ob_[hh % 2]
                                    pend_ep.append(lambda o_=o_, hh=hh: B.copy("act", o_, oacc[:, hh, :].k(hh)))
                                    pend_ep.append(lambda o_=o_, hh=hh: B.dma("sp", T(OT.ap[4 * g + hh, :, u0:u0 + 512], "OT"), o_, wkey=("OT", g, hh, qt)))
                            if hh == 3:
                                for fn in pend_ep:
                                    fn()
                                pend_ep = []
            S.fence()

        if "F" in ph:
            with ExitStack() as st:
                dw = B.sb(st, "dw", [128, 8, 31], F32)
                B.dma("sp", dw, dwT)
                cv = B.sb(st, "cv", [128, 3, 8], F32)
                B.dma("sp", cv, cvec)
                pwb = B.sb(st, "pwb", [128, 16], F32)
                B.dma("sp", pwb, pwb_col)
                pw = B.sb(st, "pw", [128, 8, D], BF16)
                for cg in range(4):
                    B.dma("pool", pw[:, :, cg * 512:(cg + 1) * 512].k(cg), T(conv_pw_w.ap[:, cg * 512:(cg + 1) * 512].rearrange("(c p) n -> p c n", p=128), "pww"))
                HT = 1024
                Y = B.sb(st, "Y", [128, 8, HT], F32)
                ycT = B.sb(st, "ycT", [128, 8, HT], BF16)
                ysq = [B.sb(st, "ysq%d" % i, [128, 512], F32) for i in range(2)]
                mean = B.sb(st, "mean", [128, 512], F32)
                msq = B.sb(st, "msq", [128, 512], F32)
                rstd = B.sb(st, "rstdF", [128, 512], F32)
                zt = [B.sb(st, "zt%d" % i, [128, 512], F32) for i in range(2)]
                gm1 = [B.sb(st, "gm1_%d" % i, [128, 512], BF16) for i in range(2)]
                yo = [B.sb(st, "yo%d" % i, [128, 512], BF16) for i in range(2)]
                Dg = B.sb(st, "Dg", [128, 8, 31, 128], BF16)
                for cj in range(8):
                    for k in range(31):
                        B.ts("dve" if k % 2 == 0 else "pool", Dg[:, cj, k, :].k(cj, k), ident, dw[:, cj, k:k + 1], ALU.mult)
                Ub = [B.sb(st, "Ub%d" % i, [128, HT + 32], BF16) for i in range(2)]
                for th in range(2):
                    t0 = th * HT
                    for cj in range(8):
                        U = Ub[cj % 2]
                        B.dma("pool", U, T(UT.ap[cj * 128:(cj + 1) * 128, t0:t0 + HT + 32], "UT"))
                        for grp in range(2):
                            pt = ps[4 + (cj * 2 + grp) % 4]
                            for k in range(31):
                                o = 2 + k + grp * 512
                                B.mm(pt, Dg[:, cj, k, :].k(cj, k), U[:, o:o + 512], k == 0, k == 30)
                            B.act(Y[:, cj, grp * 512:(grp + 1) * 512].k(cj), pt, AF.Identity, bias=cv[:, 0, cj:cj + 1])
                    for grp in range(2):
                        cs = slice(grp * 512, (grp + 1) * 512)
                        p1, p2 = ps[0], ps[1]
                        for cj in range(8):
                            B.mm(p1, ones_f, Y[:, cj, cs].k(cj), cj == 0, cj == 7)
                        for cj in range(8):
                            q_ = ysq[cj % 2]
                            B.act(q_, Y[:, cj, cs].k(cj), AF.Square)
                            B.mm(p2, ones_f, q_, cj == 0, cj == 7)
                        B.ts("dve", mean, p1, 1.0 / 1024, ALU.mult)
                        B.tt("dve", msq, mean, mean, ALU.mult)
                        B.stt("dve", rstd, p2, 1.0 / 1024, msq, ALU.mult, ALU.subtract)
                        B.act(rstd, rstd, AF.Sqrt, bias=eps_t, scale=1.0)
                        B.recip(rstd, rstd)
                        for cj in range(8):
                            z_ = zt[cj % 2]
                            B.tt("dve", z_, Y[:, cj, cs].k(cj), mean, ALU.subtract)
                            B.tt("dve", z_, z_, rstd, ALU.mult)
                            B.act(ycT[:, cj, cs].k(cj, grp), z_, AF.Silu, bias=cv[:, 2, cj:cj + 1], scale=cv[:, 1, cj:cj + 1])
                        for j in range(16):
                            pt = ps[2 + j % 2]
                            for cj in range(8):
                                B.mm(pt, pw[:, cj, j * 128:(j + 1) * 128].k(j // 4), ycT[:, cj, cs].k(cj, grp), cj == 0, cj == 7)
                            g_ = gm1[j % 2]
                            c0 = t0 + grp * 512
                            B.dma("sp", g_, T(GM.ap[16 + j, :, c0:c0 + 512], "GM"))
                            o_ = yo[j % 2]
                            B.stt("dve", o_, pt, pwb[:, j:j + 1], g_, ALU.add, ALU.mult)
                            B.dma("sp", T(YCG.ap[j, :, c0:c0 + 512], "YCG"), o_, wkey=("YCG", j, th, grp))
            S.fence()

        if "G" in ph:
            with ExitStack() as st:
                g1b = B.sb(st, "g1b", [128, D], F32)
                B.dma("sp", g1b, T(modd.ap[0:1, 2 * D:3 * D].partition_broadcast(128), "modd"))
                HT = 1024
                oT = B.sb(st, "oT", [128, 16, HT], BF16)
                mixT = B.sb(st, "mixT", [128, 16, HT], BF16)
                wbs = [B.sb(st, "wg%d" % i, [128, KC, 512], BF16) for i in range(3)]
                gm0 = [B.sb(st, "gm0_%d" % i, [128, HT], BF16) for i in range(2)]
                ycg = [B.sb(st, "ycg_%d" % i, [128, HT], BF16) for i in range(2)]
                t1 = [B.sb(st, "t1_%d" % i, [128, 512], F32) for i in range(2)]
                xt_ = [B.sb(st, "xg_%d" % i, [128, 512], F32) for i in range(3)]
                xo_ = [B.sb(st, "xo_%d" % i, [128, 512], F32) for i in range(3)]
                wc = [0]
                for th in range(2):
                    t0 = th * HT
                    B.dma("sp", oT, T(OT.ap[:, :, t0:t0 + HT].rearrange("h d t -> d h t"), "OT"))
                    for cg in range(4):
                        w = wbs[wc[0] % 3]
                        wc[0] += 1
                        B.dma("pool", w, T(w_nsa_out.ap[:, cg * 512:(cg + 1) * 512].rearrange("(kc p) n -> p kc n", p=128), "wno"))
                        for m in range(4):
                            j = cg * 4 + m
                            g_, y_ = gm0[j % 2], ycg[j % 2]
                            B.dma("sp", g_, T(GM.ap[j, :, t0:t0 + HT], "GM"))
                            B.dma("sp", y_, T(YCG.ap[j, :, t0:t0 + HT], "YCG"))
                            for grp in range(2):
                                cs = slice(grp * 512, (grp + 1) * 512)
                                pt = ps[(j * 2 + grp) % 4]
                                for h in range(16):
                                    B.mm(pt, w[:, h, m * 128:(m + 1) * 128], oT[:, h, cs], h == 0, h == 15)
                                t_ = t1[grp]
                                B.tt("dve", t_, pt, g_[:, cs], ALU.mult)
                                B.tt("pool", mixT[:, j, cs].k(j, grp), t_, y_[:, cs], ALU.add)
                    for cg in range(4):
                        w = wbs[wc[0] % 3]
                        wc[0] += 1
                        B.dma("pool", w, T(w_out.ap[:, cg * 512:(cg + 1) * 512].rearrange("(kc p) n -> p kc n", p=128), "wout"))
                        for tt_ in range(8):
                            i = cg * 8 + tt_
                            pt = ps[4 + i % 3]
                            for kc in range(KC):
                                B.mm(pt, mixT[:, kc, tt_ * 128:(tt_ + 1) * 128].k(kc, tt_ // 4), w[:, kc, :], kc == 0, kc == KC - 1)
                            x_, o_ = xt_[i % 3], xo_[i % 3]
                            r0 = t0 + tt_ * 128
                            B.dma("sp", x_, T(xtok.ap[r0:r0 + 128, cg * 512:(cg + 1) * 512], "xtok"))
                            B.tt("dve", o_, pt, g1b[:, cg * 512:(cg + 1) * 512], ALU.mult)
                            B.tt("pool", o_, o_, x_, ALU.add)
                            B.dma("sp", T(X1.ap[r0:r0 + 128, cg * 512:(cg + 1) * 512], "X1"), o_, wkey=("X1", th, cg, tt_))
            S.fence()

        if "H" in ph:
            with ExitStack() as st:
                NT = 512
                ntile = NT // 128
                g2b = B.sb(st, "g2b", [128, D], F32)
                B.dma("sp", g2b, T(modd.ap[0:1, 5 * D:6 * D].partition_broadcast(128), "modd"))
                h2T = B.sb(st, "h2T", [128, KC, NT], BF16)
                acc = B.sb(st, "acc", [128, ntile, D], F32)
                tau = B.sb(st, "tau", [128, ntile, 8], F32)
                negb = B.sb(st, "negb", [128, ntile, 8], F32)
                rz = B.sb(st, "rz", [128, ntile, 8], F32)
                wbs = [B.sb(st, "wh%d" % i, [128, KC, 512], BF16) for i in range(2)]
                Vb = [B.sb(st, "Vb%d" % i, [128, 4, D], BF16) for i in range(2)]
                x1t = B.sb(st, "x1t", [128, D], F32)
                h2f = B.sb(st, "h2f", [128, D], F32)
                h2b = B.sb(st, "h2b", [128, D], BF16)
                ssq = B.sb(st, "ssq", [128, 1], F32)
                sall4 = B.sb(st, "sall4", [128, ntile, 16, 128], F32)
                gxb = [T(h2f.ap[:, 0:512], (h2f.key, "gx0")), T(h2f.ap[:, 512:1024], (h2f.key, "gx1"))]
                g1b_ = [T(h2f.ap[:, 1024:1536], (h2f.key, "g10")), T(h2f.ap[:, 1536:2048], (h2f.key, "g11"))]
                wcnt = [0]

                def vmax(o, i):
                    S.add("dve", lambda e, o=o.ap, i=i.ap: e.max(out=o, in_=i), reads=[i.key], writes=[o.key])

                def vmr(o, r, i):
                    S.add("dve", lambda e, o=o.ap, r=r.ap, i=i.ap: e.match_replace(out=o, in_to_replace=r, in_values=i, imm_value=-3.0e38),
                          reads=[r.key, i.key], writes=[o.key])

                for tg in range(CFG.get('h_tg', NTOK // NT)):
                    for tt_ in range(ntile):
                        for dc in range(4):
                            B.memset("pool", T(acc.ap[:, tt_, dc * 512:(dc + 1) * 512], (acc.key, tt_, dc)), 0.0)
                    with ExitStack() as s1:
                        keysT = B.sb(s1, "keysT", [128, 16, 128], BF16)
                        B.dma("pool", keysT, keysT_in)
                        qT = B.sb(s1, "qT", [128, 16, NT], BF16)
                        s2 = B.sb(s1, "s2", [128, 128], F32)
                        tv = B.sb(s1, "tv", [128, 16, 16], F32)
                        cand = B.sb(s1, "cand", [128, 256], F32)
                        cand2 = B.sb(s1, "cand2", [128, 256], F32)
                        ce = B.sb(s1, "ce", [128, 256], F32)
                        m3 = B.sb(s1, "m3", [128, 3, 8], F32)
                        ntau = B.sb(s1, "ntau", [128, 1], F32)
                        zz = B.sb(s1, "zz", [128, 8], F32)
                        for tt_ in range(ntile):
                            r0 = tg * NT + tt_ * 128
                            B.dma("sp", x1t, T(X1.ap[r0:r0 + 128, :], "X1"))
                            B.memset("dve", ssq, 0.0)
                            B.act(h2b, x1t, AF.Square, accum=ssq)
                            B.act(ssq, ssq, AF.Sqrt, bias=eps_t, scale=1.0 / D)
                            B.recip(ssq, ssq)
                            B.ts("dve", h2b, x1t, ssq[:, 0:1], ALU.mult)
                            for half in range(2):
                                pT = pst if half == 0 else pst6
                                for k8 in range(8):
                                    kc = half * 8 + k8
                                    B.transpose(pT[:, k8 * 128:(k8 + 1) * 128].k("h", k8), h2b[:, kc * 128:(kc + 1) * 128], ident)
                                for k8 in range(8):
                                    kc = half * 8 + k8
                                    B.act(T(h2T.ap[:, kc, tt_ * 128:(tt_ + 1) * 128], (h2T.key, kc, tt_)),
                                          pT[:, k8 * 128:(k8 + 1) * 128].k("h", k8), AF.Identity,
                                          bias=modc[:, 48 + kc:49 + kc], scale=A2[:, kc:kc + 1],
                                          reads=[(pT.key, "h", k_) for k_ in range(8)])
                        for cg in range(4):
                            w = wbs[wcnt[0] % 2]
                            wcnt[0] += 1
                            B.dma("pool", w, T(peer_w_q.ap[:, cg * 512:(cg + 1) * 512].rearrange("(kc p) n -> p kc n", p=128), "pwq"))
                            for m in range(4):
                                j = cg * 4 + m
                                pt = ps[j % 2]
                                for kc in range(KC):
                                    B.mm(pt, w[:, kc, m * 128:(m + 1) * 128], T(h2T.ap[:, kc, :], (h2T.key, kc, 0)), kc == 0, kc == KC - 1,
                                         reads=[(h2T.key, kc, t_) for t_ in range(1, ntile)])
                                B.copy("act", qT[:, j, :].k(j), pt)
                        for tt_ in range(ntile):
                            cs = slice(tt_ * 128, (tt_ + 1) * 128)
                            for q4_ in range(4):
                                pt = ps[2 + q4_ % 2]
                                for i in range(4):
                                    hp = q4_ * 4 + i
                                    B.mm(pt[:, i * 128:(i + 1) * 128].k(i), qT[:, hp, cs].k(hp), keysT[:, hp, :], True, True)
                                B.copy("act", T(sall4.ap[:, tt_, q4_ * 4:q4_ * 4 + 4, :], (sall4.key, tt_, q4_)),
                                       T(pt.ap.rearrange("p (a k) -> p a k", a=4), pt.key), reads=[(pt.key, i) for i in range(4)])
                            for hp in range(16):
                                sv = T(sall4.ap[:, tt_, hp, :], (sall4.key, tt_, hp // 4))
                                vmax(tv[:, hp, 0:8].k(hp), sv)
                                vmr(s2, tv[:, hp, 0:8].k(hp), sv)
                                vmax(tv[:, hp, 8:16].k(hp), s2)
                            for h in range(8):
                                a_ = T(tv.ap[:, 2 * h, :].unsqueeze(2).to_broadcast([128, 16, 16]), (tv.key, 2 * h))
                                b_ = T(tv.ap[:, 2 * h + 1, :].unsqueeze(1).to_broadcast([128, 16, 16]), (tv.key, 2 * h + 1))
                                B.tt("dve", T(cand.ap.rearrange("p (a b) -> p a b", a=16), cand.key), a_, b_, ALU.add)
                                vmax(m3[:, 0, :], cand)
                                vmr(cand2, m3[:, 0, :], cand)
                                vmax(m3[:, 1, :], cand2)
                                vmr(cand2, m3[:, 1, :], cand2)
                                vmax(m3[:, 2, :], cand2)
                                tau_ = tau[:, tt_, h:h + 1].k(tt_, h)
                                B.stt("dve", tau_, m3[:, 1, 7:8], 0.5, m3[:, 2, 0:1], ALU.mult, ALU.add)
                                B.stt("dve", tau_, m3[:, 2, 0:1], -0.5, tau_, ALU.mult, ALU.add)
                                B.ts("dve", ntau, tau_, -1.0, ALU.mult)
                                B.act(ce, cand, AF.Exp, bias=ntau)
                                B.stt("dve", ce, cand, tau_, ce, ALU.is_ge, ALU.mult)
                                S.add("dve", lambda e, o=zz.ap[:, h:h + 1], i=ce.ap: e.reduce_sum(out=o, in_=i, axis=AX.X), reads=[ce.key], writes=[zz.key])
                            B.recip(T(rz.ap[:, tt_, :], (rz.key, tt_)), zz)
                            B.act(zz, zz, AF.Ln)
                            B.tt("dve", zz, zz, T(tau.ap[:, tt_, :], tau.key), ALU.add)
                            S.ops[-1].deps |= {S.last_w[(tau.key, tt_, h)] for h in range(8)}
                            B.ts("dve", T(negb.ap[:, tt_, :], (negb.key, tt_)), zz, -1.0, ALU.mult)
                    S.fence()
                    with ExitStack() as s5:
                        Gb = [B.sb(s5, "Gb%d" % i, [128, 4, NT], BF16) for i in range(2)]
                        Eb = [B.sb(s5, "EbH%d" % i, [128, 512], F32) for i in range(4)]
                        Wh = [B.sb(s5, "Wh%d" % i, [128, 512], BF16) for i in range(4)]
                        cf = [B.sb(s5, "cf%d" % i, [128, 4, 128], BF16) for i in range(3)]
                        b4s = [B.sb(s5, "b4_%d" % i, [128, 8, 4], F32) for i in range(2)]
                        negs = CFG.get('h_eg', 32)
                        Us, Vs = {}, {}

                        def load_U(eg):
                            Us[eg] = wbs[eg % 2]
                            B.dma("pool", Us[eg], T(peer_uT.ap[:, eg * 512:(eg + 1) * 512].rearrange("(kc p) n -> p kc n", p=128), "puT"))

                        def load_V(eg):
                            Vs[eg] = Vb[eg % 2]
                            B.dma("pool", Vs[eg], T(peer_v.ap[eg * 512:(eg + 1) * 512, :].rearrange("(s p) d -> p s d", p=128), "pv"))

                        def emit_A(eg, sub, kcs):
                            pA = ps[4 + sub % 2]
                            for kc in kcs:
                                B.mm(pA, Us[eg][:, kc, sub * 128:(sub + 1) * 128], T(h2T.ap[:, kc, :], (h2T.key, kc, 0)), kc == 0, kc == KC - 1)

                        def emit_gelu1(sub):
                            pA = ps[4 + sub % 2]
                            gxs = gxb[sub % 2]
                            g1s = g1b_[sub % 2]
                            B.act(gxs, pA, AF.Identity, scale=0.5)
                            B.act(g1s, pA, AF.Square, scale=0.2114594)
                            B.stt("dve", g1s, g1s, 1.0, gxs, ALU.add, ALU.mult)

                        def emit_gelu2(eg, sub):
                            gxs = gxb[sub % 2]
                            g1s = g1b_[sub % 2]
                            B.act(g1s, g1s, AF.Tanh, scale=1.5957691216)
                            B.stt("dve", Gb[eg % 2][:, sub, :].k(sub), g1s, 1.0, gxs, ALU.add, ALU.mult)

                        def emit_coef(unit):
                            eg_, tt2, PSWT_, cf2 = unit
                            B.tt("dve", cf2, T(Gb[eg_ % 2].ap[:, :, tt2 * 128:(tt2 + 1) * 128], (Gb[eg_ % 2].key, 0)),
                                 T(PSWT_.ap.rearrange("p (s t) -> p s t", s=4), PSWT_.key), ALU.mult)
                            S.ops[-1].deps |= {S.last_w[k] for k in [(Gb[eg_ % 2].key, sb_) for sb_ in range(4)] if k in S.last_w}

                        def emit_b4t4(eg_, tt2, slot):
                            s1v = T(sall4.ap[:, tt2].rearrange("q (h p) k -> q h p k", p=2)[:, :, 0, eg_ * 4:eg_ * 4 + 4], sall4.key)
                            B.tt("dve", b4s[slot], s1v, T(negb.ap[:, tt2, :].unsqueeze(2).to_broadcast([128, 8, 4]), negb.key), ALU.add)

                        load_U(0)
                        load_V(0)
                        if negs > 1:
                            load_U(1)
                        for sub in range(4):
                            emit_A(0, sub, range(KC))
                            emit_gelu1(sub)
                            emit_gelu2(0, sub)
                        units = [(eg, tt_) for eg in range(negs) for tt_ in range(ntile)]
                        emit_b4t4(0, 0, 0)
                        done = []
                        pend_add = []
                        pend_g2 = None
                        ectr = 0
                        for ui, (eg, tt_) in enumerate(units):
                            if tt_ == 0 and eg + 2 < negs:
                                load_U(eg + 2)
                            if tt_ == 2 and eg + 1 < negs:
                                load_V(eg + 1)
                            PSWT = ps[2 + ui % 2]
                            cf_ = cf[ui % 3]
                            b4 = b4s[ui % 2]
                            if ui + 1 < len(units):
                                emit_b4t4(units[ui + 1][0], units[ui + 1][1], (ui + 1) % 2)
                            vsrc = done[ui - 2] if ui >= 2 else None
                            for h in range(8):
                                e_, w_ = Eb[ectr % 4], Wh[ectr % 4]
                                ectr += 1
                                s2v = T(sall4.ap[:, tt_, 2 * h + 1, :], sall4.key)
                                for j in range(4):
                                    B.act(e_[:, j * 128:(j + 1) * 128].k(j), s2v, AF.Exp, bias=b4[:, h, j:j + 1])
                                S.add("dve", lambda e, o=w_.ap, a=e_.ap, sc_=rz.ap[:, tt_, h:h + 1]:
                                      e.scalar_tensor_tensor(out=o, in0=a, scalar=sc_, in1=a, op0=ALU.is_ge, op1=ALU.mult),
                                      reads=[(e_.key, j) for j in range(4)] + [(rz.key, tt_)], writes=[w_.key])
                                for fn in pend_add:
                                    fn()
                                pend_add = []
                                if h == 1 and ui >= 1:
                                    emit_coef(done[ui - 1])
                                if h == 2 and pend_g2 is not None:
                                    emit_gelu2(*pend_g2)
                                    pend_g2 = None
                                if eg + 1 < negs:
                                    emit_A(eg + 1, tt_, [2 * h, 2 * h + 1])
                                if vsrc is not None:
                                    eg2, tt2, _, cf2 = vsrc
                                    dc = h // 2
                                    po = ps[6 + dc % 2]
                                    for sub in (2 * (h % 2), 2 * (h % 2) + 1):
                                        B.mm(po, cf2[:, sub, :], Vs[eg2][:, sub, dc * 512:(dc + 1) * 512], sub == 0, sub == 3)
                                    if h % 2 == 1:
                                        a_ = T(acc.ap[:, tt2, dc * 512:(dc + 1) * 512], (acc.key, tt2, dc))
                                        pend_add.append(lambda a_=a_, po=po: B.tt("dve", a_, a_, po, ALU.add))
                                for sub in range(4):
                                    B.mm(PSWT[:, sub * 128:(sub + 1) * 128], w_[:, sub * 128:(sub + 1) * 128], ident, h == 0 and sub == 0, h == 7, sgc=True)
                            if eg + 1 < negs:
                                emit_gelu1(tt_)
                                pend_g2 = (eg + 1, tt_)
                            done.append((eg, tt_, PSWT, cf_))
                        for fn in pend_add:
                            fn()
                        emit_coef(done[-1])
                        for vsrc in done[-2:]:
                            eg2, tt2, _, cf2 = vsrc
                            for dc in range(4):
                                po = ps[6 + dc % 2]
                                for sub in range(4):
                                    B.mm(po, cf2[:, sub, :], Vs[eg2][:, sub, dc * 512:(dc + 1) * 512], sub == 0, sub == 3)
                                a_ = T(acc.ap[:, tt2, dc * 512:(dc + 1) * 512], (acc.key, tt2, dc))
                                B.tt("dve", a_, a_, po, ALU.add)
                    S.fence()
                    for tt_ in range(ntile):
                        r0 = tg * NT + tt_ * 128
                        B.dma("sp", x1t, T(X1.ap[r0:r0 + 128, :], "X1"))
                        B.tt("dve", h2f, T(acc.ap[:, tt_, :], acc.key), g2b, ALU.mult)
                        S.ops[-1].deps |= {S.last_w[k] for k in [(acc.key, tt_, dc) for dc in range(4)] if k in S.last_w}
                        B.tt("pool", h2f, h2f, x1t, ALU.add)
                        final_ops.append(B.dma("sp", T(out.ap[r0:r0 + 128, :], "out"), h2f, wkey=("out", r0)))
                    S.fence()
            S.fence()

        if "Z" in ph:
            with ExitStack() as st:
                t_ = B.sb(st, "zz", [128, D], F32)
                for i in range(16):
                    B.dma("sp", t_, T(xtok.ap[i * 128:(i + 1) * 128, :], "xtok"))
                    final_ops.append(B.dma("sp", T(out.ap[i * 128:(i + 1) * 128, :], "out"), t_, wkey=("out", i)))
    stats = S.emit(final_ops)
    return nc, stats


def host_inputs(inputs):
    x = np.asarray(inputs["x"], np.float32)
    c = np.asarray(inputs["c"], np.float32)
    g = lambda k: np.asarray(inputs[k], np.float32)[0]
    w_in, b_in = g("w_in"), g("b_in")
    shared = {
        "w_ada": np.ascontiguousarray(g("w_ada")),
        "b_ada": g("b_ada").reshape(1, -1),
        "w_in": np.ascontiguousarray(w_in),
        "b_in": b_in.reshape(1, -1),
        "g1_col": np.ascontiguousarray(g("norm1_g").reshape(KC, 128).T),
        "g2_col": np.ascontiguousarray(g("norm2_g").reshape(KC, 128).T),
    }
    bc = np.zeros((128, 96), np.float32)
    def put(col0, off, n):
        for i in range(n):
            bc[:, col0 + i] = b_in[off + i * 128: off + (i + 1) * 128]
    put(0, OQ, 16); put(16, OKC, 4); put(20, OVC, 4); put(24, OKS, 4); put(28, OKW, 4)
    bc[0:48, 32] = b_in[OGN:OGN + 48]
    put(48, OGLU, 8); put(56, OGLU + 1024, 8); put(64, OGM, 32)
    shared["b_in_col"] = bc
    kg = g("k_norm_g")
    shared["qkg_col"] = np.ascontiguousarray(np.stack([g("q_norm_g"), kg[0], kg[1], kg[2]], axis=1))
    shared["cmp_k_w1"] = g("cmp_k_w1"); shared["cmp_v_w1"] = g("cmp_v_w1")
    shared["cmp_k_w2"] = g("cmp_k_w2"); shared["cmp_v_w2"] = g("cmp_v_w2")
    shared["cmp_posT_k"] = np.ascontiguousarray(g("cmp_pos_k").T); shared["cmp_posT_v"] = np.ascontiguousarray(g("cmp_pos_v").T)
    shared["w_nsa_out"] = g("w_nsa_out"); shared["conv_pw_w"] = g("conv_pw_w"); shared["w_out"] = g("w_out")
    shared["peer_w_q"] = g("peer_w_q")
    shared["dwT"] = np.ascontiguousarray(g("conv_dw_w").T.reshape(8, 128, 31).transpose(1, 0, 2))
    shared["cvec"] = np.ascontiguousarray(np.stack([g("conv_dw_b"), g("conv_ln_g"), g("conv_ln_b")], 0).reshape(3, 8, 128).transpose(2, 0, 1))
    shared["pwb_col"] = np.ascontiguousarray(g("conv_pw_b").reshape(16, 128).T)
    shared["keysT"] = np.ascontiguousarray(g("peer_sub_keys").reshape(16, 128, 128).transpose(2, 0, 1))
    if "H" in CFG["phases"]:
        shared["peer_uT"] = np.ascontiguousarray(g("peer_u").T)
        shared["peer_v"] = g("peer_v")
    else:
        shared["peer_uT"] = np.zeros((128, 128), np.float32)
        shared["peer_v"] = np.zeros((128, 128), np.float32)
    shared.update(_shared_consts())
    maps = []
    for core in range(8):
        b, hf = core // 2, core % 2
        xb = x[b]
        if hf == 1:
            seg = xb
        else:
            seg = np.concatenate([np.zeros((NPRE, D), np.float32), xb[:NTOK]], axis=0)
        m = dict(shared)
        m["xT"] = np.ascontiguousarray(seg.T.reshape(KC, 128, NPRE + NTOK).transpose(1, 0, 2))
        m["xtok"] = np.ascontiguousarray(xb[hf * NTOK:(hf + 1) * NTOK])
        m["c_col"] = np.ascontiguousarray(c[b].reshape(KC, 128).T)
        m["pvalid"] = np.full((128, 1), float(hf), np.float32)
        m.update(_core_consts(hf))
        maps.append(m)
    return maps


def _shared_consts():
    c = {}
    c["ident"] = np.eye(128, dtype=np.float32)
    i = np.arange(256)[:, None]; j = np.arange(64)[None, :]
    ov = ((16 * i < 64 * j + 64) & (16 * i + 32 > 64 * j) & (i < 255)).astype(np.float32)
    c["OVL"] = np.ascontiguousarray(ov.reshape(2, 128, 64).transpose(1, 0, 2))
    p = np.arange(128)[:, None, None]; di = np.arange(4)[None, :, None]; u = np.arange(512)[None, None, :]
    c["SELC"] = (128 * di + p <= u).astype(np.float32)
    jj = np.arange(64)[:, None, None]; cc = np.arange(32)[None, :, None]; pp = np.arange(128)[None, None, :]
    c["EX"] = (jj == 2 * cc + pp // 64).astype(np.float32)
    return c


def _core_consts(hf):
    c = {}
    u = np.arange(NTOK)
    col = NPRE + u
    cur = col // 64
    j = np.arange(64)[None, :]
    glob_j = j - 32 * (1 - hf)
    glob_cur = (cur - 32 * (1 - hf))[:, None]
    forced = (glob_j == 0) | (glob_j == glob_cur) | (glob_j == glob_cur - 1)
    valid = (glob_j >= 0) & (glob_j <= glob_cur)
    fval = np.where(glob_j == 0, 3e30, np.where(glob_j == glob_cur, 2e30, 1e30))
    selb = np.where(valid, np.where(forced, fval, 0.0), -1e30).astype(np.float32)
    c["SELB"] = selb
    i = np.arange(256)[:, None]
    vis = (16 * i + 31 <= col[None, :]) & (i < 255) & ((i >= 128) | (hf == 1))
    c["CMPM"] = np.ascontiguousarray(vis.astype(np.float32).reshape(2, 128, NTOK).transpose(1, 0, 2))
    wm = np.zeros((4, 128, 8, 512), np.float32)
    p = np.arange(128)[:, None]; uu = np.arange(512)[None, :]
    for qt in range(4):
        q0 = NPRE + qt * 512
        for k in range(8):
            k0 = q0 - 512 + 128 * k
            diff = (q0 + uu) - (k0 + p)
            ok = (diff >= 0) & (diff < 512) & ((k0 + p >= NPRE) | (hf == 1))
            wm[qt, :, k, :] = ok
    c["WINM"] = wm
    return c


_CACHE = {}


def kernel(**inputs):
    maps = host_inputs(inputs)
    if "nc" not in _CACHE:
        _CACHE["nc"] = build_program()
    nc, stats = _CACHE["nc"]
    res = run_bass_kernel_spmd(nc, maps, core_ids=list(range(8)))
    _CACHE["res"] = res
    outp = np.zeros((4, 4096, D), np.float32)
    for core in range(8):
        b, hf = core // 2, core % 2
        outp[b, hf * NTOK:(hf + 1) * NTOK] = res.results[core]["out"]
    return outp
```

```python
import numpy as np
import ml_dtypes
from contextlib import ExitStack
import concourse.bass as bass
import concourse.mybir as mybir
from concourse.bass_utils import run_bass_kernel_spmd

F32 = mybir.dt.float32
BF16 = mybir.dt.bfloat16
AF = mybir.ActivationFunctionType
ALU = mybir.AluOpType
AX = mybir.AxisListType

ENGS = ("pe", "act", "dve", "pool", "sp")
NDSEM = 24


class Op:
    __slots__ = ("eng", "fn", "deps", "is_dma", "sig", "idx", "dma_n")


class Sched:
    def __init__(self, nc):
        self.nc = nc
        self.ops = []
        self.last_w = {}
        self.rd_eng = {}
        self.rd_dma = {}
        self.n_dma = {"hw": 0, "sw": 0}
        self.fence_deps = set()
        self.last_on_eng = {}
        self.dma_since_fence = []

    def add(self, eng, fn, reads=(), writes=(), dma=False):
        op = Op()
        op.eng, op.fn, op.is_dma, op.sig, op.dma_n = eng, fn, dma, None, -1
        op.idx = len(self.ops)
        deps = set(self.fence_deps)
        for r in reads:
            w = self.last_w.get(r)
            if w is not None:
                deps.add(w)
        for k in writes:
            w = self.last_w.get(k)
            if w is not None:
                deps.add(w)
            for rd in self.rd_eng.get(k, {}).values():
                deps.add(rd)
            for rd in self.rd_dma.get(k, ()):
                deps.add(rd)
        op.deps = deps
        for r in reads:
            if dma:
                self.rd_dma.setdefault(r, []).append(op.idx)
            else:
                self.rd_eng.setdefault(r, {})[eng] = op.idx
        for k in writes:
            self.last_w[k] = op.idx
            self.rd_eng[k] = {}
            self.rd_dma[k] = []
        if dma:
            cls = "sw" if eng == "pool" else "hw"
            op.dma_n = (cls, self.n_dma[cls])
            self.n_dma[cls] += 1
            self.dma_since_fence.append(op.idx)
        else:
            self.last_on_eng[eng] = op.idx
        self.ops.append(op)
        return op

    def fence(self):
        d = set(self.last_on_eng.values()) | set(self.dma_since_fence)
        self.fence_deps = d
        self.dma_since_fence = []
        self.last_w.clear()
        self.rd_eng.clear()
        self.rd_dma.clear()

    def emit(self, final_ops=()):
        nc, ops = self.nc, self.ops
        needed = [False] * len(ops)
        for op in ops:
            for d in op.deps:
                needed[d] = True
        for op in final_ops:
            needed[op.idx] = True
        esem = {e: nc.alloc_semaphore(name="se_" + e) for e in ENGS}
        dsem = {("hw", i): nc.alloc_semaphore(name="sdh_%d" % i) for i in range(NDSEM)}
        dsem.update({("sw", i): nc.alloc_semaphore(name="sds_%d" % i) for i in range(NDSEM)})
        cnt = {e: 0 for e in ENGS}
        for op in ops:
            if op.is_dma:
                op.sig = (dsem[(op.dma_n[0], op.dma_n[1] % NDSEM)], 16 * (op.dma_n[1] // NDSEM + 1))
            elif needed[op.idx]:
                cnt[op.eng] += 1
                op.sig = (esem[op.eng], cnt[op.eng])
        per_eng = {e: [op for op in ops if op.eng == e] for e in ENGS}
        nwait = {e: 0 for e in ENGS}

        def run_engine(ename, eng):
            known = {e: 0 for e in ENGS}
            known_d = {}
            for op in per_eng[ename]:
                waits = {}
                for d in op.deps:
                    p = ops[d]
                    if p.is_dma:
                        k, v = (p.dma_n[0], p.dma_n[1] % NDSEM), p.sig[1]
                        if known_d.get(k, 0) < v:
                            waits[("d", k)] = max(waits.get(("d", k), 0), v)
                    else:
                        if p.eng == ename and ename == "pe":
                            continue
                        v = p.sig[1]
                        if known[p.eng] < v:
                            waits[("e", p.eng)] = max(waits.get(("e", p.eng), 0), v)
                if op.is_dma and op.dma_n[1] >= NDSEM:
                    k, v = (op.dma_n[0], op.dma_n[1] % NDSEM), 16 * (op.dma_n[1] // NDSEM)
                    if known_d.get(k, 0) < v:
                        waits[("d", k)] = max(waits.get(("d", k), 0), v)
                for (kind, k), v in waits.items():
                    if kind == "d":
                        eng.wait_ge(dsem[k], v)
                        known_d[k] = v
                    else:
                        eng.wait_ge(esem[k], v)
                        known[k] = v
                    nwait[ename] += 1
                ins = op.fn(eng)
                if op.sig is not None:
                    ins.then_inc(op.sig[0], 16 if op.is_dma else 1)
            if ename == "sp":
                for op in final_ops:
                    eng.wait_ge(op.sig[0], op.sig[1])

        with nc.Block() as block:
            @block.tensor
            def _(e):
                run_engine("pe", e)

            @block.scalar
            def _(e):
                run_engine("act", e)

            @block.vector
            def _(e):
                run_engine("dve", e)

            @block.gpsimd
            def _(e):
                run_engine("pool", e)

            @block.sync
            def _(e):
                run_engine("sp", e)
        return {e: (len(per_eng[e]), nwait[e]) for e in ENGS}


class T:
    __slots__ = ("ap", "key")

    def __init__(self, ap, key):
        self.ap, self.key = ap, key

    def __getitem__(self, idx):
        return T(self.ap[idx], self.key)

    def k(self, *sub):
        return T(self.ap, (self.key,) + sub)


D = 2048
NTOK = 2048
NPRE = 2048
KC = 16
EPS = 1e-6
OQ, OKC, OVC, OKS, OVS, OKW, OVW, OGN, OGLU, OGM = 0, 2048, 2560, 3072, 3584, 4096, 4608, 5120, 5168, 7216
INC = 11312

CFG = {"phases": "ABCDFGH", "debug": ()}


class Builder:
    def __init__(self):
        self.nc = bass.Bass("TRN2", target_bir_lowering=False)
        self.S = Sched(self.nc)
        self.dram = {}
        self.uid = 0
        self.ps = None

    def din(self, name, shape, dt=F32):
        t = T(self.nc.dram_tensor(name, list(shape), dt, kind="ExternalInput").ap(), name)
        self.dram[name] = t
        return t

    def dscr(self, name, shape, dt, out=False):
        kind = "ExternalOutput" if (out or name in CFG["debug"]) else "Internal"
        t = T(self.nc.dram_tensor(name, list(shape), dt, kind=kind).ap(), name)
        self.dram[name] = t
        return t

    def sb(self, st, name, shape, dt):
        self.uid += 1
        h = st.enter_context(self.nc.sbuf_tensor("%s_%d" % (name, self.uid), list(shape), dt))
        return T(h.ap(), "%s_%d" % (name, self.uid))

    def psum_banks(self):
        self.ps = [T(self.nc.alloc_psum_tensor("psb%d" % i, [128, 512], F32).ap(), "ps%d" % i) for i in range(8)]

    def dma(self, q, out, in_, wkey=None, reads=None):
        self.uid += 1
        wk = out.key if wkey is None else wkey
        return self.S.add(q, lambda e, o=out.ap, i=in_.ap: e.dma_start(out=o, in_=i),
                          reads=[in_.key] if reads is None else reads, writes=[wk], dma=True)

    def mm(self, out, lhsT, rhs, start, stop, sgc=False, reads=()):
        return self.S.add("pe", lambda e, o=out.ap, l=lhsT.ap, r=rhs.ap, s=start, p=stop, g=sgc:
                          e.matmul(o, lhsT=l, rhs=r, start=s, stop=p, skip_group_check=g),
                          reads=[lhsT.key, rhs.key] + list(reads), writes=[out.key])

    def transpose(self, out, in_, ident):
        return self.S.add("pe", lambda e, o=out.ap, i=in_.ap, d=ident.ap: e.transpose(o, i, d),
                          reads=[in_.key, ident.key], writes=[out.key])

    def act(self, out, in_, func, bias=None, scale=1.0, accum=None, reads=()):
        rd = [in_.key] + list(reads)
        kw = {}
        if isinstance(bias, T):
            rd.append(bias.key)
            kw["bias"] = bias.ap
        elif bias is not None:
            kw["bias"] = bias
        if isinstance(scale, T):
            rd.append(scale.key)
            kw["scale"] = scale.ap
        else:
            kw["scale"] = scale
        wr = [out.key]
        if accum is not None:
            kw["accum_out"] = accum.ap
            wr.append(accum.key)
        return self.S.add("act", lambda e, o=out.ap, i=in_.ap, f=func, k=kw: e.activation(out=o, in_=i, func=f, **k),
                          reads=rd, writes=wr)

    def tt(self, eng, out, in0, in1, op, reads=()):
        return self.S.add(eng, lambda e, o=out.ap, a=in0.ap, b=in1.ap, p=op: e.tensor_tensor(out=o, in0=a, in1=b, op=p),
                          reads=[in0.key, in1.key] + list(reads), writes=[out.key])

    def ts(self, eng, out, in0, s1, op0, s2=None, op1=None):
        rd = [in0.key]
        a1 = s1
        if isinstance(s1, T):
            rd.append(s1.key)
            a1 = s1.ap
        a2 = s2
        if isinstance(s2, T):
            rd.append(s2.key)
            a2 = s2.ap
        kw = {} if op1 is None else {"op1": op1}
        return self.S.add(eng, lambda e, o=out.ap, a=in0.ap, x=a1, y=a2, p=op0, k=kw:
                          e.tensor_scalar(out=o, in0=a, scalar1=x, scalar2=y, op0=p, **k),
                          reads=rd, writes=[out.key])

    def stt(self, eng, out, in0, scalar, in1, op0, op1):
        rd = [in0.key, in1.key]
        sc = scalar
        if isinstance(scalar, T):
            rd.append(scalar.key)
            sc = scalar.ap
        return self.S.add(eng, lambda e, o=out.ap, a=in0.ap, s=sc, b=in1.ap, p=op0, q=op1:
                          e.scalar_tensor_tensor(out=o, in0=a, scalar=s, in1=b, op0=p, op1=q),
                          reads=rd, writes=[out.key])

    def copy(self, eng, out, in_, reads=None):
        rd = [in_.key] if reads is None else reads
        if eng == "act":
            return self.S.add("act", lambda e, o=out.ap, i=in_.ap: e.copy(out=o, in_=i), reads=rd, writes=[out.key])
        return self.S.add(eng, lambda e, o=out.ap, i=in_.ap: e.tensor_copy(out=o, in_=i), reads=rd, writes=[out.key])

    def recip(self, out, in_):
        return self.S.add("dve", lambda e, o=out.ap, i=in_.ap: e.reciprocal(out=o, in_=i), reads=[in_.key], writes=[out.key])

    def memset(self, eng, out, val):
        return self.S.add(eng, lambda e, o=out.ap, v=val: e.memset(o, v), writes=[out.key])

    def gelu_tanh(self, out, xs, t1, t2):
        self.tt("pool", t1, xs, xs, ALU.mult)
        self.ts("pool", t1, t1, 0.044715, ALU.mult, 1.0, ALU.add)
        self.tt("pool", t1, t1, xs, ALU.mult)
        self.act(t2, t1, AF.Exp, scale=-1.5957691216)
        self.ts("dve", t2, t2, 1.0, ALU.add)
        self.recip(t2, t2)
        self.tt("pool", out, xs, t2, ALU.mult)

    def rstd_from_psum(self, st_tile, ps, n_feat, width):
        self.act(st_tile, ps, AF.Sqrt, bias=self.eps_t, scale=1.0 / n_feat)
        self.recip(st_tile, st_tile)


def build_program():
    B = Builder()
    nc, S = B.nc, B.S
    ph = CFG["phases"]
    xT = B.din("xT", [128, KC, NPRE + NTOK])
    xtok = B.din("xtok", [NTOK, D])
    c_col = B.din("c_col", [128, KC])
    g1_col = B.din("g1_col", [128, KC])
    g2_col = B.din("g2_col", [128, KC])
    w_ada = B.din("w_ada", [D, 6 * D])
    b_ada = B.din("b_ada", [1, 6 * D])
    w_in = B.din("w_in", [D, INC])
    b_in = B.din("b_in", [1, INC])
    b_in_col = B.din("b_in_col", [128, 96])
    qkg_col = B.din("qkg_col", [128, 4])
    pvalid = B.din("pvalid", [128, 1])
    cmp_w1 = [B.din("cmp_k_w1", [4096, 128]), B.din("cmp_v_w1", [4096, 128])]
    cmp_w2 = [B.din("cmp_k_w2", [128, 128]), B.din("cmp_v_w2", [128, 128])]
    cmp_posT = [B.din("cmp_posT_k", [128, 32]), B.din("cmp_posT_v", [128, 32])]
    ident_in = B.din("ident", [128, 128])
    OVL_in = B.din("OVL", [128, 2, 64])
    SELB_in = B.din("SELB", [NTOK, 64])
    CMPM_in = B.din("CMPM", [128, 2, NTOK])
    WINM_in = B.din("WINM", [4, 128, 8, 512])
    SELC_in = B.din("SELC", [128, 4, 512])
    EX_in = B.din("EX", [64, 32, 128])
    w_nsa_out = B.din("w_nsa_out", [D, D])
    dwT = B.din("dwT", [128, 8, 31])
    cvec = B.din("cvec", [128, 3, 8])
    pwb_col = B.din("pwb_col", [128, 16])
    conv_pw_w = B.din("conv_pw_w", [1024, D])
    w_out = B.din("w_out", [D, D])
    peer_w_q = B.din("peer_w_q", [D, D])
    keysT_in = B.din("keysT", [128, 16, 128])
    peer_uT = B.din("peer_uT", [D, 16384] if "H" in ph else [128, 128])
    peer_v = B.din("peer_v", [16384, D] if "H" in ph else [128, 128])
    out = B.dscr("out", [NTOK, D], F32, out=True)
    modd = B.dscr("modd", [1, 6 * D], F32)
    QT = B.dscr("QT", [16, 128, NTOK], BF16)
    KCT = B.dscr("KCT", [4, 128, NPRE + NTOK], BF16)
    VCT = B.dscr("VCT", [4, 128, NPRE + NTOK], BF16)
    KST = B.dscr("KST", [4, 128, NPRE + NTOK], BF16)
    KWT = B.dscr("KWT", [4, 128, NPRE + NTOK], BF16)
    VS = B.dscr("VS", [NPRE + NTOK, 512], BF16)
    VW = B.dscr("VW", [NPRE + NTOK, 512], BF16)
    GNT = B.dscr("GNT", [48, NTOK], F32)
    UT = B.dscr("UT", [1024, 32 + NTOK], F32)
    GM = B.dscr("GM", [32, 128, NTOK], BF16)
    OT = B.dscr("OT", [16, 128, NTOK], BF16)
    YCG = B.dscr("YCG", [16, 128, NTOK], BF16)
    X1 = B.dscr("X1", [NTOK, D], F32)
    B.psum_banks()
    ps = B.ps
    pst = T(ps[7].ap.bitcast(BF16), "ps7")
    pst6 = T(ps[6].ap.bitcast(BF16), "ps6")
    final_ops = []

    with ExitStack() as gst:
        ones_bf = B.sb(gst, "ones_bf", [128, 128], BF16)
        B.memset("pool", ones_bf, 1.0)
        one_f = B.sb(gst, "one_f", [128, 1], F32)
        B.memset("pool", one_f, 1.0)
        eps_t = B.sb(gst, "eps_t", [128, 1], F32)
        B.memset("pool", eps_t, EPS)
        B.eps_t = eps_t
        modc = B.sb(gst, "modc", [128, 96], F32)
        A1 = B.sb(gst, "A1", [128, KC], F32)
        A2 = B.sb(gst, "A2", [128, KC], F32)
        bcol = B.sb(gst, "bcol", [128, 96], F32)
        B.dma("sp", bcol, b_in_col)
        gcol = B.sb(gst, "gcol", [128, 4], F32)
        B.dma("sp", gcol, qkg_col)
        gqs = B.sb(gst, "gqs", [128, 1], F32)
        B.ts("dve", gqs, gcol[:, 0:1], 128.0 ** -0.5, ALU.mult)
        pv = B.sb(gst, "pv", [128, 1], F32)
        B.dma("sp", pv, pvalid)
        ident = B.sb(gst, "ident", [128, 128], BF16)
        B.dma("pool", ident, ident_in)
        ones_f = B.sb(gst, "ones_f", [128, 128], F32)
        B.memset("pool", ones_f, 1.0)
        kcmpT = B.sb(gst, "kcmpT", [128, 4, 256], BF16)
        vcmp = B.sb(gst, "vcmp", [128, 4, 2, 128], BF16)

        if "A" in ph:
            with ExitStack() as st:
                cc = B.sb(st, "cc", [128, KC], F32)
                B.dma("sp", cc, c_col)
                sc = B.sb(st, "sc", [128, KC], F32)
                B.act(sc, cc, AF.Silu)
                brow = B.sb(st, "brow", [1, 6 * D], F32)
                B.dma("sp", brow, b_ada)
                mrow = B.sb(st, "mrow", [1, 6 * D], F32)
                wa = [B.sb(st, "wa%d" % i, [128, KC, 512], F32) for i in range(2)]
                for n in range(24):
                    w = wa[n % 2]
                    B.dma("sp" if n % 2 == 0 else "act", w, T(w_ada.ap[:, n * 512:(n + 1) * 512].rearrange("(kc p) n -> p kc n", p=128), "w_ada"))
                    pt = ps[n % 2]
                    for kc in range(KC):
                        B.mm(pt[0:1, :], sc[:, kc:kc + 1], w[:, kc, :], kc == 0, kc == KC - 1)
                    B.tt("dve", mrow[0:1, n * 512:(n + 1) * 512].k(n), pt[0:1, :], brow[0:1, n * 512:(n + 1) * 512], ALU.add)
                    for j in range(4):
                        jj = n * 4 + j
                        B.mm(ps[2][:, jj:jj + 1].k(jj), mrow[0:1, jj * 128:(jj + 1) * 128].k(n), one_f[0:1, 0:1], True, True)
                B.copy("dve", modc, T(ps[2].ap[:, 0:96], ps[2].key), reads=[(ps[2].key, jj) for jj in range(96)])
                B.dma("sp", modd, mrow, reads=[(mrow.key, n) for n in range(24)])
                g1 = B.sb(st, "g1", [128, KC], F32)
                B.dma("sp", g1, g1_col)
                B.stt("dve", A1, modc[:, 16:32], 1.0, g1, ALU.add, ALU.mult)
                g2c = B.sb(st, "g2c", [128, KC], F32)
                B.dma("sp", g2c, g2_col)
                B.stt("dve", A2, modc[:, 64:80], 1.0, g2c, ALU.add, ALU.mult)
            S.fence()

        def proj_pass(st, tok0, own):
            hT = B.sb(st, "hT", [128, KC, 2048], BF16)
            with ExitStack() as s1:
                xs = [B.sb(s1, "xs%d" % i, [128, KC, 256], F32) for i in range(2)]
                sq = [B.sb(s1, "sq%d" % i, [128, KC, 256], BF16) for i in range(2)]
                rs = [B.sb(s1, "rs%d" % i, [128, 256], F32) for i in range(2)]
                tm = [B.sb(s1, "tm%d" % i, [128, 256], F32) for i in range(2)]
                for sg in range(8):
                    x_, q_, r_ = xs[sg % 2], sq[sg % 2], rs[sg % 2]
                    c0 = tok0 + sg * 256
                    B.dma("sp", x_, T(xT.ap[:, :, c0:c0 + 256], "xT"))
                    B.act(q_, x_, AF.Square)
                    pt = ps[sg % 2]
                    for kc in range(KC):
                        B.mm(pt[:, 0:256], ones_bf, q_[:, kc, :], kc == 0, kc == KC - 1)
                    B.rstd_from_psum(r_, pt[:, 0:256], D, 256)
                    for kc in range(KC):
                        t_ = tm[kc % 2]
                        B.stt("dve", t_, x_[:, kc, :], A1[:, kc:kc + 1], r_, ALU.mult, ALU.mult)
                        B.act(hT[:, kc, sg * 256:(sg + 1) * 256].k(kc, sg // 2), t_, AF.Identity, bias=modc[:, kc:kc + 1])
            S.fence()
            if "HTD" in CFG["debug"] and own:
                htd = B.dscr("HTD", [128, KC, 2048], BF16)
                B.dma("sp", htd, hT, reads=[(hT.key, kc, g_) for kc in range(KC) for g_ in range(4)])
            wb = [B.sb(st, "wb%d" % i, [128, KC, 512], BF16) for i in range(3)]
            wctr = [0]

            def load_w(c0, cw):
                w = wb[wctr[0] % 3]
                wctr[0] += 1
                B.dma("pool", w[:, :, 0:cw], T(w_in.ap[:, c0:c0 + cw].rearrange("(kc p) n -> p kc n", p=128), "w_in"))
                return w

            pctr = [0]

            def fm_matmul(w, m0, mw, grp):
                pt = ps[pctr[0] % 4]
                pctr[0] += 1
                for kc in range(KC):
                    B.mm(pt[0:mw, :], w[:, kc, m0:m0 + mw], hT[:, kc, grp * 512:(grp + 1) * 512].k(kc, grp), kc == 0, kc == KC - 1)
                return pt

            ob = [B.sb(st, "ob%d" % i, [128, 512], BF16) for i in range(3)]
            of = [B.sb(st, "of%d" % i, [128, 512], F32) for i in range(3)]
            sqb = [B.sb(st, "sqb%d" % i, [128, 512], BF16) for i in range(2)]
            rq = [B.sb(st, "rq%d" % i, [128, 512], F32) for i in range(2)]
            octr = [0]

            def normed_seg(c0, nchunk, bcol0, gsc, dst, dst_tok0, grps=(0, 1, 2, 3)):
                for c4 in range(0, nchunk, 4):
                    w = load_w(c0 + c4 * 128, 512)
                    for m in range(4):
                        ch = c4 + m
                        for grp in grps:
                            pt = fm_matmul(w, m * 128, 128, grp)
                            i = octr[0]
                            octr[0] += 1
                            qf, sq_, r_, o_ = of[i % 3], sqb[i % 2], rq[i % 2], ob[i % 3]
                            bc_ = bcol[:, bcol0 + ch:bcol0 + ch + 1]
                            B.act(qf, pt, AF.Identity, bias=bc_)
                            B.act(sq_, pt, AF.Square, bias=bc_)
                            p2 = ps[4 + i % 2]
                            B.mm(p2, ones_bf, sq_, True, True)
                            B.rstd_from_psum(r_, p2, 128, 512)
                            B.stt("dve", o_, qf, gsc, r_, ALU.mult, ALU.mult)
                            B.dma("sp", T(dst.ap[ch, :, dst_tok0 + grp * 512:dst_tok0 + (grp + 1) * 512], dst.key), o_, wkey=(dst.key, ch, grp, tok0))

            def plain_seg(c0, nchunk, bcol0, dst, dst_tok0):
                for c4 in range(0, nchunk, 4):
                    w = load_w(c0 + c4 * 128, 512)
                    for m in range(4):
                        ch = c4 + m
                        for grp in range(4):
                            pt = fm_matmul(w, m * 128, 128, grp)
                            i = octr[0]
                            octr[0] += 1
                            o_ = ob[i % 3]
                            B.act(o_, pt, AF.Identity, bias=bcol[:, bcol0 + ch:bcol0 + ch + 1])
                            B.dma("sp", T(dst.ap[ch, :, dst_tok0 + grp * 512:dst_tok0 + (grp + 1) * 512], dst.key), o_, wkey=(dst.key, ch, grp, tok0))

            def tokmajor_seg(c0, dst, dst_tok0, brow_b, tiles=range(16)):
                w = load_w(c0, 512)
                for tt_ in tiles:
                    pt = ps[pctr[0] % 4]
                    pctr[0] += 1
                    for kc in range(KC):
                        B.mm(pt, hT[:, kc, tt_ * 128:(tt_ + 1) * 128].k(kc, tt_ // 4), w[:, kc, :], kc == 0, kc == KC - 1)
                    i = octr[0]
                    octr[0] += 1
                    o_ = ob[i % 3]
                    B.tt("dve", o_, pt, brow_b, ALU.add)
                    r0 = dst_tok0 + tt_ * 128
                    B.dma("sp", T(dst.ap[r0:r0 + 128, :], dst.key), o_, wkey=(dst.key, tt_, tok0))

            def glu_seg(grps, dst_c0, keep_last32):
                for half in range(2):
                    wu = load_w(OGLU + half * 512, 512)
                    wv = load_w(OGLU + 1024 + half * 512, 512)
                    for m in range(4):
                        ch = half * 4 + m
                        for grp in grps:
                            pu = fm_matmul(wu, m * 128, 128, grp)
                            pvv = fm_matmul(wv, m * 128, 128, grp)
                            i = octr[0]
                            octr[0] += 1
                            sg_, o_ = of[i % 3], rq[i % 2]
                            B.act(sg_, pvv, AF.Sigmoid, bias=bcol[:, 56 + ch:57 + ch])
                            B.stt("dve", o_, pu, bcol[:, 48 + ch:49 + ch], sg_, ALU.add, ALU.mult)
                            if keep_last32:
                                B.ts("dve", o_[:, 480:512], o_[:, 480:512], pv[:, 0:1], ALU.mult)
                                B.dma("sp", T(UT.ap[ch * 128:(ch + 1) * 128, 0:32], "UT"), o_[:, 480:512], wkey=("UT", ch, "pre"))
                            else:
                                c_ = 32 + grp * 512
                                B.dma("sp", T(UT.ap[ch * 128:(ch + 1) * 128, c_:c_ + 512], "UT"), o_, wkey=("UT", ch, grp))

            bvs = B.sb(st, "bvs", [128, 512], F32)
            bvw = B.sb(st, "bvw", [128, 512], F32)
            B.dma("sp", bvs, T(b_in.ap[0:1, OVS:OVS + 512].partition_broadcast(128), "b_in"))
            B.dma("sp", bvw, T(b_in.ap[0:1, OVW:OVW + 512].partition_broadcast(128), "b_in"))
            plain_seg(OKC, 4, 16, KCT, tok0)
            if CFG.get("bquick"):
                return
            plain_seg(OVC, 4, 20, VCT, tok0)
            normed_seg(OKS, 4, 24, gcol[:, 2:3], KST, tok0)
            normed_seg(OKW, 4, 28, gcol[:, 3:4], KWT, tok0, grps=(0, 1, 2, 3) if own else (3,))
            tokmajor_seg(OVS, VS, tok0, bvs)
            tokmajor_seg(OVW, VW, tok0, bvw, tiles=range(16) if own else range(12, 16))
            if not own:
                glu_seg([3], 0, True)
            else:
                normed_seg(OQ, 16, 0, gqs[:, 0:1], QT, 0)
                glu_seg([0, 1, 2, 3], 32, False)
                w = load_w(OGN, 48)
                for grp in range(4):
                    pt = fm_matmul(w, 0, 48, grp)
                    i = octr[0]
                    octr[0] += 1
                    o_ = of[i % 3]
                    B.act(o_[0:48, :], pt[0:48, :], AF.Sigmoid, bias=bcol[0:48, 32:33])
                    B.dma("sp", T(GNT.ap[:, grp * 512:(grp + 1) * 512], "GNT"), o_[0:48, :], wkey=("GNT", grp))
                for c4 in range(0, 32, 4):
                    w = load_w(OGM + c4 * 128, 512)
                    for m in range(4):
                        ch = c4 + m
                        for grp in range(4):
                            pt = fm_matmul(w, m * 128, 128, grp)
                            i = octr[0]
                            octr[0] += 1
                            o_ = ob[i % 3]
                            B.act(o_, pt, AF.Sigmoid, bias=bcol[:, 64 + ch:65 + ch])
                            B.dma("sp", T(GM.ap[ch, :, grp * 512:(grp + 1) * 512], "GM"), o_, wkey=("GM", ch, grp))

        if "B" in ph:
            with ExitStack() as st:
                proj_pass(st, 0, False)
            S.fence()
            with ExitStack() as st:
                proj_pass(st, NPRE, True)
            S.fence()


        if "C" in ph:
            with ExitStack() as st:
                for kv in range(2):
                    w1 = B.sb(st, "w1_%d" % kv, [128, 32, 128], BF16)
                    B.dma("pool", w1, T(cmp_w1[kv].ap.rearrange("(l d) h -> d l h", d=128), "cw1"))
                    w2 = B.sb(st, "w2_%d" % kv, [128, 128], BF16)
                    B.dma("pool", w2, cmp_w2[kv])
                    posT = B.sb(st, "posT_%d" % kv, [128, 32], BF16)
                    B.dma("pool", posT, cmp_posT[kv])
                    posb = B.sb(st, "posb_%d" % kv, [128, 1], F32)
                    for l in range(32):
                        B.mm(ps[6][:, 0:1], w1[:, l, :], posT[:, l:l + 1], l == 0, l == 31)
                    B.copy("dve", posb, ps[6][:, 0:1])
                    src = KCT if kv == 0 else VCT
                    for g in range(4):
                        xt_ = B.sb(st, "cx_%d_%d" % (kv, g), [128, NPRE + NTOK], BF16)
                        B.dma("sp", xt_, T(src.ap[g], src.key))
                        pt = ps[g % 2]
                        for l in range(32):
                            B.mm(pt[:, 0:255], w1[:, l, :], T(xt_.ap[:, l:l + 16 * 254 + 1:16], xt_.key), l == 0, l == 31)
                        hid = B.sb(st, "hid_%d_%d" % (kv, g), [128, 256], BF16)
                        hx = B.sb(st, "hx_%d_%d" % (kv, g), [128, 256], F32)
                        h1 = B.sb(st, "h1_%d_%d" % (kv, g), [128, 256], F32)
                        h2_ = B.sb(st, "h2_%d_%d" % (kv, g), [128, 256], F32)
                        B.act(hx[:, 0:255], pt[:, 0:255], AF.Identity, bias=posb)
                        B.gelu_tanh(hid[:, 0:255], hx[:, 0:255], h1[:, 0:255], h2_[:, 0:255])
                        if kv == 0:
                            p2 = ps[2 + g % 2]
                            B.mm(p2[:, 0:255], w2, hid[:, 0:255], True, True)
                            sq_ = B.sb(st, "csq_%d" % g, [128, 256], BF16)
                            B.act(sq_[:, 0:255], p2[:, 0:255], AF.Square)
                            p3 = ps[4 + g % 2]
                            B.mm(p3[:, 0:255], ones_bf, sq_[:, 0:255], True, True)
                            r_ = B.sb(st, "crs_%d" % g, [128, 256], F32)
                            B.rstd_from_psum(r_[:, 0:255], p3[:, 0:255], 128, 255)
                            B.stt("dve", kcmpT[:, g, 0:255].k(g), p2[:, 0:255], gcol[:, 1:2], r_[:, 0:255], ALU.mult, ALU.mult)
                        else:
                            for c in range(2):
                                rows = 128 if c == 0 else 127
                                p2 = ps[2 + c]
                                B.mm(p2[0:rows, 0:128], hid[:, c * 128:c * 128 + rows], w2, True, True)
                                B.copy("dve", vcmp[0:rows, g, c, :].k(g, c), p2[0:rows, 0:128])
                if "KCMPD" in CFG["debug"]:
                    B.dma("sp", B.dscr("KCMPD", [128, 4, 256], BF16), kcmpT, reads=[(kcmpT.key, g) for g in range(4)])
                    B.dma("sp", B.dscr("VCMPD", [128, 4, 2, 128], BF16), vcmp, reads=[(vcmp.key, g, c) for g in range(4) for c in range(2)])
            S.fence()

        if "D" in ph:
            with ExitStack() as st:
                OVL = B.sb(st, "OVL", [128, 2, 64], BF16)
                B.dma("pool", OVL, OVL_in)
                SELC = B.sb(st, "SELC", [128, 4, 512], BF16)
                B.dma("pool", SELC, SELC_in)
                EX = B.sb(st, "EX", [64, 32, 128], BF16)
                B.dma("pool", EX, EX_in)
                KS_g = B.sb(st, "KS_g", [128, NPRE + NTOK], BF16)
                KW_g = B.sb(st, "KW_g", [128, NPRE + NTOK], BF16)
                VS_g = B.sb(st, "VS_g", [128, 32, 128], BF16)
                VW_g = B.sb(st, "VW_g", [128, 32, 128], BF16)
                BMs = B.sb(st, "BMs", [128, 32, 512], BF16)
                q4s = [B.sb(st, "q4_%d" % i, [128, 4, 512], BF16) for i in range(2)]
                gbs = B.sb(st, "gbs", [128, 12, 512], F32)
                WINM = B.sb(st, "WINM", [128, 8, 512], BF16)
                CMPM = B.sb(st, "CMPM", [128, 2, 512], BF16)
                SELB = B.sb(st, "SELB", [128, 4, 64], F32)
                oacc = B.sb(st, "oacc", [128, 4, 512], F32)
                Ef = [B.sb(st, "Ef%d" % i, [128, 512], F32) for i in range(2)]
                Eb = [B.sb(st, "Eb%d" % i, [128, 512], BF16) for i in range(6)]
                Pb = [B.sb(st, "Pb%d" % i, [128, 512], BF16) for i in range(6)]
                Em2 = [[B.sb(st, "Em%d_%d" % (k, i), [128, 512], BF16) for i in range(2)] for k in range(2)]
                Pn2 = [[B.sb(st, "Pn%d_%d" % (k, i), [128, 512], BF16) for i in range(2)] for k in range(2)]
                rden = B.sb(st, "rden", [128, 512], F32)
                wgt = B.sb(st, "wgt", [128, 512], F32)
                tmpo = B.sb(st, "tmpo", [128, 512], F32)
                ob_ = [B.sb(st, "obD%d" % i, [128, 512], BF16) for i in range(2)]
                sc = B.sb(st, "sc", [128, 64], F32)
                sc2 = B.sb(st, "sc2", [128, 64], F32)
                m8a = B.sb(st, "m8a", [128, 8], F32)
                m8b = B.sb(st, "m8b", [128, 8], F32)
                thr = B.sb(st, "thr", [128, 1], F32)
                maskf = B.sb(st, "maskf", [128, 64], BF16)
                maskT = B.sb(st, "maskT", [64, 512], BF16)
                PS_S = [ps[0], ps[1]]
                PS_ON, PS_DEN, PS_IMP, PS_BM = ps[2], ps[3], ps[4], ps[5]
                ectr = [0]

                bctr = [0]
                PS_S3 = [ps[0], ps[1], ps[6]]

                def branch_epilogue(hh, row, first, pON, pDEN):
                    steps = [lambda: B.ts("dve", rden, pDEN, 1e-30, ALU.max)]
                    for qq in range(4):
                        steps.append(lambda qq=qq: B.recip(rden[:, qq * 128:(qq + 1) * 128], rden[:, qq * 128:(qq + 1) * 128]))
                    steps.append(lambda: B.tt("dve", wgt, rden, gbs[:, row, :].k(row), ALU.mult))
                    steps.append(lambda: B.tt("dve", tmpo, pON, wgt, ALU.mult))
                    steps.append(lambda: B.tt("pool", oacc[:, hh, :].k(hh), oacc[:, hh, :].k(hh), tmpo, ALU.add))
                    return steps

                for g in range(4):
                    B.dma("sp", KS_g, T(KST.ap[g], "KST"))
                    B.dma("sp", KW_g[:, 1536:NPRE + NTOK], T(KWT.ap[g, :, 1536:NPRE + NTOK], "KWT"))
                    B.dma("sp", VS_g, T(VS.ap[:, g * 128:(g + 1) * 128].rearrange("(c p) d -> p c d", p=128), "VS"))
                    B.dma("sp", VW_g[:, 12:32, :], T(VW.ap[1536:NPRE + NTOK, g * 128:(g + 1) * 128].rearrange("(c p) d -> p c d", p=128), "VW"))
                    for qt in range(4):
                        u0 = qt * 512
                        q4 = q4s[qt % 2]
                        B.dma("sp", q4, T(QT.ap[4 * g:4 * g + 4, :, u0:u0 + 512].rearrange("h d t -> d h t"), "QT"))
                        for r in range(12):
                            row = g * 12 + r
                            B.dma("sp", gbs[:, r, :].k(r), T(GNT.ap[row:row + 1, u0:u0 + 512].partition_broadcast(128), "GNT"))
                        B.dma("pool", WINM, T(WINM_in.ap[qt], "WINM_in"))
                        B.dma("pool", CMPM, T(CMPM_in.ap[:, :, u0:u0 + 512], "CMPM_in"))
                        B.dma("sp", SELB, T(SELB_in.ap[u0:u0 + 512, :].rearrange("(a p) j -> p a j", p=128), "SELB_in"))
                        nch = (NPRE + u0 + 512) // 128
                        for hh in range(4):
                            Em, Pn = Em2[hh % 2], Pn2[hh % 2]
                            for c in range(2):
                                rows = 128 if c == 0 else 127
                                pS = PS_S[ectr[0] % 2]
                                ef = Ef[ectr[0] % 2]
                                ectr[0] += 1
                                B.mm(pS[0:rows, :], kcmpT[:, g, c * 128:c * 128 + rows].k(g), q4[:, hh, :], True, True)
                                B.act(ef[0:rows, :], pS[0:rows, :], AF.Exp)
                                B.tt("dve", Em[c][0:rows, :], ef[0:rows, :], CMPM[0:rows, c, :], ALU.mult)
                            for c in range(2):
                                rows = 128 if c == 0 else 127
                                B.mm(PS_DEN, ones_bf[0:rows, :], Em[c][0:rows, :], c == 0, c == 1)
                            B.ts("dve", rden, PS_DEN, 1e-30, ALU.max)
                            B.recip(rden, rden)
                            for c in range(2):
                                rows = 128 if c == 0 else 127
                                B.tt("dve", Pn[c][0:rows, :], Em[c][0:rows, :], rden[0:rows, :], ALU.mult)
                            for c in range(2):
                                rows = 128 if c == 0 else 127
                                B.mm(PS_ON, vcmp[0:rows, g, c, :].k(g, c), Pn[c][0:rows, :], c == 0, c == 1)
                            for ut in range(4):
                                for c in range(2):
                                    rows = 128 if c == 0 else 127
                                    B.mm(PS_IMP[:, ut * 64:(ut + 1) * 64].k(ut), Pn[c][0:rows, ut * 128:(ut + 1) * 128], OVL[0:rows, c, :],
                                         hh == 0 and c == 0 and ut == 0, hh == 3 and c == 1, sgc=True)
                            B.tt("dve", oacc[:, hh, :].k(hh), PS_ON, gbs[:, hh * 3 + 0, :].k(hh * 3), ALU.mult)
                        for ut in range(4):
                            B.tt("dve", sc, PS_IMP[:, ut * 64:(ut + 1) * 64].k(ut), SELB[:, ut, :], ALU.add,
                                 reads=[(PS_IMP.key, u_) for u_ in range(4)])
                            S.add("dve", lambda e, o=m8a.ap, i=sc.ap: e.max(out=o, in_=i), reads=[sc.key], writes=[m8a.key])
                            S.add("dve", lambda e, o=sc2.ap, r=m8a.ap, i=sc.ap: e.match_replace(out=o, in_to_replace=r, in_values=i, imm_value=-3.0e38),
                                  reads=[sc.key, m8a.key], writes=[sc2.key])
                            S.add("dve", lambda e, o=m8b.ap, i=sc2.ap: e.max(out=o, in_=i), reads=[sc2.key], writes=[m8b.key])
                            B.ts("dve", thr, m8b[:, 7:8], -1.0e29, ALU.max)
                            B.ts("dve", maskf, sc, thr[:, 0:1], ALU.is_ge)
                            B.transpose(pst[0:64, ut * 128:(ut + 1) * 128], maskf, ident)
                            B.copy("act", maskT[:, ut * 128:(ut + 1) * 128], pst[0:64, ut * 128:(ut + 1) * 128])
                        for c in range(nch):
                            pbm = PS_BM if c % 2 == 0 else ps[6]
                            B.mm(pbm, EX[:, c, :], maskT, True, True)
                            di = c - (nch - 4)
                            if di >= 0:
                                B.tt("dve", BMs[:, c, :].k(c), pbm, SELC[:, di, :], ALU.mult)
                            else:
                                B.copy("act", BMs[:, c, :].k(c), pbm)
                        LAG = 3
                        pend_ep = []
                        for hh in range(4):
                            for br in (1, 2):
                                if br == 1:
                                    chunks = [(c, KS_g, VS_g, BMs[:, c, :].k(c)) for c in range(nch)]
                                else:
                                    c0 = (NPRE + u0 - 512) // 128
                                    chunks = [(c0 + i, KW_g, VW_g, WINM[:, i, :]) for i in range(8)]
                                bctr[0] += 1
                                pON, pDEN = (ps[2], ps[3]) if bctr[0] % 2 == 0 else (ps[4], ps[5])
                                n = len(chunks)
                                pbs = {}
                                for ci in range(n + LAG):
                                    if ci < n:
                                        c, Kg, Vg, msk = chunks[ci]
                                        pS = PS_S3[ectr[0] % 3]
                                        eb, pb = Eb[ectr[0] % 6], Pb[ectr[0] % 6]
                                        ectr[0] += 1
                                        B.mm(pS, Kg[:, c * 128:(c + 1) * 128], q4[:, hh, :], True, True)
                                        B.act(eb, pS, AF.Exp)
                                        B.tt("dve", pb, eb, msk, ALU.mult)
                                        pbs[ci] = pb
                                        if pend_ep and ci >= 1:
                                            pend_ep.pop(0)()
                                    cj_ = ci - LAG
                                    if cj_ >= 0:
                                        c, Kg, Vg, msk = chunks[cj_]
                                        B.mm(pON, Vg[:, c, :], pbs[cj_], cj_ == 0, cj_ == n - 1)
                                        B.mm(pDEN, ones_bf, pbs[cj_], cj_ == 0, cj_ == n - 1)
                                for fn in pend_ep:
                                    fn()
                                pend_ep = branch_epilogue(hh, hh * 3 + br, False, pON, pDEN)
                                if br == 2:
                                    o_ = ob_[hh % 2]
                                    pend_ep.append(lambda o_=o_, hh=hh: B.copy("act", o_, oacc[:, hh, :].k(hh)))
                                    pend_ep.append(lambda o_=o_, hh=hh: B.dma("sp", T(OT.ap[4 * g + hh, :, u0:u0 + 512], "OT"), o_, wkey=("OT", g, hh, qt)))
                            if hh == 3:
                                for fn in pend_ep:
                                    fn()
                                pend_ep = []
            S.fence()

        if "F" in ph:
            with ExitStack() as st:
                dw = B.sb(st, "dw", [128, 8, 31], F32)
                B.dma("sp", dw, dwT)
                cv = B.sb(st, "cv", [128, 3, 8], F32)
                B.dma("sp", cv, cvec)
                pwb = B.sb(st, "pwb", [128, 16], F32)
                B.dma("sp", pwb, pwb_col)
                pw = B.sb(st, "pw", [128, 8, D], BF16)
                for cg in range(4):
                    B.dma("pool", pw[:, :, cg * 512:(cg + 1) * 512].k(cg), T(conv_pw_w.ap[:, cg * 512:(cg + 1) * 512].rearrange("(c p) n -> p c n", p=128), "pww"))
                HT = 1024
                Y = B.sb(st, "Y", [128, 8, HT], F32)
                ycT = B.sb(st, "ycT", [128, 8, HT], BF16)
                ysq = [B.sb(st, "ysq%d" % i, [128, 512], F32) for i in range(2)]
                mean = B.sb(st, "mean", [128, 512], F32)
                msq = B.sb(st, "msq", [128, 512], F32)
                rstd = B.sb(st, "rstdF", [128, 512], F32)
                zt = [B.sb(st, "zt%d" % i, [128, 512], F32) for i in range(2)]
                gm1 = [B.sb(st, "gm1_%d" % i, [128, 512], BF16) for i in range(2)]
                yo = [B.sb(st, "yo%d" % i, [128, 512], BF16) for i in range(2)]
                Dg = B.sb(st, "Dg", [128, 8, 31, 128], BF16)
                for cj in range(8):
                    for k in range(31):
                        B.ts("dve" if k % 2 == 0 else "pool", Dg[:, cj, k, :].k(cj, k), ident, dw[:, cj, k:k + 1], ALU.mult)
                Ub = [B.sb(st, "Ub%d" % i, [128, HT + 32], BF16) for i in range(2)]
                for th in range(2):
                    t0 = th * HT
                    for cj in range(8):
                        U = Ub[cj % 2]
                        B.dma("pool", U, T(UT.ap[cj * 128:(cj + 1) * 128, t0:t0 + HT + 32], "UT"))
                        for grp in range(2):
                            pt = ps[4 + (cj * 2 + grp) % 4]
                            for k in range(31):
                                o = 2 + k + grp * 512
                                B.mm(pt, Dg[:, cj, k, :].k(cj, k), U[:, o:o + 512], k == 0, k == 30)
                            B.act(Y[:, cj, grp * 512:(grp + 1) * 512].k(cj), pt, AF.Identity, bias=cv[:, 0, cj:cj + 1])
                    for grp in range(2):
                        cs = slice(grp * 512, (grp + 1) * 512)
                        p1, p2 = ps[0], ps[1]
                        for cj in range(8):
                            B.mm(p1, ones_f, Y[:, cj, cs].k(cj), cj == 0, cj == 7)
                        for cj in range(8):
                            q_ = ysq[cj % 2]
                            B.act(q_, Y[:, cj, cs].k(cj), AF.Square)
                            B.mm(p2, ones_f, q_, cj == 0, cj == 7)
                        B.ts("dve", mean, p1, 1.0 / 1024, ALU.mult)
                        B.tt("dve", msq, mean, mean, ALU.mult)
                        B.stt("dve", rstd, p2, 1.0 / 1024, msq, ALU.mult, ALU.subtract)
                        B.act(rstd, rstd, AF.Sqrt, bias=eps_t, scale=1.0)
                        B.recip(rstd, rstd)
                        for cj in range(8):
                            z_ = zt[cj % 2]
                            B.tt("dve", z_, Y[:, cj, cs].k(cj), mean, ALU.subtract)
                            B.tt("dve", z_, z_, rstd, ALU.mult)
                            B.act(ycT[:, cj, cs].k(cj, grp), z_, AF.Silu, bias=cv[:, 2, cj:cj + 1], scale=cv[:, 1, cj:cj + 1])
                        for j in range(16):
                            pt = ps[2 + j % 2]
                            for cj in range(8):
                                B.mm(pt, pw[:, cj, j * 128:(j + 1) * 128].k(j // 4), ycT[:, cj, cs].k(cj, grp), cj == 0, cj == 7)
                            g_ = gm1[j % 2]
                            c0 = t0 + grp * 512
                            B.dma("sp", g_, T(GM.ap[16 + j, :, c0:c0 + 512], "GM"))
                            o_ = yo[j % 2]
                            B.stt("dve", o_, pt, pwb[:, j:j + 1], g_, ALU.add, ALU.mult)
                            B.dma("sp", T(YCG.ap[j, :, c0:c0 + 512], "YCG"), o_, wkey=("YCG", j, th, grp))
            S.fence()

        if "G" in ph:
            with ExitStack() as st:
                g1b = B.sb(st, "g1b", [128, D], F32)
                B.dma("sp", g1b, T(modd.ap[0:1, 2 * D:3 * D].partition_broadcast(128), "modd"))
                HT = 1024
                oT = B.sb(st, "oT", [128, 16, HT], BF16)
                mixT = B.sb(st, "mixT", [128, 16, HT], BF16)
                wbs = [B.sb(st, "wg%d" % i, [128, KC, 512], BF16) for i in range(3)]
                gm0 = [B.sb(st, "gm0_%d" % i, [128, HT], BF16) for i in range(2)]
                ycg = [B.sb(st, "ycg_%d" % i, [128, HT], BF16) for i in range(2)]
                t1 = [B.sb(st, "t1_%d" % i, [128, 512], F32) for i in range(2)]
                xt_ = [B.sb(st, "xg_%d" % i, [128, 512], F32) for i in range(3)]
                xo_ = [B.sb(st, "xo_%d" % i, [128, 512], F32) for i in range(3)]
                wc = [0]
                for th in range(2):
                    t0 = th * HT
                    B.dma("sp", oT, T(OT.ap[:, :, t0:t0 + HT].rearrange("h d t -> d h t"), "OT"))
                    for cg in range(4):
                        w = wbs[wc[0] % 3]
                        wc[0] += 1
                        B.dma("pool", w, T(w_nsa_out.ap[:, cg * 512:(cg + 1) * 512].rearrange("(kc p) n -> p kc n", p=128), "wno"))
                        for m in range(4):
                            j = cg * 4 + m
                            g_, y_ = gm0[j % 2], ycg[j % 2]
                            B.dma("sp", g_, T(GM.ap[j, :, t0:t0 + HT], "GM"))
                            B.dma("sp", y_, T(YCG.ap[j, :, t0:t0 + HT], "YCG"))
                            for grp in range(2):
                                cs = slice(grp * 512, (grp + 1) * 512)
                                pt = ps[(j * 2 + grp) % 4]
                                for h in range(16):
                                    B.mm(pt, w[:, h, m * 128:(m + 1) * 128], oT[:, h, cs], h == 0, h == 15)
                                t_ = t1[grp]
                                B.tt("dve", t_, pt, g_[:, cs], ALU.mult)
                                B.tt("pool", mixT[:, j, cs].k(j, grp), t_, y_[:, cs], ALU.add)
                    for cg in range(4):
                        w = wbs[wc[0] % 3]
                        wc[0] += 1
                        B.dma("pool", w, T(w_out.ap[:, cg * 512:(cg + 1) * 512].rearrange("(kc p) n -> p kc n", p=128), "wout"))
                        for tt_ in range(8):
                            i = cg * 8 + tt_
                            pt = ps[4 + i % 3]
                            for kc in range(KC):
                                B.mm(pt, mixT[:, kc, tt_ * 128:(tt_ + 1) * 128].k(kc, tt_ // 4), w[:, kc, :], kc == 0, kc == KC - 1)
                            x_, o_ = xt_[i % 3], xo_[i % 3]
                            r0 = t0 + tt_ * 128
                            B.dma("sp", x_, T(xtok.ap[r0:r0 + 128, cg * 512:(cg + 1) * 512], "xtok"))
                            B.tt("dve", o_, pt, g1b[:, cg * 512:(cg + 1) * 512], ALU.mult)
                            B.tt("pool", o_, o_, x_, ALU.add)
                            B.dma("sp", T(X1.ap[r0:r0 + 128, cg * 512:(cg + 1) * 512], "X1"), o_, wkey=("X1", th, cg, tt_))
            S.fence()

        if "H" in ph:
            with ExitStack() as st:
                NT = 512
                ntile = NT // 128
                g2b = B.sb(st, "g2b", [128, D], F32)
                B.dma("sp", g2b, T(modd.ap[0:1, 5 * D:6 * D].partition_broadcast(128), "modd"))
                B.ts("dve", g2b, g2b, 0.5, ALU.mult)
                h2T = B.sb(st, "h2T", [128, KC, NT], BF16)
                acc = B.sb(st, "acc", [128, ntile, D], F32)
                tau = B.sb(st, "tau", [128, ntile, 8], F32)
                negb = B.sb(st, "negb", [128, ntile, 8], F32)
                rz = B.sb(st, "rz", [128, ntile, 8], F32)
                wbs = [B.sb(st, "wh%d" % i, [128, KC, 512], BF16) for i in range(2)]
                Vb = [B.sb(st, "Vb%d" % i, [128, 4, D], BF16) for i in range(2)]
                x1t = B.sb(st, "x1t", [128, D], F32)
                h2f = B.sb(st, "h2f", [128, D], F32)
                h2b = B.sb(st, "h2b", [128, D], BF16)
                ssq = B.sb(st, "ssq", [128, 1], F32)
                sall4 = B.sb(st, "sall4", [128, ntile, 16, 128], F32)
                gxb = [T(h2f.ap[:, 0:512], (h2f.key, "gx0")), T(h2f.ap[:, 512:1024], (h2f.key, "gx1"))]
                g1b_ = [T(h2f.ap[:, 1024:1536], (h2f.key, "g10")), T(h2f.ap[:, 1536:2048], (h2f.key, "g11"))]
                wcnt = [0]

                def vmax(o, i):
                    S.add("dve", lambda e, o=o.ap, i=i.ap: e.max(out=o, in_=i), reads=[i.key], writes=[o.key])

                def vmr(o, r, i):
                    S.add("dve", lambda e, o=o.ap, r=r.ap, i=i.ap: e.match_replace(out=o, in_to_replace=r, in_values=i, imm_value=-3.0e38),
                          reads=[r.key, i.key], writes=[o.key])

                for tg in range(CFG.get('h_tg', NTOK // NT)):
                    for tt_ in range(ntile):
                        for dc in range(4):
                            B.memset("pool", T(acc.ap[:, tt_, dc * 512:(dc + 1) * 512], (acc.key, tt_, dc)), 0.0)
                    with ExitStack() as s1:
                        keysT = B.sb(s1, "keysT", [128, 16, 128], BF16)
                        B.dma("pool", keysT, keysT_in)
                        qT = B.sb(s1, "qT", [128, 16, NT], BF16)
                        s2 = B.sb(s1, "s2", [128, 128], F32)
                        tv = B.sb(s1, "tv", [128, 16, 16], F32)
                        cand = B.sb(s1, "cand", [128, 256], F32)
                        cand2 = B.sb(s1, "cand2", [128, 256], F32)
                        ce = B.sb(s1, "ce", [128, 256], F32)
                        m3 = B.sb(s1, "m3", [128, 3, 8], F32)
                        ntau = B.sb(s1, "ntau", [128, 1], F32)
                        zz = B.sb(s1, "zz", [128, 8], F32)
                        for tt_ in range(ntile):
                            r0 = tg * NT + tt_ * 128
                            B.dma("sp", x1t, T(X1.ap[r0:r0 + 128, :], "X1"))
                            B.memset("dve", ssq, 0.0)
                            B.act(h2b, x1t, AF.Square, accum=ssq)
                            B.act(ssq, ssq, AF.Sqrt, bias=eps_t, scale=1.0 / D)
                            B.recip(ssq, ssq)
                            B.ts("dve", h2b, x1t, ssq[:, 0:1], ALU.mult)
                            for half in range(2):
                                pT = pst if half == 0 else pst6
                                for k8 in range(8):
                                    kc = half * 8 + k8
                                    B.transpose(pT[:, k8 * 128:(k8 + 1) * 128].k("h", k8), h2b[:, kc * 128:(kc + 1) * 128], ident)
                                for k8 in range(8):
                                    kc = half * 8 + k8
                                    B.act(T(h2T.ap[:, kc, tt_ * 128:(tt_ + 1) * 128], (h2T.key, kc, tt_)),
                                          pT[:, k8 * 128:(k8 + 1) * 128].k("h", k8), AF.Identity,
                                          bias=modc[:, 48 + kc:49 + kc], scale=A2[:, kc:kc + 1],
                                          reads=[(pT.key, "h", k_) for k_ in range(8)])
                        for cg in range(4):
                            w = wbs[wcnt[0] % 2]
                            wcnt[0] += 1
                            B.dma("pool", w, T(peer_w_q.ap[:, cg * 512:(cg + 1) * 512].rearrange("(kc p) n -> p kc n", p=128), "pwq"))
                            for m in range(4):
                                j = cg * 4 + m
                                pt = ps[j % 2]
                                for kc in range(KC):
                                    B.mm(pt, w[:, kc, m * 128:(m + 1) * 128], T(h2T.ap[:, kc, :], (h2T.key, kc, 0)), kc == 0, kc == KC - 1,
                                         reads=[(h2T.key, kc, t_) for t_ in range(1, ntile)])
                                B.copy("act", qT[:, j, :].k(j), pt)
                        for tt_ in range(ntile):
                            cs = slice(tt_ * 128, (tt_ + 1) * 128)
                            for q4_ in range(4):
                                pt = ps[2 + q4_ % 2]
                                for i in range(4):
                                    hp = q4_ * 4 + i
                                    B.mm(pt[:, i * 128:(i + 1) * 128].k(i), qT[:, hp, cs].k(hp), keysT[:, hp, :], True, True)
                                B.copy("act", T(sall4.ap[:, tt_, q4_ * 4:q4_ * 4 + 4, :], (sall4.key, tt_, q4_)),
                                       T(pt.ap.rearrange("p (a k) -> p a k", a=4), pt.key), reads=[(pt.key, i) for i in range(4)])
                            for hp in range(16):
                                sv = T(sall4.ap[:, tt_, hp, :], (sall4.key, tt_, hp // 4))
                                vmax(tv[:, hp, 0:8].k(hp), sv)
                                vmr(s2, tv[:, hp, 0:8].k(hp), sv)
                                vmax(tv[:, hp, 8:16].k(hp), s2)
                            for h in range(8):
                                a_ = T(tv.ap[:, 2 * h, :].unsqueeze(2).to_broadcast([128, 16, 16]), (tv.key, 2 * h))
                                b_ = T(tv.ap[:, 2 * h + 1, :].unsqueeze(1).to_broadcast([128, 16, 16]), (tv.key, 2 * h + 1))
                                B.tt("dve", T(cand.ap.rearrange("p (a b) -> p a b", a=16), cand.key), a_, b_, ALU.add)
                                vmax(m3[:, 0, :], cand)
                                vmr(cand2, m3[:, 0, :], cand)
                                vmax(m3[:, 1, :], cand2)
                                vmr(cand2, m3[:, 1, :], cand2)
                                vmax(m3[:, 2, :], cand2)
                                tau_ = tau[:, tt_, h:h + 1].k(tt_, h)
                                B.stt("dve", tau_, m3[:, 1, 7:8], 0.5, m3[:, 2, 0:1], ALU.mult, ALU.add)
                                B.stt("dve", tau_, m3[:, 2, 0:1], -0.5, tau_, ALU.mult, ALU.add)
                                B.ts("dve", ntau, tau_, -1.0, ALU.mult)
                                B.act(ce, cand, AF.Exp, bias=ntau)
                                B.stt("dve", ce, cand, tau_, ce, ALU.is_ge, ALU.mult)
                                S.add("dve", lambda e, o=zz.ap[:, h:h + 1], i=ce.ap: e.reduce_sum(out=o, in_=i, axis=AX.X), reads=[ce.key], writes=[zz.key])
                            B.recip(T(rz.ap[:, tt_, :], (rz.key, tt_)), zz)
                            B.act(zz, zz, AF.Ln)
                            B.tt("dve", zz, zz, T(tau.ap[:, tt_, :], tau.key), ALU.add)
                            S.ops[-1].deps |= {S.last_w[(tau.key, tt_, h)] for h in range(8)}
                            B.ts("dve", T(negb.ap[:, tt_, :], (negb.key, tt_)), zz, -1.0, ALU.mult)
                    S.fence()
                    with ExitStack() as s5:
                        Gb = [B.sb(s5, "Gb%d" % i, [128, 4, NT], BF16) for i in range(2)]
                        Eb = [B.sb(s5, "EbH%d" % i, [128, 512], F32) for i in range(4)]
                        Wh = [B.sb(s5, "Wh%d" % i, [128, 512], BF16) for i in range(4)]
                        cf = [B.sb(s5, "cf%d" % i, [128, 4, 128], BF16) for i in range(3)]
                        b4s = [B.sb(s5, "b4_%d" % i, [128, 8, 4], F32) for i in range(2)]
                        negs = CFG.get('h_eg', 32)
                        Us, Vs = {}, {}

                        def load_U(eg):
                            Us[eg] = wbs[eg % 2]
                            B.dma("pool", Us[eg], T(peer_uT.ap[:, eg * 512:(eg + 1) * 512].rearrange("(kc p) n -> p kc n", p=128), "puT"))

                        def load_V(eg):
                            Vs[eg] = Vb[eg % 2]
                            B.dma("pool", Vs[eg], T(peer_v.ap[eg * 512:(eg + 1) * 512, :].rearrange("(s p) d -> p s d", p=128), "pv"))

                        def emit_A(eg, sub, kcs):
                            pA = ps[4 + sub % 2]
                            for kc in kcs:
                                B.mm(pA, Us[eg][:, kc, sub * 128:(sub + 1) * 128], T(h2T.ap[:, kc, :], (h2T.key, kc, 0)), kc == 0, kc == KC - 1)

                        def emit_gelu1(sub):
                            pA = ps[4 + sub % 2]
                            gxs = gxb[sub % 2]
                            g1s = g1b_[sub % 2]
                            B.act(g1s, pA, AF.Square, scale=0.2114594)
                            B.stt("dve", g1s, g1s, 1.0, pA, ALU.add, ALU.mult)

                        def emit_gelu2(eg, sub):
                            gxs = gxb[sub % 2]
                            g1s = g1b_[sub % 2]
                            pA = ps[4 + sub % 2]
                            B.act(g1s, g1s, AF.Tanh, scale=0.7978845608)
                            B.stt("dve", Gb[eg % 2][:, sub, :].k(sub), g1s, 1.0, pA, ALU.add, ALU.mult)

                        def emit_coef(unit):
                            eg_, tt2, PSWT_, cf2 = unit
                            B.tt("dve", cf2, T(Gb[eg_ % 2].ap[:, :, tt2 * 128:(tt2 + 1) * 128], (Gb[eg_ % 2].key, 0)),
                                 T(PSWT_.ap.rearrange("p (s t) -> p s t", s=4), PSWT_.key), ALU.mult)
                            S.ops[-1].deps |= {S.last_w[k] for k in [(Gb[eg_ % 2].key, sb_) for sb_ in range(4)] if k in S.last_w}

                        def emit_b4t4(eg_, tt2, slot):
                            s1v = T(sall4.ap[:, tt2].rearrange("q (h p) k -> q h p k", p=2)[:, :, 0, eg_ * 4:eg_ * 4 + 4], sall4.key)
                            B.tt("dve", b4s[slot], s1v, T(negb.ap[:, tt2, :].unsqueeze(2).to_broadcast([128, 8, 4]), negb.key), ALU.add)

                        load_U(0)
                        load_V(0)
                        if negs > 1:
                            load_U(1)
                        for sub in range(4):
                            emit_A(0, sub, range(KC))
                            emit_gelu1(sub)
                            emit_gelu2(0, sub)
                        units = [(eg, tt_) for eg in range(negs) for tt_ in range(ntile)]
                        emit_b4t4(0, 0, 0)
                        done = []
                        pend_add = []
                        pend_g2 = None
                        ectr = 0
                        for ui, (eg, tt_) in enumerate(units):
                            if tt_ == 0 and eg + 2 < negs:
                                load_U(eg + 2)
                            if tt_ == 2 and eg + 1 < negs:
                                load_V(eg + 1)
                            PSWT = ps[2 + ui % 2]
                            cf_ = cf[ui % 3]
                            b4 = b4s[ui % 2]
                            if ui + 1 < len(units):
                                emit_b4t4(units[ui + 1][0], units[ui + 1][1], (ui + 1) % 2)
                            vsrc = done[ui - 2] if ui >= 2 else None
                            for h in range(8):
                                e_, w_ = Eb[ectr % 4], Wh[ectr % 4]
                                ectr += 1
                                s2v = T(sall4.ap[:, tt_, 2 * h + 1, :], sall4.key)
                                for j in range(4):
                                    B.act(e_[:, j * 128:(j + 1) * 128].k(j), s2v, AF.Exp, bias=b4[:, h, j:j + 1])
                                S.add("dve", lambda e, o=w_.ap, a=e_.ap, sc_=rz.ap[:, tt_, h:h + 1]:
                                      e.scalar_tensor_tensor(out=o, in0=a, scalar=sc_, in1=a, op0=ALU.is_ge, op1=ALU.mult),
                                      reads=[(e_.key, j) for j in range(4)] + [(rz.key, tt_)], writes=[w_.key])
                                for fn in pend_add:
                                    fn()
                                pend_add = []
                                if h == 1 and ui >= 1:
                                    emit_coef(done[ui - 1])
                                if h == 2 and pend_g2 is not None:
                                    emit_gelu2(*pend_g2)
                                    pend_g2 = None
                                if eg + 1 < negs:
                                    emit_A(eg + 1, tt_, [2 * h, 2 * h + 1])
                                if vsrc is not None:
                                    eg2, tt2, _, cf2 = vsrc
                                    dc = h // 2
                                    po = ps[6 + dc % 2]
                                    for sub in (2 * (h % 2), 2 * (h % 2) + 1):
                                        B.mm(po, cf2[:, sub, :], Vs[eg2][:, sub, dc * 512:(dc + 1) * 512], sub == 0, sub == 3)
                                    if h % 2 == 1:
                                        a_ = T(acc.ap[:, tt2, dc * 512:(dc + 1) * 512], (acc.key, tt2, dc))
                                        pend_add.append(lambda a_=a_, po=po: B.tt("dve", a_, a_, po, ALU.add))
                                for sub in range(4):
                                    B.mm(PSWT[:, sub * 128:(sub + 1) * 128], w_[:, sub * 128:(sub + 1) * 128], ident, h == 0 and sub == 0, h == 7, sgc=True)
                            if eg + 1 < negs:
                                emit_gelu1(tt_)
                                pend_g2 = (eg + 1, tt_)
                            done.append((eg, tt_, PSWT, cf_))
                        for fn in pend_add:
                            fn()
                        emit_coef(done[-1])
                        for vsrc in done[-2:]:
                            eg2, tt2, _, cf2 = vsrc
                            for dc in range(4):
                                po = ps[6 + dc % 2]
                                for sub in range(4):
                                    B.mm(po, cf2[:, sub, :], Vs[eg2][:, sub, dc * 512:(dc + 1) * 512], sub == 0, sub == 3)
                                a_ = T(acc.ap[:, tt2, dc * 512:(dc + 1) * 512], (acc.key, tt2, dc))
                                B.tt("dve", a_, a_, po, ALU.add)
                    S.fence()
                    for tt_ in range(ntile):
                        r0 = tg * NT + tt_ * 128
                        B.dma("sp", x1t, T(X1.ap[r0:r0 + 128, :], "X1"))
                        B.tt("dve", h2f, T(acc.ap[:, tt_, :], acc.key), g2b, ALU.mult)
                        S.ops[-1].deps |= {S.last_w[k] for k in [(acc.key, tt_, dc) for dc in range(4)] if k in S.last_w}
                        B.tt("pool", h2f, h2f, x1t, ALU.add)
                        final_ops.append(B.dma("sp", T(out.ap[r0:r0 + 128, :], "out"), h2f, wkey=("out", r0)))
                    S.fence()
            S.fence()

        if "Z" in ph:
            with ExitStack() as st:
                t_ = B.sb(st, "zz", [128, D], F32)
                for i in range(16):
                    B.dma("sp", t_, T(xtok.ap[i * 128:(i + 1) * 128, :], "xtok"))
                    final_ops.append(B.dma("sp", T(out.ap[i * 128:(i + 1) * 128, :], "out"), t_, wkey=("out", i)))
    stats = S.emit(final_ops)
    return nc, stats


def host_inputs(inputs):
    x = np.asarray(inputs["x"], np.float32)
    c = np.asarray(inputs["c"], np.float32)
    g = lambda k: np.asarray(inputs[k], np.float32)[0]
    w_in, b_in = g("w_in"), g("b_in")
    shared = {
        "w_ada": np.ascontiguousarray(g("w_ada")),
        "b_ada": g("b_ada").reshape(1, -1),
        "w_in": np.ascontiguousarray(w_in),
        "b_in": b_in.reshape(1, -1),
        "g1_col": np.ascontiguousarray(g("norm1_g").reshape(KC, 128).T),
        "g2_col": np.ascontiguousarray(g("norm2_g").reshape(KC, 128).T),
    }
    bc = np.zeros((128, 96), np.float32)
    def put(col0, off, n):
        for i in range(n):
            bc[:, col0 + i] = b_in[off + i * 128: off + (i + 1) * 128]
    put(0, OQ, 16); put(16, OKC, 4); put(20, OVC, 4); put(24, OKS, 4); put(28, OKW, 4)
    bc[0:48, 32] = b_in[OGN:OGN + 48]
    put(48, OGLU, 8); put(56, OGLU + 1024, 8); put(64, OGM, 32)
    shared["b_in_col"] = bc
    kg = g("k_norm_g")
    shared["qkg_col"] = np.ascontiguousarray(np.stack([g("q_norm_g"), kg[0], kg[1], kg[2]], axis=1))
    shared["cmp_k_w1"] = g("cmp_k_w1"); shared["cmp_v_w1"] = g("cmp_v_w1")
    shared["cmp_k_w2"] = g("cmp_k_w2"); shared["cmp_v_w2"] = g("cmp_v_w2")
    shared["cmp_posT_k"] = np.ascontiguousarray(g("cmp_pos_k").T); shared["cmp_posT_v"] = np.ascontiguousarray(g("cmp_pos_v").T)
    shared["w_nsa_out"] = g("w_nsa_out"); shared["conv_pw_w"] = g("conv_pw_w"); shared["w_out"] = g("w_out")
    shared["peer_w_q"] = g("peer_w_q")
    shared["dwT"] = np.ascontiguousarray(g("conv_dw_w").T.reshape(8, 128, 31).transpose(1, 0, 2))
    shared["cvec"] = np.ascontiguousarray(np.stack([g("conv_dw_b"), g("conv_ln_g"), g("conv_ln_b")], 0).reshape(3, 8, 128).transpose(2, 0, 1))
    shared["pwb_col"] = np.ascontiguousarray(g("conv_pw_b").reshape(16, 128).T)
    shared["keysT"] = np.ascontiguousarray(g("peer_sub_keys").reshape(16, 128, 128).transpose(2, 0, 1))
    if "H" in CFG["phases"]:
        shared["peer_uT"] = np.ascontiguousarray(g("peer_u").T)
        shared["peer_v"] = g("peer_v")
    else:
        shared["peer_uT"] = np.zeros((128, 128), np.float32)
        shared["peer_v"] = np.zeros((128, 128), np.float32)
    shared.update(_shared_consts())
    maps = []
    for core in range(8):
        b, hf = core // 2, core % 2
        xb = x[b]
        if hf == 1:
            seg = xb
        else:
            seg = np.concatenate([np.zeros((NPRE, D), np.float32), xb[:NTOK]], axis=0)
        m = dict(shared)
        m["xT"] = np.ascontiguousarray(seg.T.reshape(KC, 128, NPRE + NTOK).transpose(1, 0, 2))
        m["xtok"] = np.ascontiguousarray(xb[hf * NTOK:(hf + 1) * NTOK])
        m["c_col"] = np.ascontiguousarray(c[b].reshape(KC, 128).T)
        m["pvalid"] = np.full((128, 1), float(hf), np.float32)
        m.update(_core_consts(hf))
        maps.append(m)
    return maps


def _shared_consts():
    c = {}
    c["ident"] = np.eye(128, dtype=np.float32)
    i = np.arange(256)[:, None]; j = np.arange(64)[None, :]
    ov = ((16 * i < 64 * j + 64) & (16 * i + 32 > 64 * j) & (i < 255)).astype(np.float32)
    c["OVL"] = np.ascontiguousarray(ov.reshape(2, 128, 64).transpose(1, 0, 2))
    p = np.arange(128)[:, None, None]; di = np.arange(4)[None, :, None]; u = np.arange(512)[None, None, :]
    c["SELC"] = (128 * di + p <= u).astype(np.float32)
    jj = np.arange(64)[:, None, None]; cc = np.arange(32)[None, :, None]; pp = np.arange(128)[None, None, :]
    c["EX"] = (jj == 2 * cc + pp // 64).astype(np.float32)
    return c


def _core_consts(hf):
    c = {}
    u = np.arange(NTOK)
    col = NPRE + u
    cur = col // 64
    j = np.arange(64)[None, :]
    glob_j = j - 32 * (1 - hf)
    glob_cur = (cur - 32 * (1 - hf))[:, None]
    forced = (glob_j == 0) | (glob_j == glob_cur) | (glob_j == glob_cur - 1)
    valid = (glob_j >= 0) & (glob_j <= glob_cur)
    fval = np.where(glob_j == 0, 3e30, np.where(glob_j == glob_cur, 2e30, 1e30))
    selb = np.where(valid, np.where(forced, fval, 0.0), -1e30).astype(np.float32)
    c["SELB"] = selb
    i = np.arange(256)[:, None]
    vis = (16 * i + 31 <= col[None, :]) & (i < 255) & ((i >= 128) | (hf == 1))
    c["CMPM"] = np.ascontiguousarray(vis.astype(np.float32).reshape(2, 128, NTOK).transpose(1, 0, 2))
    wm = np.zeros((4, 128, 8, 512), np.float32)
    p = np.arange(128)[:, None]; uu = np.arange(512)[None, :]
    for qt in range(4):
        q0 = NPRE + qt * 512
        for k in range(8):
            k0 = q0 - 512 + 128 * k
            diff = (q0 + uu) - (k0 + p)
            ok = (diff >= 0) & (diff < 512) & ((k0 + p >= NPRE) | (hf == 1))
            wm[qt, :, k, :] = ok
    c["WINM"] = wm
    return c


_CACHE = {}


def kernel(**inputs):
    maps = host_inputs(inputs)
    if "nc" not in _CACHE:
        _CACHE["nc"] = build_program()
    nc, stats = _CACHE["nc"]
    res = run_bass_kernel_spmd(nc, maps, core_ids=list(range(8)))
    _CACHE["res"] = res
    outp = np.zeros((4, 4096, D), np.float32)
    for core in range(8):
        b, hf = core // 2, core % 2
        outp[b, hf * NTOK:(hf + 1) * NTOK] = res.results[core]["out"]
    return outp
```
